# Optimizing a Trainium2 kernel written in Bass

```python
import math
import jax, jax.numpy as jnp
from jax import lax
import numpy as np

D_MODEL = 1024
BATCH = 8
SEQ = 2048
DEPTH = 2

HEAD_DIM = 64
NORM_EPS = 1e-6
NEG_INF = -1e30
SWA_PATTERNS = ((128, 1), (512, 4), (2048, 16))
SWA_HEADS_PER_GROUP = 4
SWA_HEADS = SWA_HEADS_PER_GROUP * len(SWA_PATTERNS)
SWA_WIDTH = SWA_HEADS * HEAD_DIM
SWA_OUT_WIDTH = SWA_HEADS_PER_GROUP * HEAD_DIM
RWKV_HEADS = 12
RWKV_WIDTH = RWKV_HEADS * HEAD_DIM
DECAY_LORA = 64
ICLR_LORA = 64
GATE_LORA = 128
RWKV_STREAM = 3 * RWKV_WIDTH + 2 * DECAY_LORA + 2 * ICLR_LORA + GATE_LORA
RWKV_GN_EPS = 64e-5
DIFF_HEADS = 6
DIFF_WIDTH = DIFF_HEADS * 2 * HEAD_DIM
DIFF_Q_BLOCK = 128
DIFF_SUBLN_EPS = 1e-5
REL_BUCKETS = 32
REL_MAX_DIST = 128
REL_HEADS = SWA_HEADS + DIFF_HEADS
IN_COLS = 3 * SWA_WIDTH + RWKV_STREAM + 3 * DIFF_WIDTH + 3 * D_MODEL
N_EXPERTS = 32
TOP_K = 4
D_EXPERT = 1024
SWIGLU_LIMIT = 7.0
SWIGLU_ALPHA = 1.702

kernel_name = 'hybrid_gated_dilated_rwkv7_diffattn_moe_encoder'


def _split(t, sizes):
    out, off = [], 0
    for s in sizes:
        out.append(t[..., off:off + s])
        off += s
    return out


def _rmsnorm(x, g, eps=NORM_EPS):
    xf = x.astype(jnp.float32)
    y = xf * lax.rsqrt(jnp.mean(xf * xf, axis=-1, keepdims=True) + eps)
    return (y * g.astype(jnp.float32)).astype(x.dtype)


def _t5_bucket(rel):
    nb = REL_BUCKETS // 2
    max_exact = nb // 2
    ret = jnp.where(rel > 0, nb, 0)
    n = jnp.abs(rel)
    nf = jnp.maximum(n, 1).astype(jnp.float32)
    large = max_exact + (jnp.log(nf / max_exact) / math.log(REL_MAX_DIST / max_exact) * (nb - max_exact)).astype(jnp.int32)
    large = jnp.minimum(large, nb - 1)
    return ret + jnp.where(n < max_exact, n, large)


def _dilated_window_attn(q, k, v, bias, dilation, half):
    B, T, H, Dh = q.shape
    L = T // dilation
    nb = -(-L // half)
    Lp = nb * half
    Z = B * dilation

    def to_strided(t):
        return t.reshape(B, L, dilation, H, Dh).transpose(0, 2, 3, 1, 4).reshape(Z, H, L, Dh)

    qs = jnp.pad(to_strided(q), ((0, 0), (0, 0), (0, Lp - L), (0, 0))).reshape(Z, H, nb, half, Dh)

    def windows(t):
        t = jnp.pad(to_strided(t), ((0, 0), (0, 0), (half, half + Lp - L), (0, 0))).reshape(Z, H, nb + 2, half, Dh)
        return jnp.concatenate([t[:, :, 0:nb], t[:, :, 1:nb + 1], t[:, :, 2:nb + 2]], axis=3)

    kw, vw = windows(k), windows(v)
    s = jnp.einsum('zhnqd,zhnkd->zhnqk', qs, kw).astype(jnp.float32) * (Dh ** -0.5)
    a_idx = jnp.arange(half)[:, None]
    c_idx = jnp.arange(3 * half)[None, :]
    j = c_idx - half - a_idx
    key_m = jnp.arange(nb)[:, None] * half - half + jnp.arange(3 * half)[None, :]
    mask = (jnp.abs(j) <= half)[None] & ((key_m >= 0) & (key_m < L))[:, None, :]
    b = bias[:, jnp.clip(j + half, 0, 2 * half)].astype(jnp.float32)
    s = jnp.where(mask, s + b[:, None], NEG_INF)
    lse = jax.nn.logsumexp(s, axis=-1)
    p = jnp.exp(s - lse[..., None])
    o = jnp.einsum('zhnqk,zhnkd->zhnqd', p.astype(v.dtype), vw)
    o = o.reshape(B, dilation, H, Lp, Dh)[:, :, :, :L].transpose(0, 3, 1, 2, 4).reshape(B, T, H, Dh)
    lse = lse.reshape(B, dilation, H, Lp)[..., :L].transpose(0, 3, 1, 2).reshape(B, T, H)
    return o, lse


def _centred_shift(p, mu):
    prev = jnp.pad(p[:, :-1], ((0, 0), (1, 0), (0, 0)))
    nxt = jnp.pad(p[:, 1:], ((0, 0), (0, 1), (0, 0)))
    return p + mu[0] * (prev - p) + mu[1] * (nxt - p)


def _wkv7_scan(r, w, k, v, kk, a, reverse):
    B, T, H, N = r.shape

    def step(S, inp):
        r_t, w_t, k_t, v_t, kk_t, a_t = inp
        sa = jnp.einsum('bhvk,bhk->bhv', S, -kk_t)
        S = S * w_t[:, :, None, :] + sa[..., None] * (kk_t * a_t)[:, :, None, :] + v_t[..., None] * k_t[:, :, None, :]
        return S, jnp.einsum('bhvk,bhk->bhv', S, r_t)

    xs = tuple(jnp.moveaxis(t.astype(jnp.float32), 1, 0) for t in (r, w, k, v, kk, a))
    S0 = jnp.zeros((B, H, N, N), jnp.float32)
    _, y = lax.scan(step, S0, xs, reverse=reverse)
    return jnp.moveaxis(y, 0, 1)


def _rwkv7_bidir(stream, mu, w0, w_up, a0, a_up, g_up, k_k, k_a, r_k, ln_g, ln_b):
    B, T, _ = stream.shape
    H, N = RWKV_HEADS, HEAD_DIM
    s = _centred_shift(stream, mu)
    r, k, v, wd, ad, gd = _split(s, (RWKV_WIDTH, RWKV_WIDTH, RWKV_WIDTH, 2 * DECAY_LORA, 2 * ICLR_LORA, GATE_LORA))
    wd = wd.reshape(B, T, 2, DECAY_LORA)
    ad = ad.reshape(B, T, 2, ICLR_LORA)

    def heads(t):
        return t.reshape(B, T, H, N)

    rh, vh = heads(r), heads(v)
    y = jnp.zeros((B, T, H, N), jnp.float32)
    bonus = jnp.zeros((B, T, H, N), jnp.float32)
    for di, rev in enumerate((False, True)):
        w_log = -jax.nn.softplus(-(w0[di] + jnp.tanh(wd[:, :, di]) @ w_up[di])) - 0.5
        decay = jnp.exp(-jnp.exp(w_log.astype(jnp.float32)))
        a = jax.nn.sigmoid(a0[di] + ad[:, :, di] @ a_up[di])
        kk = heads(k * k_k[di]).astype(jnp.float32)
        kk = kk * lax.rsqrt(jnp.maximum(jnp.sum(kk * kk, axis=-1, keepdims=True), 1e-24))
        kd = heads(k * (1 + (a - 1) * k_a[di]))
        y = y + _wkv7_scan(rh, heads(decay), kd, vh, kk, heads(a), rev)
        bonus = bonus + jnp.sum((rh * kd * r_k).astype(jnp.float32), axis=-1, keepdims=True) * vh.astype(jnp.float32)
    mean = jnp.mean(y, axis=-1, keepdims=True)
    var = jnp.mean(jnp.square(y - mean), axis=-1, keepdims=True)
    y = ((y - mean) * lax.rsqrt(var + RWKV_GN_EPS)).reshape(B, T, RWKV_WIDTH) * ln_g.astype(jnp.float32) + ln_b.astype(jnp.float32)
    g = jax.nn.sigmoid(gd) @ g_up
    return ((y + bonus.reshape(B, T, RWKV_WIDTH)) * g.astype(jnp.float32)).astype(stream.dtype)


def _diff_attention(q, k, v, table, lam, lambda_init, subln_g):
    B, T, H, _, Dh = q.shape
    nqb = T // DIFF_Q_BLOCK
    qb = jnp.moveaxis(q.reshape(B, nqb, DIFF_Q_BLOCK, H, 2, Dh), 1, 0)
    kpos = jnp.arange(T)

    def block(args):
        q_blk, i = args
        qpos = i * DIFF_Q_BLOCK + jnp.arange(DIFF_Q_BLOCK)
        bias = table[_t5_bucket(kpos[None, :] - qpos[:, None])]
        s = jnp.einsum('bqhcd,bkhcd->bchqk', q_blk, k).astype(jnp.float32) * (Dh ** -0.5)
        p = jax.nn.softmax(s + jnp.transpose(bias, (2, 0, 1)).astype(jnp.float32)[None, None], axis=-1)
        attn = p[:, 0] - lam * p[:, 1]
        return jnp.einsum('bhqk,bkhd->bqhd', attn.astype(v.dtype), v)

    o = lax.map(block, (qb, jnp.arange(nqb)))
    o = jnp.moveaxis(o, 0, 1).reshape(B, T, H, 2 * Dh)
    o = _rmsnorm(o, subln_g, DIFF_SUBLN_EPS) * (1.0 - lambda_init)
    return o.reshape(B, T, DIFF_WIDTH)


def _moe(h, router_w, router_b, w1, b1, w2, b2):
    B, T, D = h.shape
    ht = h.reshape(B * T, D)
    logits = (ht @ router_w + router_b).astype(jnp.float32)
    top_v, top_i = lax.top_k(logits, TOP_K)
    probs = jax.nn.softmax(top_v, axis=-1)
    gates = jnp.sum(jax.nn.one_hot(top_i, N_EXPERTS, dtype=jnp.float32) * probs[..., None], axis=1)

    def expert(acc, params):
        w1e, b1e, w2e, b2e, ge = params
        hh = ht @ w1e + b1e
        x_glu = jnp.minimum(hh[:, ::2], SWIGLU_LIMIT)
        x_lin = jnp.clip(hh[:, 1::2], -SWIGLU_LIMIT, SWIGLU_LIMIT)
        act = x_glu * jax.nn.sigmoid(SWIGLU_ALPHA * x_glu) * (x_lin + 1)
        out = act @ w2e + b2e
        return acc + ge[:, None] * out.astype(jnp.float32), None

    acc0 = jnp.zeros((B * T, D), jnp.float32)
    acc, _ = lax.scan(expert, acc0, (w1, b1, w2, b2, gates.T))
    return acc.reshape(B, T, D).astype(h.dtype)


def setup_inputs(seed: int = 0) -> dict:
    key = jax.random.key(seed)
    ks = iter(jax.random.split(key, 40))
    L, D, W, F, E = DEPTH, D_MODEL, RWKV_WIDTH, D_EXPERT, N_EXPERTS

    def nrm(shape, scale):
        return jax.random.normal(next(ks), shape, jnp.float32) * scale

    def uni(shape, lo, hi):
        return jax.random.uniform(next(ks), shape, jnp.float32, lo, hi)

    return {
        'x': nrm((BATCH, SEQ, D), 1.0),
        'c': nrm((BATCH, D), 1.0),
        'w_mod': nrm((L, D, 6 * D), 0.3 * D ** -0.5),
        'b_mod': nrm((L, 6 * D), 0.1),
        'norm1_g': 1.0 + nrm((L, D), 0.05),
        'norm2_g': 1.0 + nrm((L, D), 0.05),
        'w_in': nrm((L, D, IN_COLS), D ** -0.5),
        'rwkv_mu': uni((L, 2, RWKV_STREAM), 0.0, 0.5),
        'rwkv_w0': uni((L, 2, W), -6.0, 1.0),
        'rwkv_w_up': nrm((L, 2, DECAY_LORA, W), 0.5 * DECAY_LORA ** -0.5),
        'rwkv_a0': nrm((L, 2, W), 0.5),
        'rwkv_a_up': nrm((L, 2, ICLR_LORA, W), 0.5 * ICLR_LORA ** -0.5),
        'rwkv_g_up': nrm((L, GATE_LORA, W), GATE_LORA ** -0.5),
        'rwkv_k_k': 0.85 + nrm((L, 2, W), 0.05),
        'rwkv_k_a': 1.0 + nrm((L, 2, W), 0.05),
        'rwkv_r_k': nrm((L, RWKV_HEADS, HEAD_DIM), 0.1),
        'rwkv_ln_g': 1.0 + nrm((L, W), 0.05),
        'rwkv_ln_b': nrm((L, W), 0.02),
        'diff_lambda': nrm((L, 4, HEAD_DIM), 0.1),
        'diff_subln_g': 1.0 + nrm((L, 2 * HEAD_DIM), 0.05),
        'rel_bias': nrm((REL_BUCKETS, REL_HEADS), 0.5),
        'w_branch_a': nrm((L, SWA_OUT_WIDTH, D), SWA_OUT_WIDTH ** -0.5),
        'w_branch_b': nrm((L, RWKV_WIDTH, D), RWKV_WIDTH ** -0.5),
        'w_branch_c': nrm((L, DIFF_WIDTH, D), DIFF_WIDTH ** -0.5),
        'w_out': nrm((L, D, D), D ** -0.5),
        'router_w': nrm((L, D, E), D ** -0.5),
        'router_b': nrm((L, E), 0.01),
        'moe_w1': nrm((L, E, D, 2 * F), D ** -0.5),
        'moe_b1': nrm((L, E, 2 * F), 0.01),
        'moe_w2': nrm((L, E, F, D), F ** -0.5),
        'moe_b2': nrm((L, E, D), 0.01),
        'final_norm_g': 1.0 + nrm((D,), 0.05),
    }


def reference(x, c, w_mod, b_mod, norm1_g, norm2_g, w_in, rwkv_mu, rwkv_w0, rwkv_w_up, rwkv_a0, rwkv_a_up, rwkv_g_up, rwkv_k_k, rwkv_k_a, rwkv_r_k, rwkv_ln_g, rwkv_ln_b, diff_lambda, diff_subln_g, rel_bias, w_branch_a, w_branch_b, w_branch_c, w_out, router_w, router_b, moe_w1, moe_b1, moe_w2, moe_b2, final_norm_g):
    B, T, D = x.shape
    n_groups = len(SWA_PATTERNS)
    bias_a = []
    for g, (window, dil) in enumerate(SWA_PATTERNS):
        half = window // (2 * dil)
        offs = jnp.arange(-half, half + 1) * dil
        bias_a.append(rel_bias[_t5_bucket(offs)][:, g * SWA_HEADS_PER_GROUP:(g + 1) * SWA_HEADS_PER_GROUP].T)
    table_c = rel_bias[:, SWA_HEADS:]
    cond = jax.nn.silu(c)

    for l in range(DEPTH):
        mod = (cond @ w_mod[l] + b_mod[l])[:, None, :]
        sh1, sc1, gt1, sh2, sc2, gt2 = jnp.split(mod, 6, axis=-1)

        h = _rmsnorm(x, norm1_g[l]) * (1 + sc1) + sh1
        proj = h @ w_in[l]
        a_qkv, b_stream, c_qkv, gate_logits = _split(proj, (3 * SWA_WIDTH, RWKV_STREAM, 3 * DIFF_WIDTH, 3 * D))

        aq, ak, av = [t.reshape(B, T, n_groups, SWA_HEADS_PER_GROUP, HEAD_DIM) for t in _split(a_qkv, (SWA_WIDTH, SWA_WIDTH, SWA_WIDTH))]
        outs, lses = [], []
        for g, (window, dil) in enumerate(SWA_PATTERNS):
            o_g, lse_g = _dilated_window_attn(aq[:, :, g], ak[:, :, g], av[:, :, g], bias_a[g], dil, window // (2 * dil))
            outs.append(o_g.astype(jnp.float32))
            lses.append(lse_g)
        wts = jax.nn.softmax(jnp.stack(lses, axis=0), axis=0)
        o_a = jnp.sum(wts[..., None] * jnp.stack(outs, axis=0), axis=0).reshape(B, T, SWA_OUT_WIDTH).astype(x.dtype)

        o_b = _rwkv7_bidir(b_stream, rwkv_mu[l], rwkv_w0[l], rwkv_w_up[l], rwkv_a0[l], rwkv_a_up[l], rwkv_g_up[l], rwkv_k_k[l], rwkv_k_a[l], rwkv_r_k[l], rwkv_ln_g[l], rwkv_ln_b[l])

        cq, ck, cv = _split(c_qkv, (DIFF_WIDTH, DIFF_WIDTH, DIFF_WIDTH))
        dl = diff_lambda[l].astype(jnp.float32)
        lambda_init = 0.8 - 0.6 * math.exp(-0.3 * l)
        lam = jnp.exp(jnp.sum(dl[0] * dl[1])) - jnp.exp(jnp.sum(dl[2] * dl[3])) + lambda_init
        o_c = _diff_attention(cq.reshape(B, T, DIFF_HEADS, 2, HEAD_DIM), ck.reshape(B, T, DIFF_HEADS, 2, HEAD_DIM), cv.reshape(B, T, DIFF_HEADS, 2 * HEAD_DIM), table_c, lam, lambda_init, diff_subln_g[l])

        g_a, g_b, g_c = jnp.split(jax.nn.sigmoid(gate_logits), 3, axis=-1)
        merged = g_a * (o_a @ w_branch_a[l]) + g_b * (o_b @ w_branch_b[l]) + g_c * (o_c @ w_branch_c[l])
        x = x + gt1 * (merged @ w_out[l])

        h2 = _rmsnorm(x, norm2_g[l]) * (1 + sc2) + sh2
        x = x + gt2 * _moe(h2, router_w[l], router_b[l], moe_w1[l], moe_b1[l], moe_w2[l], moe_b2[l])

    return _rmsnorm(x, final_norm_g)
```

```python
import math
import numpy as np
from contextlib import ExitStack
import concourse.bass as bass
import concourse.mybir as mybir
from concourse.bass_utils import run_bass_kernel_spmd

F32 = mybir.dt.float32
BF16 = mybir.dt.bfloat16
AF = mybir.ActivationFunctionType
ALU = mybir.AluOpType
AX = mybir.AxisListType

D = 1024
T = 2048
NT = 16
KC = 8
L = 2
E = 32
INC = 10368
SEM_CHUNK = 30000
A_OFF, B_OFF, C_OFF, G_OFF = 0, 2304, 4992, 7296
DECAY_C = math.exp(-0.5)


class Res:
    __slots__ = ("name", "w", "r", "dsem", "dcnt")

    def __init__(self, name):
        self.name = name
        self.w = None
        self.r = []
        self.dsem = None
        self.dcnt = 0


class Prog:
    ENGS = ("sp", "act", "dve", "pool", "pe")

    def __init__(self, nc):
        self.nc = nc
        self.q = {e: [] for e in self.ENGS}
        self.cnt = {e: 0 for e in self.ENGS}
        self.known = {e: {} for e in self.ENGS}
        self.ndsem = 0
        self.dma_final = {}

    def _collect(self, eng, reads, writes, is_dma):
        waits = {}

        def need(ev, kind):
            if ev is None:
                return
            key, val, src = ev
            if src == eng and not is_dma and key[0] == "e":
                if eng == "pe" or kind == "war":
                    return
            if self.known[eng].get(key, 0) >= val:
                return
            if waits.get(key, 0) < val:
                waits[key] = val

        for r in reads:
            need(r.w, "raw")
        for w in writes:
            need(w.w, "waw")
            for ev in w.r:
                need(ev, "war")
        for k, v in waits.items():
            self.known[eng][k] = v
        return waits

    def _commit(self, ev, reads, writes):
        for r in reads:
            r.r.append(ev)
            if len(r.r) > 24:
                best = {}
                for e2 in r.r:
                    if e2[0] not in best or best[e2[0]][1] < e2[1]:
                        best[e2[0]] = e2
                r.r = list(best.values())
        for w in writes:
            w.w = ev
            w.r = []

    def op(self, eng, fn, reads=(), writes=()):
        waits = self._collect(eng, reads, writes, False)
        n = self.cnt[eng] + 1
        self.cnt[eng] = n
        ev = (("e", eng, (n - 1) // SEM_CHUNK), (n - 1) % SEM_CHUNK + 1, eng)
        self.q[eng].append((waits, fn, [(ev[0], 1)]))
        self._commit(ev, reads, writes)
        return ev

    def dma(self, out_ap, in_ap, reads=(), writes=(), eng="sp", **kw):
        waits = self._collect(eng, reads, writes, True)
        w0 = writes[0]
        if w0.dsem is None:
            w0.dsem = self.ndsem
            self.ndsem += 1
        w0.dcnt += 16
        ev = (("d", w0.dsem), w0.dcnt, "dma")
        self.dma_final[ev[0]] = w0.dcnt

        def fn(e, out_ap=out_ap, in_ap=in_ap, kw=kw):
            return e.dma_start(out=out_ap, in_=in_ap, **kw)

        self.q[eng].append((waits, fn, [(ev[0], 16)]))
        self._commit(ev, reads, writes)
        return ev

    def final_wait(self, eng, res_list):
        waits = dict(self.dma_final)
        for r in res_list:
            if r.w is not None:
                key, val, _ = r.w
                waits[key] = max(waits.get(key, 0), val)
        self.q[eng].append((waits, None, []))

    def emit(self, stack):
        nc = self.nc
        sems = {}
        for e in self.ENGS:
            for waits, fn, incs in self.q[e]:
                for k in list(waits) + [k for k, _ in incs]:
                    if k not in sems:
                        sems[k] = stack.enter_context(nc.semaphore("s_" + "_".join(str(x) for x in k)))
        self.nsems = len(sems)
        block = stack.enter_context(nc.Block())

        def replay(engname):
            def body(e):
                for waits, fn, incs in self.q[engname]:
                    for k, v in waits.items():
                        e.wait_ge(sems[k], v)
                    if fn is None:
                        continue
                    ins = fn(e)
                    for k, v in incs:
                        ins = ins.then_inc(sems[k], v)
            return body

        block.sync(replay("sp"))
        block.scalar(replay("act"))
        block.vector(replay("dve"))
        block.gpsimd(replay("pool"))
        block.tensor(replay("pe"))


def _t5_bucket_np(rel):
    nb, max_exact = 16, 8
    rel = np.asarray(rel, np.int64)
    ret = np.where(rel > 0, nb, 0)
    n = np.abs(rel)
    nf = np.maximum(n, 1).astype(np.float32)
    large = max_exact + (np.log(nf / np.float32(max_exact)) / np.float32(math.log(128 / max_exact)) * np.float32(nb - max_exact)).astype(np.int32)
    large = np.minimum(large, nb - 1)
    return ret + np.where(n < max_exact, n, large)


def _host_consts():
    cst = np.zeros((128, 1792), np.float32)
    cst[:, 0:128] = np.eye(128, dtype=np.float32)
    r = np.arange(128)[:, None]
    c = np.arange(128)[None, :]
    su, iu = (r < c), (r <= c)
    sl, il = (r > c), (r >= c)
    cst[:, 128:768] = np.concatenate([su, iu, su, iu, sl], 1)
    cst[:, 768:1408] = np.concatenate([sl, il, sl, il, su], 1)
    cst[:, 1408:1536] = 1.0
    EA = np.zeros((33, 1536), np.float32)
    for g, d in enumerate((1, 4, 16)):
        j = np.arange(512) - 255
        b = _t5_bucket_np(j * d)
        b = np.where(np.abs(j) <= 64, b, 32)
        EA[b, g * 512 + np.arange(512)] = 1.0
    EC = np.zeros((33, 1536), np.float32)
    b = _t5_bucket_np(np.arange(1536) - 767)
    EC[b, np.arange(1536)] = 1.0
    return cst, EA, EC


class Builder:
    def __init__(self, layers=(0, 1), do_mixer=True, do_moe=True, dbg=()):
        self.layers = layers
        self.do_mixer = do_mixer
        self.do_moe = do_moe
        self.dbg = dbg
        self.nc = bass.Bass("TRN2", target_bir_lowering=False)
        self.st = ExitStack()
        self.P = Prog(self.nc)
        self.outs = []
        self._uid = 0

    def din(self, name, shape, dt=F32):
        return self.nc.dram_tensor(name, list(shape), dt, kind="ExternalInput").ap()

    def dout(self, name, shape, dt=F32):
        return self.nc.dram_tensor(name, list(shape), dt, kind="ExternalOutput").ap()

    def dscratch(self, name, shape, dt=F32):
        return self.nc.dram_tensor(name, list(shape), dt, kind="Internal").ap()

    def sb(self, name, shape, dt=F32):
        return self.st.enter_context(self.nc.sbuf_tensor(name, list(shape), dt))

    def ps(self, name, shape, dt=F32):
        return self.st.enter_context(self.nc.psum_tensor(name, list(shape), dt))

    def R(self, name):
        self._uid += 1
        return Res(f"{name}{self._uid}")

    def dump(self, name, ap_sb, res, shape, dt=F32):
        o = self.dout(name, shape, dt)
        r = self.R("dump")
        self.P.dma(o, ap_sb, reads=[res], writes=[r])
        self.outs.append(r)

    @staticmethod
    def carve(reg, off_bytes, shape, dt):
        n = 1
        for s in shape[1:]:
            n *= s
        esz = 4 if dt == F32 else 2
        assert off_bytes % 4 == 0
        a = off_bytes // 2
        b = a + n * esz // 2
        assert b <= reg.shape[1], (b, reg.shape)
        ap = reg[0:shape[0], a:b]
        if dt == F32:
            ap = ap.bitcast(F32)
        if len(shape) == 2:
            return ap
        names = " ".join(f"d{i}" for i in range(len(shape) - 1))
        kw = {f"d{i}": shape[i + 1] for i in range(len(shape) - 2)}
        return ap.rearrange(f"p ({names}) -> p {names}", **kw)

    def declare(self):
        d = self.din
        self.x_in = d("x", [T, D])
        self.cT_in = d("cT", [128, 8])
        self.w_mod = d("w_mod", [L, D, 6 * D])
        self.bmodT = d("bmodT", [L, 128, 48])
        self.bmod_row = d("bmod_row", [L, 1, 6 * D])
        self.ncol = d("ncol", [L, 128, 16])
        self.w_in = d("w_in", [L, D, INC])
        self.muT = d("muT", [L, 128, 36, 2])
        self.muL = d("muL", [L, 128, 3, 2])
        self.rwcol = d("rwcol", [L, 128, 12, 4])
        self.rkcol = d("rkcol", [L, 128, 12])
        self.w_up = d("w_up", [L, 128, 768])
        self.a_up = d("a_up", [L, 128, 768])
        self.g_up = d("g_up", [L, 128, 768])
        self.ln_g = d("ln_g", [L, 768])
        self.ln_b = d("ln_b", [L, 768])
        self.dlam = d("dlam", [L, 256])
        self.subln = d("subln", [L, 128, 1])
        self.rel_bias = d("rel_bias", [32, 18])
        self.w_br_a = d("w_br_a", [L, 256, D])
        self.w_br_b = d("w_br_b", [L, 768, D])
        self.w_br_c = d("w_br_c", [L, 768, D])
        self.w_out = d("w_out", [L, D, D])
        self.router_w = d("router_w", [L, D, E])
        self.router_b = d("router_b", [L, E])
        self.moe_w1 = d("moe_w1", [L, E, D, 2, 1024])
        self.b1T = d("b1T", [L, 128, E, 2, 8])
        self.moe_w2 = d("moe_w2", [L, E, 1024, D])
        self.moe_b2 = d("moe_b2", [L, E, D])
        self.fng = d("fng", [1, D])
        self.cst_in = d("cst", [128, 1792])
        self.EA_in = d("EA", [33, 1536])
        self.EC_in = d("EC", [33, 1536])
        self.y_out = self.dout("y", [T, D])
        self.x_spill = self.dscratch("x_spill", [T, D])
        self.ebA = self.dscratch("ebA", [12, 512])
        self.ebC = self.dscratch("ebC", [6, 1536])

    def alloc(self):
        sb, ps = self.sb, self.ps
        self.ARENA = sb("ARENA", [128, 32768], BF16)
        self.HT = sb("HT", [128, KC, T], BF16)
        self.MG = sb("MG", [128, 16384], BF16)
        self.EX = sb("EX", [128, 20 * 1024], BF16)
        self.CST = sb("CST", [128, 1792], F32)
        self.IDB = sb("IDB", [128, 128], BF16)
        self.WB = [sb(f"WB{i}", [128, KC, 512], BF16) for i in range(2)]
        self.MODT = sb("MODT", [128, 48], F32)
        self.GEFF = sb("GEFF", [128, 16], F32)
        self.NCOL = sb("NCOL", [128, 16], F32)
        self.SS = sb("SS", [128, 32], F32)
        self.GTB = sb("GTB", [128, 2, D], F32)
        self.CONDT = sb("CONDT", [128, 8], F32)
        self.JUNK = sb("JUNK", [128, D], BF16)
        self.X = self.ARENA[:, :].bitcast(F32).rearrange("p (t d) -> p t d", t=NT)
        self.IDF = self.CST[:, 0:128]
        self.ONES = self.CST[:, 1408:1536]
        self.PB = [ps(f"PB{i}", [128, 512], F32) for i in range(6)]
        self.PW = ps("PW", [128, 1024], F32)
        R = self.R
        self.rX = R("X")
        self.rHT = R("HT")
        self.rMG = R("MG")
        self.rCST = R("CST")
        self.rIDB = R("IDB")
        self.rWB = [R("WB0"), R("WB1")]
        self.rPB = [R(f"PB{i}") for i in range(6)]
        self.rPW = R("PW")
        self.rMODT, self.rGEFF, self.rNCOL, self.rSS = R("MODT"), R("GEFF"), R("NCOL"), R("SS")
        self.rGTB, self.rCONDT, self.rJUNK = R("GTB"), R("CONDT"), R("JUNK")
        self.rEX = R("EX")
        self.wbi = 0

    def prologue(self):
        P = self.P
        P.dma(self.CST[:], self.cst_in[:, :], writes=[self.rCST])
        P.op("dve", lambda e: e.tensor_copy(out=self.IDB[:], in_=self.IDF), reads=[self.rCST], writes=[self.rIDB])
        P.dma(self.CONDT[:], self.cT_in[:, :], writes=[self.rCONDT])
        P.op("act", lambda e: e.activation(out=self.CONDT[:], in_=self.CONDT[:], func=AF.Silu), reads=[self.rCONDT], writes=[self.rCONDT])
        xin = self.x_in.rearrange("(t p) d -> p t d", p=128)
        for q in range(4):
            P.dma(self.X[:, q * 4:(q + 1) * 4, :], xin[:, q * 4:(q + 1) * 4, :], writes=[self.rX])

    def mod_phase(self, l):
        P = self.P
        _w = [self.view("MG", i * 16384, [128, KC, 512], F32, f"wst{i}") for i in range(2)]
        WST = [a for a, _ in _w]
        rW = [r_ for _, r_ in _w]
        PC, rPC = self.PB[0], self.rPB[0]
        PR, rPR = self.PB[1], self.rPB[1]
        wsrc = self.w_mod[l].rearrange("(kc p) n -> p kc n", p=128)
        bt, rbt = self.view("EX", 0, [128, 48], F32, "bt")
        P.dma(bt, self.bmodT[l], writes=[rbt])
        brow, rbrow = self.view("EX", 256, [1, 2 * D], F32, "brow")
        self.GTROW, self.rGTROW = self.view("EX", 256 + 8192, [1, 2 * D], F32, "gtrow")
        P.dma(brow[0:1, 0:D], self.bmod_row[l][:, 2 * D:3 * D], writes=[rbrow])
        P.dma(brow[0:1, D:2 * D], self.bmod_row[l][:, 5 * D:6 * D], writes=[rbrow])
        P.dma(self.NCOL[:], self.ncol[l], writes=[self.rNCOL])
        for cb in range(12):
            s = cb % 2
            vec = cb // 2
            P.dma(WST[s][:], wsrc[:, :, cb * 512:(cb + 1) * 512], writes=[rW[s]])
            if vec in (2, 5):
                col0 = (0 if vec == 2 else D) + (cb % 2) * 512

                def f(e, s=s):
                    for kc in range(KC):
                        ins = e.matmul(PR[0:1, :], lhsT=self.CONDT[:, kc:kc + 1], rhs=WST[s][:, kc, :], start=(kc == 0), stop=(kc == KC - 1))
                    return ins
                P.op("pe", f, reads=[rW[s], self.rCONDT], writes=[rPR])
                P.op("dve", lambda e, col0=col0: e.tensor_tensor(out=self.GTROW[0:1, col0:col0 + 512], in0=PR[0:1, :], in1=brow[0:1, col0:col0 + 512], op=ALU.add),
                     reads=[rPR, rbrow], writes=[self.rGTROW])
            else:
                def f(e, s=s, cb=cb):
                    for j in range(4):
                        for kc in range(KC):
                            ins = e.matmul(PC[:, cb * 4 + j:cb * 4 + j + 1], lhsT=WST[s][:, kc, j * 128:(j + 1) * 128], rhs=self.CONDT[:, kc:kc + 1],
                                           start=(kc == 0), stop=(kc == KC - 1))
                    return ins
                P.op("pe", f, reads=[rW[s], self.rCONDT], writes=[rPC])
        for (a, b) in ((0, 16), (24, 40)):
            P.op("dve", lambda e, a=a, b=b: e.tensor_tensor(out=self.MODT[:, a:b], in0=PC[:, a:b], in1=bt[:, a:b], op=ALU.add),
                 reads=[rPC, rbt], writes=[self.rMODT])
        for i, c0 in ((0, 8), (1, 32)):
            P.op("dve", lambda e, i=i, c0=c0: e.scalar_tensor_tensor(out=self.GEFF[:, i * 8:(i + 1) * 8], in0=self.MODT[:, c0:c0 + 8], scalar=1.0,
                                                                   in1=self.NCOL[:, i * 8:(i + 1) * 8], op0=ALU.add, op1=ALU.mult),
                 reads=[self.rMODT, self.rNCOL], writes=[self.rGEFF])
        for i in range(2):
            for hf in range(2):
                pb, rpb = self.PB[2 + hf], self.rPB[2 + hf]
                P.op("pe", lambda e, i=i, hf=hf, pb=pb: e.matmul(pb[:, :], lhsT=self.ONES[0:1, :], rhs=self.GTROW[0:1, i * D + hf * 512:i * D + (hf + 1) * 512], start=True, stop=True),
                     reads=[self.rGTROW, self.rCST], writes=[rpb])
                P.op("act", lambda e, i=i, hf=hf, pb=pb: e.activation(out=self.GTB[:, i, hf * 512:(hf + 1) * 512], in_=pb[:, :], func=AF.Copy),
                     reads=[rpb], writes=[self.rGTB])
        self.release("MG")

    def norm_phase(self, which, router=None):
        P = self.P
        gcol = self.GEFF[:, which * 8:(which + 1) * 8]
        shc = self.MODT[:, (0 if which == 0 else 24):(8 if which == 0 else 32)]
        SS, RS = self.SS[:, 0:16], self.SS[:, 16:32]
        for t in range(NT):
            P.op("act", lambda e, t=t: e.activation(out=self.JUNK[:], in_=self.X[:, t, :], func=AF.Square, accum_out=SS[:, t:t + 1]),
                 reads=[self.rX], writes=[self.rJUNK, self.rSS])
        import os
        NCUT = int(os.environ.get("NCUT", "99"))
        if NCUT <= 1:
            return
        P.op("act", lambda e: e.activation(out=RS, in_=SS, func=AF.Ln, scale=1.0 / D, bias=1e-6), reads=[self.rSS], writes=[self.rSS])
        P.op("act", lambda e: e.activation(out=RS, in_=RS, func=AF.Exp, scale=-0.5), reads=[self.rSS], writes=[self.rSS])
        if NCUT <= 2:
            return
        _x = [self.view("EX", 1024 + i * 4096, [128, D], F32, f"xn{i}") for i in range(4)]
        XNs = [a for a, _ in _x]
        rXN = [r_ for _, r_ in _x]
        if router is not None:
            H2F, rH2F = self.view("EX", 1024 + 16384, [128, KC, 512], F32, "h2f")
        for tg in range(4):
            for tt in range(4):
                t = tg * 4 + tt
                P.op("act", lambda e, t=t, tt=tt: e.activation(out=XNs[tt], in_=self.X[:, t, :], func=AF.Copy, scale=RS[:, t:t + 1]),
                     reads=[self.rX, self.rSS], writes=[rXN[tt]])
            if NCUT <= 3:
                continue
            for kc in range(KC):
                pb, rpb = self.PB[kc % 2], self.rPB[kc % 2]

                def f(e, kc=kc, pb=pb):
                    for tt in range(4):
                        ins = e.matmul(pb[:, tt * 128:(tt + 1) * 128], lhsT=XNs[tt][:, kc * 128:(kc + 1) * 128], rhs=self.IDF, start=True, stop=True)
                    return ins
                P.op("pe", f, reads=rXN + [self.rCST], writes=[rpb])
                if NCUT <= 4:
                    continue
                P.op("dve", lambda e, kc=kc, tg=tg, pb=pb: e.tensor_scalar(out=self.HT[:, kc, tg * 512:(tg + 1) * 512], in0=pb[:, :], scalar1=gcol[:, kc:kc + 1],
                                                                          scalar2=shc[:, kc:kc + 1], op0=ALU.mult, op1=ALU.add),
                     reads=[rpb, self.rGEFF, self.rMODT], writes=[self.rHT])
                if NCUT <= 5:
                    continue
                if router is not None:
                    P.op("dve", lambda e, kc=kc, pb=pb: e.tensor_scalar(out=H2F[:, kc, :], in0=pb[:, :], scalar1=gcol[:, kc:kc + 1],
                                                                        scalar2=shc[:, kc:kc + 1], op0=ALU.mult, op1=ALU.add),
                         reads=[rpb, self.rGEFF, self.rMODT], writes=[rH2F])
            if router is not None and NCUT > 6:
                router(tg, H2F, rH2F)

    def view(self, regname, off, shape, dt, name="v"):
        reg = {"EX": self.EX, "MG": self.MG, "ARENA": self.ARENA}[regname]
        n = 1
        for s in shape[1:]:
            n *= s
        size = n * (4 if dt == F32 else 2)
        if not hasattr(self, "_live"):
            self._live = {"EX": [], "MG": [], "ARENA": []}
        res = self.R(name)
        keep = []
        evs = []
        for (o, sz, r_) in self._live[regname]:
            if o < off + size and off < o + sz:
                evs.extend(r_.r)
                if r_.w is not None:
                    evs.append(r_.w)
            else:
                keep.append((o, sz, r_))
        base = {"EX": self.rEX, "MG": self.rMG, "ARENA": self.rX}[regname]
        evs.extend(base.r)
        if base.w is not None:
            evs.append(base.w)
        best = {}
        for e2 in evs:
            if e2[0] not in best or best[e2[0]][1] < e2[1]:
                best[e2[0]] = e2
        res.r = list(best.values())
        keep.append((off, size, res))
        self._live[regname] = keep
        return self.carve(reg, off, shape, dt), res

    def release(self, regname):
        base = {"EX": self.rEX, "MG": self.rMG, "ARENA": self.rX}[regname]
        evs = list(base.r)
        for (o, sz, r_) in getattr(self, "_live", {}).get(regname, []):
            evs.extend(r_.r)
            if r_.w is not None:
                evs.append(r_.w)
        best = {}
        for e2 in evs:
            if e2[0] not in best or best[e2[0]][1] < e2[1]:
                best[e2[0]] = e2
        base.r = list(best.values())
        if hasattr(self, "_live"):
            self._live[regname] = []

    def moe_phase(self, l):
        P = self.P
        K1 = 1024
        GATES, rGATES = self.view("EX", 33 * K1, [128, NT, E], F32, "gates")
        RWf, rRWf = self.view("EX", 35 * K1, [128, KC, E], F32, "rwf")
        RB, rRB = self.view("EX", 36 * K1, [128, E], F32, "rb")
        SM, rSM = self.view("EX", 36 * K1 + 128, [128, 96], F32, "sm")
        M8, rM8 = self.view("EX", 36 * K1 + 512, [128, 16], F32, "m8")
        B1C, rB1C = self.view("EX", 37 * K1, [128, E, 2, 8], F32, "b1c")
        B1L7, rB1L7 = self.view("EX", 39 * K1, [128, E, 8], F32, "b1l7")
        GTt, rGTt = self.view("MG", 0, [32, T], F32, "gtt")
        B2, rB2 = self.view("MG", 8 * K1, [32, D], F32, "b2")
        P.dma(RWf, self.router_w[l].rearrange("(kc p) e -> p kc e", p=128), writes=[rRWf])
        rb_src = self.router_b[l:l + 1, :].partition_broadcast(128)
        P.dma(RB, rb_src, writes=[rRB])
        P.dma(B1C, self.b1T[l], writes=[rB1C])
        P.dma(B2, self.moe_b2[l], writes=[rB2])
        P.op("dve", lambda e: e.tensor_scalar(out=B1L7, in0=B1C[:, :, 1, :], scalar1=7.0, scalar2=None, op0=ALU.add), reads=[rB1C], writes=[rB1L7])

        def router(tg, H2F, rH2F):
            for tt in range(4):
                t = tg * 4 + tt
                pb, rpb = self.PB[2], self.rPB[2]

                def f(e, tt=tt):
                    for kc in range(KC):
                        ins = e.matmul(pb[:, 0:E], lhsT=H2F[:, kc, tt * 128:(tt + 1) * 128], rhs=RWf[:, kc, :], start=(kc == 0), stop=(kc == KC - 1))
                    return ins
                P.op("pe", f, reads=[rH2F, rRWf], writes=[rpb])
                LG, EXPV, MASK = SM[:, 0:32], SM[:, 32:64], SM[:, 64:96]
                P.op("dve", lambda e: e.tensor_tensor(out=LG, in0=pb[:, 0:E], in1=RB, op=ALU.add), reads=[rpb, rRB], writes=[rSM])
                P.op("dve", lambda e: e.max(out=M8[:, 0:8], in_=LG), reads=[rSM], writes=[rM8])
                P.op("dve", lambda e: e.tensor_scalar(out=M8[:, 8:9], in0=M8[:, 0:1], scalar1=-1.0, scalar2=None, op0=ALU.mult), reads=[rM8], writes=[rM8])
                P.op("act", lambda e: e.activation(out=EXPV, in_=LG, func=AF.Exp, bias=M8[:, 8:9]), reads=[rSM, rM8], writes=[rSM])
                P.op("dve", lambda e: e.tensor_scalar(out=MASK, in0=LG, scalar1=M8[:, 3:4], scalar2=None, op0=ALU.is_ge), reads=[rSM, rM8], writes=[rSM])
                P.op("dve", lambda e: e.tensor_tensor(out=EXPV, in0=EXPV, in1=MASK, op=ALU.mult), reads=[rSM], writes=[rSM])
                P.op("dve", lambda e: e.tensor_reduce(out=M8[:, 9:10], in_=EXPV, axis=AX.X, op=ALU.add), reads=[rSM], writes=[rM8])
                P.op("dve", lambda e: e.reciprocal(out=M8[:, 10:11], in_=M8[:, 9:10]), reads=[rM8], writes=[rM8])
                P.op("dve", lambda e, t=t: e.tensor_scalar(out=GATES[:, t, :], in0=EXPV, scalar1=M8[:, 10:11], scalar2=None, op0=ALU.mult), reads=[rSM, rM8], writes=[rGATES])
                pb3, rpb3 = self.PB[3], self.rPB[3]
                P.op("pe", lambda e, t=t: e.matmul(pb3[0:E, 0:128], lhsT=GATES[:, t, :], rhs=self.IDF, start=True, stop=True), reads=[rGATES, self.rCST], writes=[rpb3])
                P.op("act", lambda e, t=t: e.activation(out=GTt[:, t * 128:(t + 1) * 128], in_=pb3[0:E, 0:128], func=AF.Copy), reads=[rpb3], writes=[rGTt])

        self.norm_phase(1, router=router)
        if getattr(self, "moe_stop", 0) == 1:
            self.dump("dbg_gates", GATES, rGATES, [128, NT, E])
            return

        GT2 = self.GTB[:, 1, :]
        TMPB, rTMPB = self.view("EX", 1 * K1, [128, 512], F32, "tmpb")
        for t in range(NT):
            for nb in range(2):
                pb, rpb = self.PB[4 + nb], self.rPB[4 + nb]
                P.op("pe", lambda e, t=t, nb=nb, pb=pb: e.matmul(pb[:, :], lhsT=GTt[:, t * 128:(t + 1) * 128], rhs=B2[:, nb * 512:(nb + 1) * 512], start=True, stop=True),
                     reads=[rGTt, rB2], writes=[rpb])
                P.op("dve", lambda e, nb=nb, pb=pb: e.tensor_tensor(out=TMPB, in0=pb[:, :], in1=GT2[:, nb * 512:(nb + 1) * 512], op=ALU.mult),
                     reads=[rpb, self.rGTB], writes=[rTMPB])
                P.op("pool", lambda e, t=t, nb=nb: e.tensor_tensor(out=self.X[:, t, nb * 512:(nb + 1) * 512], in0=self.X[:, t, nb * 512:(nb + 1) * 512], in1=TMPB, op=ALU.add),
                     reads=[rTMPB, self.rX], writes=[self.rX])

        if getattr(self, "moe_stop", 0) == 2:
            return
        GT2b, rGT2b = self.view("EX", 3 * K1, [128, D], BF16, "gt2b")
        P.op("act", lambda e: e.activation(out=GT2b, in_=GT2, func=AF.Copy), reads=[self.rGTB], writes=[rGT2b])
        W1 = []
        for i in range(2):
            W1.append(self.view("MG", i * 16 * K1, [128, KC, 2, 512], BF16, f"w1_{i}"))
        W2 = [(self.WB[i][:].rearrange("p a b -> p (a b)").rearrange("p (f d) -> p f d", f=4), self.rWB[i]) for i in range(2)]
        ACTT = [self.view("EX", (5 + 4 * i) * K1, [128, 4, 512], BF16, f"actt{i}") for i in range(3)]
        TMP = [[self.view("EX", (17 + 8 * s + 2 * j) * K1, [128, 512], F32, f"tmp{s}{j}") for j in range(4)] for s in range(2)]
        w1src = self.moe_w1[l].rearrange("e (kc p) g f -> e p kc g f", p=128)
        w2src = self.moe_w2[l].rearrange("e (fc p) d -> e p fc d", p=128)
        blocks = [(e_, hf, tb) for e_ in range(E) for hf in range(2) for tb in range(4)]

        def load(e_, hf):
            s = (e_ * 2 + hf) % 2
            w1, rw1 = W1[s]
            w2, rw2 = W2[s]
            for kh in range(2):
                for gl in range(2):
                    P.dma(w1[:, kh * 4:(kh + 1) * 4, gl, :], w1src[e_, :, kh * 4:(kh + 1) * 4, gl, hf * 512:(hf + 1) * 512], writes=[rw1], eng="pool")
            P.dma(w2, w2src[e_, :, hf * 4:(hf + 1) * 4, :], writes=[rw2], eng="pool")
            for fc in range(4):
                P.op("pool" if fc % 2 else "dve", lambda e, fc=fc, w2=w2: e.tensor_tensor(out=w2[:, fc, :], in0=w2[:, fc, :], in1=GT2b, op=ALU.mult),
                     reads=[rw2, rGT2b], writes=[rw2])

        def phase1(bi):
            e_, hf, tb = blocks[bi]
            s = (e_ * 2 + hf) % 2
            w1, rw1 = W1[s]
            at, rat = ACTT[bi % 3]
            for fc in range(4):
                q = (bi * 4 + fc) % 2
                pg, rpg = self.PB[2 * q], self.rPB[2 * q]
                pl, rpl = self.PB[2 * q + 1], self.rPB[2 * q + 1]
                (glu, rglu), (sig, rsig), (t1, rt1), (v, rv) = TMP[q]
                fcg = hf * 4 + fc

                def f(e, fc=fc, pg=pg, pl=pl, w1=w1, tb=tb):
                    for gl, pp in ((0, pg), (1, pl)):
                        for kc in range(KC):
                            ins = e.matmul(pp[:, :], lhsT=w1[:, kc, gl, fc * 128:(fc + 1) * 128], rhs=self.HT[:, kc, tb * 512:(tb + 1) * 512], start=(kc == 0), stop=(kc == KC - 1))
                    return ins
                P.op("pe", f, reads=[rw1, self.rHT], writes=[rpg, rpl])
                P.op("dve", lambda e, pg=pg, glu=glu, e_=e_, fcg=fcg: e.tensor_scalar(out=glu, in0=pg[:, :], scalar1=B1C[:, e_, 0, fcg:fcg + 1], scalar2=7.0, op0=ALU.add, op1=ALU.min),
                     reads=[rpg, rB1C], writes=[rglu])
                P.op("act", lambda e, pl=pl, t1=t1, e_=e_, fcg=fcg: e.activation(out=t1, in_=pl[:, :], func=AF.Relu, bias=B1L7[:, e_, fcg:fcg + 1]),
                     reads=[rpl, rB1L7], writes=[rt1])
                P.op("act", lambda e, glu=glu, sig=sig: e.activation(out=sig, in_=glu, func=AF.Sigmoid, scale=1.702), reads=[rglu], writes=[rsig])
                P.op("dve", lambda e, t1=t1: e.tensor_scalar(out=t1, in0=t1, scalar1=14.0, scalar2=-6.0, op0=ALU.min, op1=ALU.add), reads=[rt1], writes=[rt1])
                P.op("pool", lambda e, glu=glu, sig=sig, v=v: e.tensor_tensor(out=v, in0=glu, in1=sig, op=ALU.mult), reads=[rglu, rsig], writes=[rv])
                P.op("dve", lambda e, v=v, t1=t1, at=at, fc=fc: e.tensor_tensor(out=at[:, fc, :], in0=v, in1=t1, op=ALU.mult), reads=[rv, rt1], writes=[rat])

        def phase2(bi):
            e_, hf, tb = blocks[bi]
            s = (e_ * 2 + hf) % 2
            w2, rw2 = W2[s]
            at, rat = ACTT[bi % 3]
            for tt in range(4):
                t = tb * 4 + tt
                for nb in range(2):
                    po, rpo = self.PB[4 + nb], self.rPB[4 + nb]

                    def f(e, tt=tt, nb=nb, po=po, at=at, w2=w2):
                        for fc in range(4):
                            ins = e.matmul(po[:, :], lhsT=at[:, fc, tt * 128:(tt + 1) * 128], rhs=w2[:, fc, nb * 512:(nb + 1) * 512], start=(fc == 0), stop=(fc == 3))
                        return ins
                    P.op("pe", f, reads=[rat, rw2], writes=[rpo])
                    P.op("dve", lambda e, t=t, nb=nb, po=po, e_=e_: e.scalar_tensor_tensor(out=self.X[:, t, nb * 512:(nb + 1) * 512], in0=po[:, :], scalar=GATES[:, t, e_:e_ + 1],
                                                                                       in1=self.X[:, t, nb * 512:(nb + 1) * 512], op0=ALU.mult, op1=ALU.add),
                         reads=[rpo, rGATES, self.rX], writes=[self.rX])

        nb_ = len(blocks)
        load(0, 0)
        for bi in range(nb_ + 1):
            if bi < nb_:
                phase1(bi)
            if bi >= 1:
                phase2(bi - 1)
            if bi < nb_:
                e_, hf, tb = blocks[bi]
                if tb == 0:
                    nxt = e_ * 2 + hf + 1
                    if nxt < 2 * E:
                        load(nxt // 2, nxt % 2)
        self.release("EX")
        self.release("MG")

    def final_phase(self):
        P = self.P
        GF, rGF = self.view("EX", 0, [128, D], F32, "gf")
        P.dma(GF, self.fng[0:1, :].partition_broadcast(128), writes=[rGF])
        SS, RS = self.SS[:, 0:16], self.SS[:, 16:32]
        for t in range(NT):
            P.op("act", lambda e, t=t: e.activation(out=self.JUNK[:], in_=self.X[:, t, :], func=AF.Square, accum_out=SS[:, t:t + 1]),
                 reads=[self.rX], writes=[self.rJUNK, self.rSS])
        P.op("act", lambda e: e.activation(out=RS, in_=SS, func=AF.Ln, scale=1.0 / D, bias=1e-6), reads=[self.rSS], writes=[self.rSS])
        P.op("act", lambda e: e.activation(out=RS, in_=RS, func=AF.Exp, scale=-0.5), reads=[self.rSS], writes=[self.rSS])
        OB = [self.view("EX", (4 + 4 * i) * 1024, [128, D], F32, f"ob{i}") for i in range(2)]
        yv = self.y_out.rearrange("(t p) d -> p t d", p=128)
        rY = self.R("y")
        for t in range(NT):
            ob, rob = OB[t % 2]
            P.op("dve", lambda e, t=t, ob=ob: e.scalar_tensor_tensor(out=ob, in0=self.X[:, t, :], scalar=RS[:, t:t + 1], in1=GF, op0=ALU.mult, op1=ALU.mult),
                 reads=[self.rX, self.rSS, rGF], writes=[rob])
            P.dma(yv[:, t, :], ob, reads=[rob], writes=[rY])
        self.outs.append(rY)

    def build(self):
        self.declare()
        self.alloc()
        self.prologue()
        if self.do_mixer:
            self.eb_build()
        for l in self.layers:
            self.mod_phase(l)
            if self.do_mixer:
                self.mixer_phase(l)
            if self.do_moe:
                self.moe_phase(l)
        self.final_phase()
        self.P.final_wait("sp", self.outs)
        self.P.emit(self.st)
        self.st.close()
        return self.nc


def prep_inputs(inp, b):
    f = lambda a: np.ascontiguousarray(a, dtype=np.float32)
    cst, EA, EC = _host_consts()
    m = {}
    m["x"] = f(inp["x"][b])
    m["cT"] = f(inp["c"][b].reshape(8, 128).T)
    m["w_mod"] = f(inp["w_mod"])
    m["bmodT"] = f(inp["b_mod"].reshape(L, 48, 128).transpose(0, 2, 1))
    m["bmod_row"] = f(inp["b_mod"].reshape(L, 1, 6 * D))
    m["ncol"] = f(np.concatenate([inp["norm1_g"].reshape(L, 8, 128).transpose(0, 2, 1), inp["norm2_g"].reshape(L, 8, 128).transpose(0, 2, 1)], axis=2))
    m["w_in"] = f(inp["w_in"])
    mu = inp["rwkv_mu"]
    mu_rkv = mu[:, :, :2304].reshape(L, 2, 36, 64)
    muT = mu_rkv.transpose(0, 3, 2, 1)
    m["muT"] = f(np.concatenate([muT, muT], axis=1))
    m["muL"] = f(mu[:, :, 2304:].reshape(L, 2, 3, 128).transpose(0, 3, 2, 1))
    rw = np.stack([inp["rwkv_w0"], inp["rwkv_a0"], inp["rwkv_k_k"], inp["rwkv_k_a"]], axis=-1)
    m["rwcol"] = f(rw.reshape(L, 2, 12, 64, 4).transpose(0, 1, 3, 2, 4).reshape(L, 128, 12, 4))
    rk = inp["rwkv_r_k"].transpose(0, 2, 1)
    m["rkcol"] = f(np.concatenate([rk, rk], axis=1))
    m["w_up"] = f(inp["rwkv_w_up"].reshape(L, 128, 768))
    m["a_up"] = f(inp["rwkv_a_up"].reshape(L, 128, 768))
    m["g_up"] = f(inp["rwkv_g_up"])
    m["ln_g"] = f(inp["rwkv_ln_g"])
    m["ln_b"] = f(inp["rwkv_ln_b"])
    m["dlam"] = f(inp["diff_lambda"].reshape(L, 256))
    m["subln"] = f(inp["diff_subln_g"].reshape(L, 128, 1))
    m["rel_bias"] = f(inp["rel_bias"])
    m["w_br_a"] = f(inp["w_branch_a"])
    m["w_br_b"] = f(inp["w_branch_b"])
    m["w_br_c"] = f(inp["w_branch_c"])
    m["w_out"] = f(inp["w_out"])
    m["router_w"] = f(inp["router_w"])
    m["router_b"] = f(inp["router_b"])
    w1 = inp["moe_w1"]
    m["moe_w1"] = f(np.stack([w1[..., 0::2], w1[..., 1::2]], axis=3))
    b1 = inp["moe_b1"].reshape(L, E, 8, 128, 2)
    m["b1T"] = f(b1.transpose(0, 3, 1, 4, 2))
    m["moe_w2"] = f(inp["moe_w2"])
    m["moe_b2"] = f(inp["moe_b2"])
    m["fng"] = f(inp["final_norm_g"].reshape(1, D))
    m["cst"], m["EA"], m["EC"] = cst, EA, EC
    return m


_SHARED = ("w_mod", "bmodT", "bmod_row", "ncol", "w_in", "muT", "muL", "rwcol", "rkcol", "w_up", "a_up", "g_up", "ln_g", "ln_b", "dlam",
           "subln", "rel_bias", "w_br_a", "w_br_b", "w_br_c", "w_out", "router_w", "router_b", "moe_w1", "b1T", "moe_w2", "moe_b2",
           "fng", "cst", "EA", "EC")


def kernel(**inp):
    nb = inp["x"].shape[0]
    nc = Builder().build()
    m0 = prep_inputs(inp, 0)
    in_maps = [m0]
    for b in range(1, nb):
        mb = dict(m0)
        mb["x"] = np.ascontiguousarray(inp["x"][b], dtype=np.float32)
        mb["cT"] = np.ascontiguousarray(inp["c"][b].reshape(8, 128).T, dtype=np.float32)
        in_maps.append(mb)
    res = run_bass_kernel_spmd(nc, in_maps, core_ids=list(range(nb)))
    return np.stack([r["y"] for r in res.results], axis=0).astype(np.float32)


def _rev(ap2, start, n):
    if start == 0:
        return ap2[:, n - 1::-1]
    return ap2[:, start + n - 1:start - 1:-1]


def _mixer_phase(self, l):
    P = self.P
    self.norm_phase(0)
    xs = self.x_spill.rearrange("(t p) d -> p t d", p=128)
    rSP = self.R("xspill")
    for q in range(4):
        P.dma(xs[:, q * 4:(q + 1) * 4, :], self.X[:, q * 4:(q + 1) * 4, :], reads=[self.rX], writes=[rSP])
    self.mg_first = True
    stages = getattr(self, "mix_stages", "bac")
    if "b" in stages:
        self.rwkv_phase(l)
    if "a" in stages:
        self.mixa_phase(l)
    if "c" in stages:
        self.mixc_phase(l)
    self.release("ARENA")
    for q in range(4):
        P.dma(self.X[:, q * 4:(q + 1) * 4, :], xs[:, q * 4:(q + 1) * 4, :], reads=[rSP], writes=[self.rX])
    self.wout_phase(l)
    self.release("EX")


def _load_w(self, src_ap, ncols, col_off=0, slot=None):
    if slot is None:
        slot = self.wbi
        self.wbi = (self.wbi + 1) % 2
    wb, rwb = self.WB[slot], self.rWB[slot]
    self.P.dma(wb[:, :, col_off:col_off + ncols], src_ap.rearrange("(kc p) n -> p kc n", p=128), writes=[rwb], eng="pool")
    return wb, rwb, slot


def _merge(self, l, branch, oT_fn, nk):
    P = self.P
    MGv = self.carve(self.MG, 0, [128, KC, T], BF16)
    SIG = [self.view("EX", (32 + i) * 1024, [128, 512], BF16, f"sig{i}") for i in range(2)]
    TMPM = [self.view("EX", (34 + i) * 1024, [128, 512], BF16, f"tmpm{i}") for i in range(2)]
    first = self.mg_first
    self.mg_first = False
    it = 0
    for dcb in range(2):
        gcol = G_OFF + branch * D + dcb * 512
        wg, rwg, _ = self.load_w(self.w_in[l][:, gcol:gcol + 512], 512)
        for dci in range(4):
            dc = dcb * 4 + dci
            for tb in range(4):
                pg, rpg = self.PB[(it % 2) * 2], self.rPB[(it % 2) * 2]
                pbr, rpbr = self.PB[(it % 2) * 2 + 1], self.rPB[(it % 2) * 2 + 1]
                sig, rsig = SIG[it % 2]
                tmp, rtmp = TMPM[it % 2]
                it += 1

                def fg(e, dci=dci, tb=tb, pg=pg, wg=wg):
                    for kc in range(KC):
                        ins = e.matmul(pg[:, :], lhsT=wg[:, kc, dci * 128:(dci + 1) * 128], rhs=self.HT[:, kc, tb * 512:(tb + 1) * 512], start=(kc == 0), stop=(kc == KC - 1))
                    return ins
                P.op("pe", fg, reads=[rwg, self.rHT], writes=[rpg])
                ress = []
                parts = [oT_fn(j) for j in range(nk)]
                for (_, _, rr) in parts:
                    ress.extend(rr)

                def fb(e, dc=dc, tb=tb, pbr=pbr, parts=parts):
                    for j, (wrow, oT, _) in enumerate(parts):
                        ins = e.matmul(pbr[:, :], lhsT=wrow[:, dc * 128:(dc + 1) * 128], rhs=oT[:, tb * 512:(tb + 1) * 512], start=(j == 0), stop=(j == len(parts) - 1))
                    return ins
                P.op("pe", fb, reads=ress, writes=[rpbr])
                P.op("act", lambda e, pg=pg, sig=sig: e.activation(out=sig, in_=pg[:, :], func=AF.Sigmoid), reads=[rpg], writes=[rsig])
                if first:
                    P.op("dve", lambda e, dc=dc, tb=tb, pbr=pbr, sig=sig: e.tensor_tensor(out=MGv[:, dc, tb * 512:(tb + 1) * 512], in0=pbr[:, :], in1=sig, op=ALU.mult),
                         reads=[rpbr, rsig], writes=[self.rMG])
                else:
                    P.op("dve", lambda e, pbr=pbr, sig=sig, tmp=tmp: e.tensor_tensor(out=tmp, in0=pbr[:, :], in1=sig, op=ALU.mult), reads=[rpbr, rsig], writes=[rtmp])
                    P.op("pool", lambda e, dc=dc, tb=tb, tmp=tmp: e.tensor_tensor(out=MGv[:, dc, tb * 512:(tb + 1) * 512], in0=MGv[:, dc, tb * 512:(tb + 1) * 512], in1=tmp, op=ALU.add),
                         reads=[rtmp, self.rMG], writes=[self.rMG])


def _wout_phase(self, l):
    P = self.P
    MGv = self.carve(self.MG, 0, [128, KC, T], BF16)
    GT1b, rGT1b = self.view("EX", 0, [128, D], BF16, "gt1b")
    P.op("act", lambda e: e.activation(out=GT1b, in_=self.GTB[:, 0, :], func=AF.Copy), reads=[self.rGTB], writes=[rGT1b])
    ws = []
    for nb in range(2):
        wb, rwb, s = self.load_w(self.w_out[l][:, nb * 512:(nb + 1) * 512], 512, slot=nb)
        for kc in range(KC):
            P.op("dve" if kc % 2 else "pool", lambda e, wb=wb, kc=kc, nb=nb: e.tensor_tensor(out=wb[:, kc, :], in0=wb[:, kc, :], in1=GT1b[:, nb * 512:(nb + 1) * 512], op=ALU.mult),
                 reads=[rwb, rGT1b], writes=[rwb])
        ws.append((wb, rwb))
    for t in range(NT):
        for nb in range(2):
            wb, rwb = ws[nb]
            po, rpo = self.PB[(t * 2 + nb) % 4], self.rPB[(t * 2 + nb) % 4]

            def f(e, t=t, wb=wb, po=po):
                for kc in range(KC):
                    ins = e.matmul(po[:, :], lhsT=MGv[:, kc, t * 128:(t + 1) * 128], rhs=wb[:, kc, :], start=(kc == 0), stop=(kc == KC - 1))
                return ins
            P.op("pe", f, reads=[self.rMG, rwb], writes=[rpo])
            P.op("dve", lambda e, t=t, nb=nb, po=po: e.tensor_tensor(out=self.X[:, t, nb * 512:(nb + 1) * 512], in0=po[:, :], in1=self.X[:, t, nb * 512:(nb + 1) * 512], op=ALU.add),
                 reads=[rpo, self.rX], writes=[self.rX])


def _eb_build(self):
    P = self.P
    RBA, rRBA = self.view("EX", 0, [33, 18], F32, "rba")
    EAs, rEAs = self.view("EX", 1024, [33, 1536], F32, "eas")
    ECs, rECs = self.view("EX", 1024 + 6144, [33, 1536], F32, "ecs")
    ROW, rROW = self.view("EX", 1024 + 12288, [12, 1536], F32, "row")
    P.op("dve", lambda e: e.memset(RBA[32:33, :], -200.0), writes=[rRBA])
    P.dma(RBA[0:32, :], self.rel_bias[:, :], writes=[rRBA])
    P.dma(EAs, self.EA_in[:, :], writes=[rEAs])
    P.dma(ECs, self.EC_in[:, :], writes=[rECs])
    rA, rC = self.R("ebA"), self.R("ebC")
    for g in range(3):
        pb, rpb = self.PB[g % 2], self.rPB[g % 2]
        P.op("pe", lambda e, g=g, pb=pb: e.matmul(pb[0:4, :], lhsT=RBA[:, g * 4:(g + 1) * 4], rhs=EAs[:, g * 512:(g + 1) * 512], start=True, stop=True), reads=[rRBA, rEAs], writes=[rpb])
        P.op("act", lambda e, g=g, pb=pb: e.activation(out=ROW[0:4, g * 512:(g + 1) * 512], in_=pb[0:4, :], func=AF.Exp), reads=[rpb], writes=[rROW])
        P.dma(self.ebA[g * 4:(g + 1) * 4, :], ROW[0:4, g * 512:(g + 1) * 512], reads=[rROW], writes=[rA])
    ROWC, rROWC = self.view("EX", 1024 + 12288 + 6144, [6, 1536], F32, "rowc")
    for j in range(3):
        pb, rpb = self.PB[2 + j % 2], self.rPB[2 + j % 2]
        P.op("pe", lambda e, j=j, pb=pb: e.matmul(pb[0:6, :], lhsT=RBA[:, 12:18], rhs=ECs[:, j * 512:(j + 1) * 512], start=True, stop=True), reads=[rRBA, rECs], writes=[rpb])
        P.op("act", lambda e, j=j, pb=pb: e.activation(out=ROWC[0:6, j * 512:(j + 1) * 512], in_=pb[0:6, :], func=AF.Exp), reads=[rpb], writes=[rROWC])
    P.dma(self.ebC[:, :], ROWC, reads=[rROWC], writes=[rC])
    self.rEBA, self.rEBC = rA, rC
    self.release("EX")


Builder.mixer_phase = _mixer_phase
Builder.load_w = _load_w
Builder.merge = _merge
Builder.wout_phase = _wout_phase
Builder.eb_build = _eb_build


def _mixa_phase(self, l):
    P = self.P
    K1 = 1024
    GRP = ((0, 1), (1, 4), (2, 16))
    QK, rQK = self.view("ARENA", 0, [128, 2, 3, T], BF16, "qk")
    VA, rVA = self.view("ARENA", 24 * K1, [128, 3, 16, 2, 65], BF16, "va")
    OACC, rOACC = self.view("ARENA", 37 * K1, [65, 2, T], F32, "oacc")
    EBA, rEBAs = self.view("ARENA", 53 * K1, [128, 12, 384], BF16, "eba")
    HST, rHST = self.view("ARENA", 62 * K1, [128, 384], F32, "hst")
    OAT, rOAT = self.view("EX", 0, [64, 4, T], BF16, "oat")
    RDEN, rRDEN = self.view("EX", 16 * K1, [65, T], F32, "rden")
    WBRA, rWBRA = self.view("EX", 24 * K1, [64, 4, D], BF16, "wbra")
    PT = [self.view("EX", 36 * K1 + i * 768, [128, 384], BF16, f"pt{i}") for i in range(3)]
    for idx in range(12):
        P.dma(HST, bass.AP(self.ebA.tensor, idx * 512, [[1, 128], [1, 384]]), reads=[self.rEBA], writes=[rHST])
        for c in range(3):
            P.op("dve", lambda e, idx=idx, c=c: e.tensor_copy(out=EBA[:, idx, c * 128:(c + 1) * 128], in_=_rev(HST, c * 128, 128)), reads=[rHST], writes=[rEBAs])
    P.dma(WBRA, self.w_br_a[l].rearrange("(h p) n -> p h n", p=64), writes=[rWBRA], eng="pool")
    inst = 0
    for hp in range(2):
        for qk in range(2):
            slot = None
            for g in range(3):
                col = A_OFF + qk * 768 + g * 256 + hp * 128
                wb, rwb, slot = self.load_w(self.w_in[l][:, col:col + 128], 128, col_off=g * 128, slot=slot)
            for g in range(3):
                for tb in range(4):
                    pb, rpb = self.PB[inst % 2], self.rPB[inst % 2]
                    inst += 1

                    def f(e, g=g, tb=tb, pb=pb, wb=wb):
                        for kc in range(KC):
                            ins = e.matmul(pb[:, :], lhsT=wb[:, kc, g * 128:(g + 1) * 128], rhs=self.HT[:, kc, tb * 512:(tb + 1) * 512], start=(kc == 0), stop=(kc == KC - 1))
                        return ins
                    P.op("pe", f, reads=[rwb, self.rHT], writes=[rpb])
                    if inst % 2:
                        P.op("act", lambda e, qk=qk, g=g, tb=tb, pb=pb: e.activation(out=QK[:, qk, g, tb * 512:(tb + 1) * 512], in_=pb[:, :], func=AF.Copy), reads=[rpb], writes=[rQK])
                    else:
                        P.op("dve", lambda e, qk=qk, g=g, tb=tb, pb=pb: e.tensor_copy(out=QK[:, qk, g, tb * 512:(tb + 1) * 512], in_=pb[:, :]), reads=[rpb], writes=[rQK])
        slot = None
        for g in range(3):
            col = A_OFF + 1536 + g * 256 + hp * 128
            wbv, rwbv, slot = self.load_w(self.w_in[l][:, col:col + 128], 128, col_off=g * 128, slot=slot)
        import os
        for g in range(3):
            if os.environ.get("NOMEMSET1") and hp == 1:
                continue
            P.op("pool", lambda e, g=g: e.memset(VA[:, g, :, :, 64:65], 1.0), writes=[rVA])
        for g, d in GRP:
            Lg = T // d
            nkt = Lg // 128
            for tq in range(4):
                pb, rpb = self.PB[inst % 2], self.rPB[inst % 2]
                inst += 1

                def f(e, g=g, d=d, tq=tq, pb=pb, nkt=nkt, wbv=wbv):
                    for ti in range(4):
                        tile_i = tq * 4 + ti
                        r_, kt = tile_i // nkt, tile_i % nkt
                        s0 = r_ + d * 128 * kt
                        for kc in range(KC):
                            ins = e.matmul(pb[:, ti * 128:(ti + 1) * 128], lhsT=self.HT[:, kc, s0:s0 + d * 127 + 1:d], rhs=wbv[:, kc, g * 128:(g + 1) * 128],
                                           start=(kc == 0), stop=(kc == KC - 1))
                    return ins
                P.op("pe", f, reads=[rwbv, self.rHT], writes=[rpb])
                P.op("dve", lambda e, g=g, tq=tq, pb=pb: e.tensor_copy(out=VA[:, g, tq * 4:(tq + 1) * 4, :, 0:64], in_=pb[:, :].rearrange("p (a b c) -> p a b c", a=4, b=2)),
                     reads=[rpb], writes=[rVA])
        if "tr" in self.dbg and hp == 1:
            self.dump("dbg_t1", OAT[:, 0:2, :], rOAT, [64, 2, T], BF16)
        for g, d in GRP:
            Lg = T // d
            nkt = Lg // 128
            for r_ in range(d):
                for qt in range(nkt):
                    q0 = r_ + d * 128 * qt
                    kts = [kt for kt in (qt - 1, qt, qt + 1) if 0 <= kt < nkt]
                    c0, c1 = kts[0] - qt + 1, kts[-1] - qt + 2
                    for hh in range(2):
                        hg = 2 * hp + hh
                        ps, rps = self.PB[2 + inst % 2], self.rPB[2 + inst % 2]
                        po, rpo = self.PB[4 + inst % 2], self.rPB[4 + inst % 2]
                        pt, rpt = PT[inst % 3]
                        inst += 1
                        rows = slice(hh * 64, (hh + 1) * 64)

                        def fs(e, g=g, d=d, r_=r_, qt=qt, kts=kts, ps=ps, rows=rows, q0=q0):
                            for kt in kts:
                                c = kt - qt + 1
                                k0 = r_ + d * 128 * kt
                                ins = e.matmul(ps[:, c * 128:(c + 1) * 128], lhsT=QK[rows, 1, g, k0:k0 + d * 127 + 1:d], rhs=QK[rows, 0, g, q0:q0 + d * 127 + 1:d], start=True, stop=True)
                            return ins
                        P.op("pe", fs, reads=[rQK], writes=[rps])
                        P.op("act", lambda e, ps=ps, pt=pt, c0=c0, c1=c1: e.activation(out=pt[:, c0 * 128:c1 * 128], in_=ps[:, c0 * 128:c1 * 128], func=AF.Exp, scale=0.125), reads=[rps], writes=[rpt])
                        P.op("pool", lambda e, pt=pt, c0=c0, c1=c1, g=g, hg=hg: e.tensor_tensor(out=pt[:, c0 * 128:c1 * 128], in0=pt[:, c0 * 128:c1 * 128], in1=EBA[:, g * 4 + hg, c0 * 128:c1 * 128], op=ALU.mult),
                             reads=[rpt, rEBAs], writes=[rpt])

                        def fo(e, g=g, r_=r_, qt=qt, kts=kts, po=po, pt=pt, hh=hh, nkt=nkt):
                            for i, kt in enumerate(kts):
                                c = kt - qt + 1
                                ins = e.matmul(po[0:65, 0:128], lhsT=VA[:, g, r_ * nkt + kt, hh, :], rhs=pt[:, c * 128:(c + 1) * 128], start=(i == 0), stop=(i == len(kts) - 1))
                            return ins
                        P.op("pe", fo, reads=[rVA, rpt], writes=[rpo])
                        dst = OACC[:, hh, q0:q0 + d * 127 + 1:d]
                        if g == 0:
                            P.op("dve", lambda e, dst=dst, po=po: e.tensor_copy(out=dst, in_=po[0:65, 0:128]), reads=[rpo], writes=[rOACC])
                        else:
                            P.op("dve", lambda e, dst=dst, po=po: e.tensor_tensor(out=dst, in0=dst, in1=po[0:65, 0:128], op=ALU.add), reads=[rpo, rOACC], writes=[rOACC])
        if "tr" in self.dbg and hp == 1:
            self.dump("dbg_t2", OAT[:, 0:2, :], rOAT, [64, 2, T], BF16)
        import os
        for hh in range(2):
            if os.environ.get("SKIPN1") and hp == 1:
                continue
            hg = 2 * hp + hh
            P.op("dve", lambda e, hh=hh: e.reciprocal(out=RDEN[64:65, :], in_=OACC[64:65, hh, :]), reads=[rOACC], writes=[rRDEN])
            for tb in range(4):
                pb, rpb = self.PB[inst % 2], self.rPB[inst % 2]
                inst += 1
                P.op("pe", lambda e, tb=tb, pb=pb: e.matmul(pb[0:64, :], lhsT=self.ONES[64:65, 0:64], rhs=RDEN[64:65, tb * 512:(tb + 1) * 512], start=True, stop=True),
                     reads=[rRDEN, self.rCST], writes=[rpb])
                P.op("dve", lambda e, tb=tb, pb=pb, hh=hh, hg=hg: e.tensor_tensor(out=OAT[:, hg, tb * 512:(tb + 1) * 512], in0=OACC[0:64, hh, tb * 512:(tb + 1) * 512], in1=pb[0:64, :], op=ALU.mult),
                     reads=[rpb, rOACC], writes=[rOAT])
        if "tr" in self.dbg and hp == 0:
            self.dump("dbg_t0", OAT[:, 0:2, :], rOAT, [64, 2, T], BF16)
            self.dump("dbg_oacc_t0", OACC, rOACC, [65, 2, T], F32)
            self.dump("dbg_va_t0", VA, rVA, [128, 3, 16, 2, 65], BF16)
            self.dump("dbg_rden_t0", RDEN[64:65, :], rRDEN, [1, T], F32)
        if "oa0" in self.dbg and hp == 0:
            self.dump("dbg_oacc0", OACC, rOACC, [65, 2, T], F32)
            self.dump("dbg_qk0", QK, rQK, [128, 2, 3, T], BF16)
            self.dump("dbg_va0", VA, rVA, [128, 3, 16, 2, 65], BF16)
            self.dump("dbg_oa", OAT, rOAT, [64, 4, T], BF16)
            break
    if "oa" in self.dbg:
        self.dump("dbg_oa", OAT, rOAT, [64, 4, T], BF16)
        self.dump("dbg_qk", QK, rQK, [128, 2, 3, T], BF16)
        self.dump("dbg_va", VA, rVA, [128, 3, 16, 2, 65], BF16)
        self.dump("dbg_oacc", OACC, rOACC, [65, 2, T], F32)
        self.dump("dbg_eba", EBA, rEBAs, [128, 12, 384], BF16)
    self.merge(l, 0, lambda j: (WBRA[:, j, :], OAT[:, j, :], [rWBRA, rOAT]), 4)


Builder.mixa_phase = _mixa_phase


def _mixc_phase(self, l):
    P = self.P
    K1 = 1024
    lambda_init = 0.8 - 0.6 * math.exp(-0.3 * l)
    OCT, rOCT = self.view("EX", 0, [128, 6, T], BF16, "oct")
    SMC, rSMC = self.view("EX", 24 * K1, [128, 512], F32, "smc")
    SC, rSC = self.view("EX", 26 * K1, [128, 64], F32, "sc")
    OF = [self.view("EX", 27 * K1 + i * 512, [128, 128], F32, f"of{i}") for i in range(2)]
    ON = [self.view("EX", 28 * K1 + i * 512, [128, 128], F32, f"on{i}") for i in range(2)]
    PT = [self.view("EX", (29 + i) * K1, [128, 512], BF16, f"ptc{i}") for i in range(3)]
    P.dma(SMC[:, 0:256], self.dlam[l:l + 1, :].partition_broadcast(128), writes=[rSMC])
    P.dma(SC[:, 0:1], self.subln[l], writes=[rSC])
    P.op("dve", lambda e: e.tensor_tensor(out=SMC[:, 256:320], in0=SMC[:, 0:64], in1=SMC[:, 64:128], op=ALU.mult), reads=[rSMC], writes=[rSMC])
    P.op("dve", lambda e: e.tensor_tensor(out=SMC[:, 320:384], in0=SMC[:, 128:192], in1=SMC[:, 192:256], op=ALU.mult), reads=[rSMC], writes=[rSMC])
    P.op("dve", lambda e: e.tensor_reduce(out=SC[:, 1:2], in_=SMC[:, 256:320], axis=AX.X, op=ALU.add), reads=[rSMC], writes=[rSC])
    P.op("dve", lambda e: e.tensor_reduce(out=SC[:, 2:3], in_=SMC[:, 320:384], axis=AX.X, op=ALU.add), reads=[rSMC], writes=[rSC])
    P.op("act", lambda e: e.activation(out=SC[:, 3:5], in_=SC[:, 1:3], func=AF.Exp), reads=[rSC], writes=[rSC])
    P.op("dve", lambda e: e.tensor_tensor(out=SC[:, 5:6], in0=SC[:, 4:5], in1=SC[:, 3:4], op=ALU.subtract), reads=[rSC], writes=[rSC])
    P.op("dve", lambda e: e.tensor_scalar(out=SC[:, 5:6], in0=SC[:, 5:6], scalar1=-lambda_init, scalar2=None, op0=ALU.add), reads=[rSC], writes=[rSC])
    P.op("dve", lambda e: e.tensor_scalar(out=SC[:, 6:7], in0=SC[:, 0:1], scalar1=1.0 - lambda_init, scalar2=None, op0=ALU.mult), reads=[rSC], writes=[rSC])
    NEGLAM, SUBC = SC[:, 5:6], SC[:, 6:7]
    inst = 0
    for ps_ in range(2):
        QC, rQC = self.view("ARENA", 0, [128, 3, T], BF16, "qc")
        KCc, rKC = self.view("ARENA", 12 * K1, [128, 3, T], BF16, "kc")
        VC, rVC = self.view("ARENA", 24 * K1, [128, 16, 3, 129], BF16, "vc")
        GREV, rGREV = self.view("ARENA", 37 * K1, [128, 3, 1408], BF16, "grev")
        HSTC, rHSTC = self.view("ARENA", 46 * K1, [128, 1408], F32, "hstc")
        for hi in range(3):
            h = ps_ * 3 + hi
            P.dma(HSTC, bass.AP(self.ebC.tensor, h * 1536, [[1, 128], [1, 1408]]), reads=[self.rEBC], writes=[rHSTC])
            P.op("dve", lambda e, hi=hi: e.tensor_copy(out=GREV[:, hi, :], in_=_rev(HSTC, 0, 1408)), reads=[rHSTC], writes=[rGREV])
        for qk, (dst, rdst) in enumerate(((QC, rQC), (KCc, rKC))):
            col = C_OFF + qk * 768 + ps_ * 384
            wb, rwb, _ = self.load_w(self.w_in[l][:, col:col + 384], 384)
            for hi in range(3):
                for tb in range(4):
                    pb, rpb = self.PB[inst % 2], self.rPB[inst % 2]
                    inst += 1

                    def f(e, hi=hi, tb=tb, pb=pb, wb=wb):
                        for kc in range(KC):
                            ins = e.matmul(pb[:, :], lhsT=wb[:, kc, hi * 128:(hi + 1) * 128], rhs=self.HT[:, kc, tb * 512:(tb + 1) * 512], start=(kc == 0), stop=(kc == KC - 1))
                        return ins
                    P.op("pe", f, reads=[rwb, self.rHT], writes=[rpb])
                    if inst % 2:
                        P.op("act", lambda e, dst=dst, hi=hi, tb=tb, pb=pb: e.activation(out=dst[:, hi, tb * 512:(tb + 1) * 512], in_=pb[:, :], func=AF.Copy), reads=[rpb], writes=[rdst])
                    else:
                        P.op("dve", lambda e, dst=dst, hi=hi, tb=tb, pb=pb: e.tensor_copy(out=dst[:, hi, tb * 512:(tb + 1) * 512], in_=pb[:, :]), reads=[rpb], writes=[rdst])
        col = C_OFF + 1536 + ps_ * 384
        wbv, rwbv, _ = self.load_w(self.w_in[l][:, col:col + 384], 384)
        P.op("pool", lambda e, VC=VC: e.memset(VC[:, :, :, 128:129], 1.0), writes=[rVC])
        for kt in range(16):
            pb, rpb = self.PB[inst % 2], self.rPB[inst % 2]
            inst += 1

            def f(e, kt=kt, pb=pb, wbv=wbv):
                for kc in range(KC):
                    ins = e.matmul(pb[:, 0:384], lhsT=self.HT[:, kc, kt * 128:(kt + 1) * 128], rhs=wbv[:, kc, 0:384], start=(kc == 0), stop=(kc == KC - 1))
                return ins
            P.op("pe", f, reads=[rwbv, self.rHT], writes=[rpb])
            P.op("dve", lambda e, kt=kt, pb=pb, VC=VC: e.tensor_copy(out=VC[:, kt, :, 0:128], in_=pb[:, 0:384].rearrange("p (a b) -> p a b", a=3)), reads=[rpb], writes=[rVC])
        ACC = [[self.PW[:, j * 256:j * 256 + 129] for j in range(4)],
               [self.PB[4 + j // 2][:, (j % 2) * 256:(j % 2) * 256 + 129] for j in range(4)]]
        rACC = [self.rPW, self.rPB[4], self.rPB[5]]
        for hi in range(3):
            h = ps_ * 3 + hi
            for qb in range(4):
                for kt in range(16):
                    delta = 128 * kt - 512 * qb
                    de = min(max(delta, -256), 640)
                    s0 = 640 - de
                    for c in range(2):
                        ps, rps = self.PB[2 + inst % 2], self.rPB[2 + inst % 2]
                        pt, rpt = PT[inst % 3]
                        inst += 1
                        rows = slice(c * 64, (c + 1) * 64)
                        P.op("pe", lambda e, ps=ps, rows=rows, hi=hi, kt=kt, qb=qb, KCc=KCc, QC=QC: e.matmul(ps[:, :], lhsT=KCc[rows, hi, kt * 128:(kt + 1) * 128], rhs=QC[rows, hi, qb * 512:(qb + 1) * 512], start=True, stop=True),
                             reads=[rQC, rKC], writes=[rps])
                        P.op("act", lambda e, ps=ps, pt=pt: e.activation(out=pt, in_=ps[:, :], func=AF.Exp, scale=0.125), reads=[rps], writes=[rpt])
                        P.op("pool" if inst % 2 else "dve", lambda e, pt=pt, hi=hi, s0=s0, GREV=GREV: e.tensor_tensor(out=pt, in0=pt, in1=GREV[:, hi, s0:s0 + 512], op=ALU.mult), reads=[rpt, rGREV], writes=[rpt])

                        def fo(e, c=c, kt=kt, hi=hi, pt=pt, VC=VC):
                            for j in range(4):
                                ins = e.matmul(ACC[c][j], lhsT=pt[:, j * 128:(j + 1) * 128], rhs=VC[:, kt, hi, :], start=(kt == 0 and j % 2 == 0), stop=(kt == 15 and j % 2 == 1))
                            return ins
                        P.op("pe", fo, reads=[rpt, rVC], writes=[rACC[0]] if c == 0 else [rACC[1], rACC[2]])
                R0, R1 = SC[:, 8:12], SC[:, 12:16]
                P.op("dve", lambda e: e.reciprocal(out=R0, in_=self.PW[:, 128:1024:256]), reads=[rACC[0]], writes=[rSC])
                for j in range(4):
                    P.op("dve", lambda e, j=j: e.reciprocal(out=SC[:, 12 + j:13 + j], in_=ACC[1][j][:, 128:129]), reads=[rACC[1], rACC[2]], writes=[rSC])
                P.op("dve", lambda e: e.tensor_scalar(out=R1, in0=R1, scalar1=NEGLAM, scalar2=None, op0=ALU.mult), reads=[rSC], writes=[rSC])
                for j in range(4):
                    t = qb * 4 + j
                    of, rof = OF[j % 2]
                    on, ron = ON[j % 2]
                    P.op("dve", lambda e, j=j, of=of: e.tensor_scalar(out=of, in0=ACC[0][j][:, 0:128], scalar1=SC[:, 8 + j:9 + j], scalar2=None, op0=ALU.mult), reads=[rACC[0], rSC], writes=[rof])
                    P.op("dve", lambda e, j=j, of=of: e.scalar_tensor_tensor(out=of, in0=ACC[1][j][:, 0:128], scalar=SC[:, 12 + j:13 + j], in1=of, op0=ALU.mult, op1=ALU.add),
                         reads=[rACC[1], rACC[2], rSC, rof], writes=[rof])
                    P.op("act", lambda e, j=j, of=of, on=on: e.activation(out=on, in_=of, func=AF.Square, accum_out=SC[:, 16 + j:17 + j]), reads=[rof], writes=[ron, rSC])
                    P.op("act", lambda e, j=j: e.activation(out=SC[:, 20 + j:21 + j], in_=SC[:, 16 + j:17 + j], func=AF.Ln, scale=1.0 / 128, bias=1e-5), reads=[rSC], writes=[rSC])
                    P.op("act", lambda e, j=j: e.activation(out=SC[:, 20 + j:21 + j], in_=SC[:, 20 + j:21 + j], func=AF.Exp, scale=-0.5), reads=[rSC], writes=[rSC])
                    P.op("act", lambda e, j=j, of=of, on=on: e.activation(out=on, in_=of, func=AF.Copy, scale=SC[:, 20 + j:21 + j]), reads=[rof, rSC], writes=[ron])
                    pbt, rpbt = self.PB[inst % 2], self.rPB[inst % 2]
                    inst += 1
                    P.op("pe", lambda e, on=on, pbt=pbt: e.matmul(pbt[:, 0:128], lhsT=on, rhs=self.IDF, start=True, stop=True), reads=[ron, self.rCST], writes=[rpbt])
                    P.op("dve", lambda e, h=h, t=t, pbt=pbt: e.tensor_scalar(out=OCT[:, h, t * 128:(t + 1) * 128], in0=pbt[:, 0:128], scalar1=SUBC, scalar2=None, op0=ALU.mult),
                         reads=[rpbt, rSC], writes=[rOCT])
    if "oc" in self.dbg:
        self.dump("dbg_oc", OCT, rOCT, [128, 6, T], BF16)
    WBRC, rWBRC = self.view("ARENA", 0, [128, 6, D], BF16, "wbrc")
    P.dma(WBRC, self.w_br_c[l].rearrange("(j p) n -> p j n", p=128), writes=[rWBRC], eng="pool")
    self.merge(l, 2, lambda j: (WBRC[:, j, :], OCT[:, j, :], [rWBRC, rOCT]), 6)


Builder.mixc_phase = _mixc_phase


def _rwkv_phase(self, l):
    P = self.P
    K1 = 1024
    CDEC = DECAY_C
    SL = 8 * K1
    def A(i, shape=(128, T), dt=F32, name="a"):
        return self.view("ARENA", i * SL, list(shape), dt, f"{name}{i}")
    OBT, rOBT = self.view("EX", 0, [128, 6, T], BF16, "obt")
    YH, rYH = self.view("EX", 24 * K1, [128, 16, 64], F32, "yh")
    OBK, rOBK = self.view("EX", 28 * K1, [128, 16, 128], BF16, "obk")
    BON, rBON = self.view("EX", 32 * K1, [128, 16, 12], F32, "bon")
    GUP, rGUP = self.view("EX", 33 * K1, [128, 768], BF16, "gup")
    BDW, rBDW = self.view("EX", 34 * K1 + 512, [128, 128], BF16, "bdw")
    BDA, rBDA = self.view("EX", 34 * K1 + 768, [128, 128], BF16, "bda")
    BONES, rBONES = self.view("EX", 35 * K1, [128, 128], F32, "bones")
    o = 35 * K1 + 512
    RWC, rRWC = self.view("EX", o, [128, 12, 4], F32, "rwc"); o += 192
    RKC, rRKC = self.view("EX", o, [128, 12], F32, "rkc"); o += 48
    OMKA, rOMKA = self.view("EX", o, [128, 12], F32, "omka"); o += 48
    MUT, rMUT = self.view("EX", o, [128, 36, 2], F32, "mut"); o += 288
    C0T, rC0T = self.view("EX", o, [128, 36], F32, "c0t"); o += 144
    MUL, rMUL = self.view("EX", o, [128, 3, 2], F32, "mul"); o += 24
    C0L, rC0L = self.view("EX", o, [128, 4], F32, "c0l"); o += 16
    PCS, rPCS = self.view("EX", o, [64, 2, 16], F32, "pcs"); o += 128
    ST, rST0 = self.view("EX", o, [64, 2, 64], F32, "st"); o += 512
    GNS, rGNS = self.view("EX", o, [128, 64], F32, "gns"); o += 256
    YC, rYC = self.view("EX", o, [128, 64], F32, "yc"); o += 256
    YB, rYB = self.view("EX", o, [128, 64], F32, "yb"); o += 256
    LNG, rLNG = self.view("EX", o, [128, 64], F32, "lng"); o += 256
    LNB, rLNB = self.view("EX", o, [128, 64], F32, "lnb"); o += 256
    assert o <= 40 * K1
    rST = [rST0, self.R("st1")]
    RAW, rRAW = self.view("MG", 0, [128, T + 2], F32, "raw")
    BT, rBT = self.view("MG", 8 * K1 + 512, [128, T], F32, "bt")
    RKD, rRKD = self.view("MG", 16 * K1 + 512, [128, T], F32, "rkd")
    SGD, rSGD = self.view("MG", 24 * K1 + 512, [128, T], BF16, "sgd")
    WUP, rWUP = self.view("MG", 28 * K1 + 512, [128, 768], BF16, "wup")
    AUP, rAUP = self.view("MG", 30 * K1, [128, 768], BF16, "aup")
    TW = self.WB[1][:].rearrange("p a b -> p (a b)")[:, 0:T]
    AD = self.WB[1][:].rearrange("p a b -> p (a b)")[:, T:2 * T]
    rTW = self.rWB[1]
    WB0, rWB0 = self.WB[0], self.rWB[0]
    IDF, ONES = self.IDF, self.ONES

    P.dma(RWC, self.rwcol[l], writes=[rRWC])
    P.dma(RKC, self.rkcol[l], writes=[rRKC])
    P.dma(MUT, self.muT[l], writes=[rMUT])
    P.dma(MUL, self.muL[l], writes=[rMUL])
    P.dma(WUP, self.w_up[l], writes=[rWUP], eng="pool")
    P.dma(AUP, self.a_up[l], writes=[rAUP], eng="pool")
    P.dma(GUP, self.g_up[l], writes=[rGUP], eng="pool")
    P.op("dve", lambda e: e.tensor_scalar(out=OMKA, in0=RWC[:, :, 3], scalar1=-1.0, scalar2=1.0, op0=ALU.mult, op1=ALU.add), reads=[rRWC], writes=[rOMKA])
    P.op("dve", lambda e: e.tensor_tensor(out=C0T, in0=MUT[:, :, 0], in1=MUT[:, :, 1], op=ALU.add), reads=[rMUT], writes=[rC0T])
    P.op("dve", lambda e: e.tensor_scalar(out=C0T, in0=C0T, scalar1=-1.0, scalar2=1.0, op0=ALU.mult, op1=ALU.add), reads=[rC0T], writes=[rC0T])
    P.op("dve", lambda e: e.tensor_tensor(out=C0L[:, 0:3], in0=MUL[:, :, 0], in1=MUL[:, :, 1], op=ALU.add), reads=[rMUL], writes=[rC0L])
    P.op("dve", lambda e: e.tensor_scalar(out=C0L[:, 0:3], in0=C0L[:, 0:3], scalar1=-1.0, scalar2=1.0, op0=ALU.mult, op1=ALU.add), reads=[rC0L], writes=[rC0L])
    P.op("dve", lambda e: e.memset(BONES, 0.0), writes=[rBONES])
    P.op("dve", lambda e: e.memset(BONES[0:64, 0:64], 1.0), writes=[rBONES])
    P.op("dve", lambda e: e.memset(BONES[64:128, 64:128], 1.0), writes=[rBONES])
    P.op("dve", lambda e: e.memset(BDW, 0.0), writes=[rBDW])
    P.op("dve", lambda e: e.memset(BDA, 0.0), writes=[rBDA])
    P.op("dve", lambda e: e.memset(RAW[:, 0:1], 0.0), writes=[rRAW])
    P.op("dve", lambda e: e.memset(RAW[:, T + 1:T + 2], 0.0), writes=[rRAW])
    P.op("dve", lambda e: e.memset(BON, 0.0), writes=[rBON])
    cnt = [0]

    def proj_shift(wb, rwb, wcols, mu0, mu1, c0, rmu, dst, rdst, post):
        for tb in range(4):
            pb, rpb = self.PB[cnt[0] % 2], self.rPB[cnt[0] % 2]
            cnt[0] += 1

            def f(e, tb=tb, pb=pb, wb=wb, wcols=wcols):
                for kc in range(KC):
                    ins = e.matmul(pb[:, :], lhsT=wb[:, kc, wcols], rhs=self.HT[:, kc, tb * 512:(tb + 1) * 512], start=(kc == 0), stop=(kc == KC - 1))
                return ins
            P.op("pe", f, reads=[rwb, self.rHT], writes=[rpb])
            P.op("act", lambda e, tb=tb, pb=pb: e.activation(out=RAW[:, 1 + tb * 512:1 + (tb + 1) * 512], in_=pb[:, :], func=AF.Copy), reads=[rpb], writes=[rRAW])
        post(mu0, mu1, c0, rmu, dst, rdst)

    def shift_to(mu0, mu1, c0, rmu, dst, rdst):
        P.op("dve", lambda e: e.tensor_scalar(out=dst, in0=RAW[:, 1:T + 1], scalar1=c0, scalar2=None, op0=ALU.mult), reads=[rRAW] + rmu, writes=[rdst])
        P.op("dve", lambda e: e.scalar_tensor_tensor(out=dst, in0=RAW[:, 0:T], scalar=mu0, in1=dst, op0=ALU.mult, op1=ALU.add), reads=[rRAW, rdst] + rmu, writes=[rdst])
        P.op("dve", lambda e: e.scalar_tensor_tensor(out=dst, in0=RAW[:, 2:T + 2], scalar=mu1, in1=dst, op0=ALU.mult, op1=ALU.add), reads=[rRAW, rdst] + rmu, writes=[rdst])

    TMPL, rTMPL = A(7, name="tmpl")
    wsrc = self.w_in[l]
    self.P.dma(WB0[:, :, 0:384], wsrc[:, B_OFF + 2304:B_OFF + 2688].rearrange("(kc p) n -> p kc n", p=128), writes=[rWB0], eng="pool")
    for q, (dstv, rdstv, fn_) in enumerate(((TW, rTW, AF.Tanh), (AD, rTW, AF.Copy), (SGD, rSGD, AF.Sigmoid))):
        def post(mu0, mu1, c0, rmu, dst, rdst, dstv=dstv, rdstv=rdstv, fn_=fn_):
            shift_to(mu0, mu1, c0, rmu, dst, rdst)
            P.op("act", lambda e: e.activation(out=dstv, in_=dst, func=fn_), reads=[rdst], writes=[rdstv])
        proj_shift(WB0, rWB0, slice(q * 128, (q + 1) * 128), MUL[:, q, 0:1], MUL[:, q, 1:2], C0L[:, q:q + 1], [rMUL, rC0L], TMPL, rTMPL, post)

    MASK = [self.CST[:, 128:768], self.CST[:, 768:1408]]
    def do_head(h):
        Rr, rR = A(0, name="r")
        Kk, rK = A(1, name="k")
        Vv, rV = A(2, name="v")
        SG, rSG = A(3, name="sg")
        CS, rCS = A(4, name="cs")
        Pp, rPp = A(5, name="p")
        AAa, rAA = A(6, name="aa")
        KK, rKK = A(7, name="kk")
        for w_ in range(3):
            col = B_OFF + w_ * 768 + h * 64
            for dup in range(2):
                P.dma(WB0[:, :, w_ * 128 + dup * 64:w_ * 128 + (dup + 1) * 64], wsrc[:, col:col + 64].rearrange("(kc p) n -> p kc n", p=128), writes=[rWB0], eng="pool")
        for w_, (dst, rdst) in enumerate(((Rr, rR), (Kk, rK), (Vv, rV))):
            ci = w_ * 12 + h
            proj_shift(WB0, rWB0, slice(w_ * 128, (w_ + 1) * 128), MUT[:, ci, 0:1], MUT[:, ci, 1:2], C0T[:, ci:ci + 1], [rMUT, rC0T], dst, rdst, shift_to)
        hc = slice(h * 64, (h + 1) * 64)
        for (BD, rBD, UP, rUP) in ((BDW, rBDW, WUP, rWUP), (BDA, rBDA, AUP, rAUP)):
            P.op("pool", lambda e, BD=BD, UP=UP: e.tensor_copy(out=BD[0:64, 0:64], in_=UP[0:64, hc]), reads=[rUP], writes=[rBD])
            P.op("pool", lambda e, BD=BD, UP=UP: e.tensor_copy(out=BD[64:128, 64:128], in_=UP[64:128, hc]), reads=[rUP], writes=[rBD])
        for (BD, rBD, src, dst, rdst, bcol) in ((BDW, rBDW, TW, SG, rSG, 0), (BDA, rBDA, AD, AAa, rAA, 1)):
            for tb in range(4):
                pb, rpb = self.PB[cnt[0] % 2], self.rPB[cnt[0] % 2]
                cnt[0] += 1
                P.op("pe", lambda e, pb=pb, BD=BD, src=src, tb=tb: e.matmul(pb[:, :], lhsT=BD, rhs=src[:, tb * 512:(tb + 1) * 512], start=True, stop=True), reads=[rBD, rTW], writes=[rpb])
                P.op("act", lambda e, pb=pb, dst=dst, tb=tb, bcol=bcol: e.activation(out=dst[:, tb * 512:(tb + 1) * 512], in_=pb[:, :], func=AF.Sigmoid, bias=RWC[:, h, bcol:bcol + 1]),
                     reads=[rpb, rRWC], writes=[rdst])
        for t in range(NT):
            c0_, c1_ = t * 128, (t + 1) * 128
            P.op("dve", lambda e, c0_=c0_, c1_=c1_: e.tensor_tensor_scan(out=CS[0:64, c0_:c1_], data0=ONES[0:64, :], data1=SG[0:64, c0_:c1_], initial=0.0, op0=ALU.mult, op1=ALU.add),
                 reads=[rSG, self.rCST], writes=[rCS])
            P.op("dve", lambda e, c0_=c0_: e.tensor_tensor_scan(out=_rev(CS[64:128, :], c0_, 128), data0=ONES[64:128, :], data1=_rev(SG[64:128, :], c0_, 128), initial=0.0, op0=ALU.mult, op1=ALU.add),
                 reads=[rSG, self.rCST], writes=[rCS])
        P.op("dve", lambda e: e.tensor_tensor(out=SG, in0=CS, in1=SG, op=ALU.subtract), reads=[rCS, rSG], writes=[rSG])
        P.op("act", lambda e: e.activation(out=SG, in_=SG, func=AF.Exp, scale=-CDEC), reads=[rSG], writes=[rSG])
        P.op("act", lambda e: e.activation(out=Pp, in_=CS, func=AF.Exp, scale=-CDEC), reads=[rCS], writes=[rPp])
        P.op("act", lambda e: e.activation(out=CS, in_=CS, func=AF.Exp, scale=CDEC), reads=[rCS], writes=[rCS])
        PPv, PI = SG, CS
        P.op("dve", lambda e: e.tensor_copy(out=PCS[:, 0, :], in_=Pp[0:64, 127:T:128]), reads=[rPp], writes=[rPCS])
        pb, rpb = self.PB[cnt[0] % 2], self.rPB[cnt[0] % 2]
        cnt[0] += 1
        P.op("pe", lambda e, pb=pb: e.matmul(pb[0:64, 0:16], lhsT=IDF[64:128, 64:128], rhs=Pp[64:128, 0:T:128], start=True, stop=True), reads=[rPp, self.rCST], writes=[rpb])
        P.op("dve", lambda e, pb=pb: e.tensor_copy(out=PCS[:, 1, :], in_=pb[0:64, 0:16]), reads=[rpb], writes=[rPCS])
        P.op("dve", lambda e: e.tensor_scalar(out=KK, in0=Kk, scalar1=RWC[:, h, 2:3], scalar2=None, op0=ALU.mult), reads=[rK, rRWC], writes=[rKK])
        P.op("pool", lambda e: e.tensor_tensor(out=RKD, in0=KK, in1=KK, op=ALU.mult), reads=[rKK], writes=[rRKD])
        for tb in range(4):
            pb, rpb = self.PB[cnt[0] % 2], self.rPB[cnt[0] % 2]
            cnt[0] += 1
            P.op("pe", lambda e, pb=pb, tb=tb: e.matmul(pb[:, :], lhsT=BONES, rhs=RKD[:, tb * 512:(tb + 1) * 512], start=True, stop=True), reads=[rBONES, rRKD], writes=[rpb])
            P.op("dve", lambda e, pb=pb, tb=tb: e.tensor_scalar(out=BT[:, tb * 512:(tb + 1) * 512], in0=pb[:, :], scalar1=1e-24, scalar2=None, op0=ALU.max), reads=[rpb], writes=[rBT])
        P.op("act", lambda e: e.activation(out=BT, in_=BT, func=AF.Ln), reads=[rBT], writes=[rBT])
        P.op("act", lambda e: e.activation(out=BT, in_=BT, func=AF.Exp, scale=-0.5), reads=[rBT], writes=[rBT])
        P.op("dve", lambda e: e.tensor_tensor(out=KK, in0=KK, in1=BT, op=ALU.mult), reads=[rKK, rBT], writes=[rKK])
        AT = PPv
        P.op("dve", lambda e: e.scalar_tensor_tensor(out=AT, in0=KK, scalar=-1.0, in1=PPv, op0=ALU.mult, op1=ALU.mult), reads=[rKK, rSG], writes=[rSG])
        P.op("pool", lambda e: e.tensor_tensor(out=BT, in0=KK, in1=AAa, op=ALU.mult), reads=[rKK, rAA], writes=[rBT])
        P.op("dve", lambda e: e.tensor_tensor(out=BT, in0=BT, in1=PI, op=ALU.mult), reads=[rBT, rCS], writes=[rBT])
        P.op("dve", lambda e: e.tensor_scalar(out=AAa, in0=AAa, scalar1=RWC[:, h, 3:4], scalar2=OMKA[:, h:h + 1], op0=ALU.mult, op1=ALU.add), reads=[rAA, rRWC, rOMKA], writes=[rAA])
        P.op("pool", lambda e: e.tensor_tensor(out=AAa, in0=AAa, in1=Kk, op=ALU.mult), reads=[rAA, rK], writes=[rAA])
        P.op("dve", lambda e: e.tensor_tensor(out=RKD, in0=Rr, in1=AAa, op=ALU.mult), reads=[rR, rAA], writes=[rRKD])
        P.op("pool", lambda e: e.tensor_tensor(out=AAa, in0=AAa, in1=PI, op=ALU.mult), reads=[rAA, rCS], writes=[rAA])
        P.op("dve", lambda e: e.tensor_tensor(out=Rr, in0=Rr, in1=Pp, op=ALU.mult), reads=[rR, rPp], writes=[rR])
        KT, RT = AAa, Rr
        rAT, rKT, rRT = rSG, rAA, rR
        pbb, rpbb = self.PB[cnt[0] % 2], self.rPB[cnt[0] % 2]
        cnt[0] += 1

        def fbon(e, pbb=pbb):
            for t in range(NT):
                ins = e.matmul(pbb[:, t:t + 1], lhsT=RKD[:, t * 128:(t + 1) * 128], rhs=RKC[:, h:h + 1], start=True, stop=True)
            return ins
        P.op("pe", fbon, reads=[rRKD, rRKC], writes=[rpbb])
        P.op("dve", lambda e, pbb=pbb: e.tensor_copy(out=BON[:, :, h], in_=pbb[:, 0:16]), reads=[rpbb], writes=[rBON])
        IB = []
        for d_ in range(2):
            base = (4 + d_) * SL
            AM = self.view("ARENA", base, [128, 640], F32, f"am{d_}")
            LV = self.view("ARENA", base + 2560, [128, 384], F32, f"lv{d_}")
            TK = self.view("ARENA", base + 4096, [128, 256], F32, f"tk{d_}")
            AV = self.view("ARENA", base + 5120, [128, 64], F32, f"av{d_}")
            XU = self.view("ARENA", base + 5376, [128, 128], F32, f"xu{d_}")
            RN = self.view("ARENA", base + 5888, [64, 192], F32, f"rn{d_}")
            IB.append((AM, LV, TK, AV, XU, RN))
        P.op("dve", lambda e: e.memset(ST, 0.0), writes=[rST[0], rST[1]])
        P.op("pool", lambda e: e.memset(YH, 0.0), writes=[rYH])
        for step in range(NT):
            tiles = (step, NT - 1 - step)
            for d_ in range(2):
                (AM, rAM), (LV, rLV), (TK, rTK), (AV, rAV), (XU, rXU), (RN, rRN) = IB[d_]
                ti = tiles[d_]
                tc_ = slice(ti * 128, (ti + 1) * 128)
                rows = slice(d_ * 64, (d_ + 1) * 64)
                px, rpx = self.PB[2 + d_], self.rPB[2 + d_]
                pd, rpd = self.PB[4 + d_], self.rPB[4 + d_]
                pm = self.PW[:, d_ * 512:(d_ + 1) * 512]
                rpm = self.rPW if d_ == 0 else self.rPB[0]
                if d_ == 1:
                    pm = self.PB[0]
                idb = IDF[rows, rows]

                def fa(e, px=px, pd=pd, rows=rows, tc_=tc_):
                    e.matmul(px[:, 0:128], lhsT=BT[rows, tc_], rhs=AT[rows, tc_], start=True, stop=True)
                    e.matmul(px[:, 128:256], lhsT=BT[rows, tc_], rhs=RT[rows, tc_], start=True, stop=True)
                    e.matmul(px[:, 256:384], lhsT=KT[rows, tc_], rhs=AT[rows, tc_], start=True, stop=True)
                    e.matmul(px[:, 384:512], lhsT=KT[rows, tc_], rhs=RT[rows, tc_], start=True, stop=True)
                    return e.matmul(pd[:, 0:128], lhsT=AT[rows, tc_], rhs=BT[rows, tc_], start=True, stop=True)
                P.op("pe", fa, reads=[rBT, rAT, rKT, rRT], writes=[rpx, rpd])
                P.op("dve", lambda e, AM=AM, px=px, d_=d_: e.tensor_tensor(out=AM[:, 0:512], in0=px[:, :], in1=MASK[d_][:, 0:512], op=ALU.mult), reads=[rpx, self.rCST], writes=[rAM])
                P.op("dve", lambda e, LV=LV, pd=pd, d_=d_: e.tensor_tensor(out=LV[:, 0:128], in0=pd[:, 0:128], in1=MASK[d_][:, 512:640], op=ALU.mult), reads=[rpd, self.rCST], writes=[rLV])
                P.op("act", lambda e, LV=LV, AM=AM: e.activation(out=LV[:, 128:256], in_=AM[:, 0:128], func=AF.Copy), reads=[rAM], writes=[rLV])
                P.op("pool", lambda e, LV=LV, AM=AM: e.tensor_tensor(out=LV[:, 256:384], in0=AM[:, 0:128], in1=IDF, op=ALU.add), reads=[rAM, self.rCST], writes=[rLV])
                def ft(e, pm=pm, rows=rows, tc_=tc_, idb=idb):
                    e.matmul(pm[:, 0:64], lhsT=AT[rows, tc_], rhs=idb, start=True, stop=True)
                    e.matmul(pm[:, 64:128], lhsT=BT[rows, tc_], rhs=idb, start=True, stop=True)
                    e.matmul(pm[:, 128:192], lhsT=KT[rows, tc_], rhs=idb, start=True, stop=True)
                    return e.matmul(pm[:, 192:256], lhsT=Vv[rows, tc_], rhs=idb, start=True, stop=True)
                P.op("pe", ft, reads=[rBT, rAT, rKT, rV, self.rCST], writes=[rpm])
                P.op("act", lambda e, TK=TK, pm=pm: e.activation(out=TK, in_=pm[:, 0:256], func=AF.Copy), reads=[rpm], writes=[rTK])
                P.op("pe", lambda e, pm=pm, AM=AM, TK=TK: e.matmul(pm[:, 256:320], lhsT=AM[:, 256:384], rhs=TK[:, 192:256], start=True, stop=True), reads=[rAM, rTK], writes=[rpm])
                P.op("act", lambda e, AV=AV, pm=pm: e.activation(out=AV, in_=pm[:, 256:320], func=AF.Copy), reads=[rpm], writes=[rAV])
            for k in range(7):
                for d_ in range(2):
                    (AM, rAM), (LV, rLV), (TK, rTK), (AV, rAV), (XU, rXU), (RN, rRN) = IB[d_]
                    pd, rpd = self.PB[4 + d_], self.rPB[4 + d_]
                    if k == 0:
                        def f0(e, pd=pd, LV=LV):
                            e.matmul(pd[:, 0:128], lhsT=LV[:, 128:256], rhs=LV[:, 0:128], start=True, stop=True)
                            return e.matmul(pd[:, 128:256], lhsT=LV[:, 0:128], rhs=LV[:, 128:256], start=True, stop=True)
                        P.op("pe", f0, reads=[rLV], writes=[rpd])
                        P.op("act" if d_ else "dve", (lambda e, pd=pd, LV=LV: e.activation(out=LV[:, 0:256], in_=pd[:, 0:256], func=AF.Copy)) if d_ else
                             (lambda e, pd=pd, LV=LV: e.tensor_copy(out=LV[:, 0:256], in_=pd[:, 0:256])), reads=[rpd], writes=[rLV])
                    elif k < 6:
                        def fk(e, pd=pd, LV=LV):
                            e.matmul(pd[:, 0:128], lhsT=LV[:, 128:256], rhs=LV[:, 0:128], start=True, stop=True)
                            return e.matmul(pd[:, 128:384], lhsT=LV[:, 0:128], rhs=LV[:, 128:384], start=True, stop=True)
                        P.op("pe", fk, reads=[rLV], writes=[rpd])
                        P.op("dve", lambda e, pd=pd, LV=LV: e.tensor_tensor(out=LV[:, 256:384], in0=pd[:, 256:384], in1=LV[:, 256:384], op=ALU.add), reads=[rpd, rLV], writes=[rLV])
                        P.op("act", lambda e, pd=pd, LV=LV: e.activation(out=LV[:, 0:256], in_=pd[:, 0:256], func=AF.Copy), reads=[rpd], writes=[rLV])
                    else:
                        P.op("pe", lambda e, pd=pd, LV=LV: e.matmul(pd[:, 256:384], lhsT=LV[:, 0:128], rhs=LV[:, 256:384], start=True, stop=True), reads=[rLV], writes=[rpd])
                        P.op("dve", lambda e, pd=pd, LV=LV: e.tensor_tensor(out=LV[:, 256:384], in0=pd[:, 256:384], in1=LV[:, 256:384], op=ALU.add), reads=[rpd, rLV], writes=[rLV])
            for d_ in range(2):
                (AM, rAM), (LV, rLV), (TK, rTK), (AV, rAV), (XU, rXU), (RN, rRN) = IB[d_]
                ti = tiles[d_]
                tc_ = slice(ti * 128, (ti + 1) * 128)
                rows = slice(d_ * 64, (d_ + 1) * 64)
                pm = self.PW[:, d_ * 512:(d_ + 1) * 512] if d_ == 0 else self.PB[0]
                rpm = self.rPW if d_ == 0 else self.rPB[0]
                px, rpx = self.PB[2 + d_], self.rPB[2 + d_]
                idb = IDF[rows, rows]
                ZT = LV[:, 256:384]
                def fx(e, pm=pm, ZT=ZT, TK=TK, AV=AV):
                    e.matmul(pm[:, 320:384], lhsT=ZT, rhs=TK[:, 0:64], start=True, stop=True)
                    return e.matmul(pm[:, 384:448], lhsT=ZT, rhs=AV, start=True, stop=True)
                P.op("pe", fx, reads=[rLV, rTK, rAV], writes=[rpm])
                P.op("dve", lambda e, XU=XU, pm=pm: e.tensor_copy(out=XU, in_=pm[:, 320:448]), reads=[rpm], writes=[rXU])
                def fr(e, px=px, XU=XU, AM=AM, TK=TK, rows=rows, tc_=tc_, idb=idb):
                    e.matmul(px[0:64, 0:128], lhsT=XU[:, 0:64], rhs=AM[:, 128:256], start=True, stop=False)
                    e.matmul(px[0:64, 0:128], lhsT=idb, rhs=RT[rows, tc_], start=False, stop=True)
                    e.matmul(px[0:64, 128:192], lhsT=XU[:, 0:64], rhs=TK[:, 64:128], start=True, stop=False)
                    return e.matmul(px[0:64, 128:192], lhsT=IDF[0:64, 0:64], rhs=IDF[0:64, 0:64], start=False, stop=True)
                P.op("pe", fr, reads=[rXU, rAM, rTK, rRT, self.rCST], writes=[rpx])
                P.op("act", lambda e, RN=RN, px=px: e.activation(out=RN, in_=px[0:64, 0:192], func=AF.Copy), reads=[rpx], writes=[rRN])
                Sd = ST[:, d_, :]
                def fy(e, pm=pm, AM=AM, XU=XU, TK=TK, RN=RN, Sd=Sd):
                    e.matmul(pm[:, 448:512], lhsT=AM[:, 128:256], rhs=XU[:, 64:128], start=True, stop=False)
                    e.matmul(pm[:, 448:512], lhsT=AM[:, 384:512], rhs=TK[:, 192:256], start=False, stop=False)
                    e.matmul(pm[:, 448:512], lhsT=RN[:, 0:128], rhs=Sd, start=False, stop=True)
                    e.matmul(pm[0:64, 0:64], lhsT=TK[:, 64:128], rhs=XU[:, 64:128], start=True, stop=False)
                    e.matmul(pm[0:64, 0:64], lhsT=TK[:, 128:192], rhs=TK[:, 192:256], start=False, stop=False)
                    return e.matmul(pm[0:64, 0:64], lhsT=RN[:, 128:192], rhs=Sd, start=False, stop=True)
                P.op("pe", fy, reads=[rAM, rXU, rTK, rRN, rST[d_]], writes=[rpm])
                P.op("dve", lambda e, pm=pm, ti=ti: e.tensor_tensor(out=YH[:, ti, :], in0=pm[:, 448:512], in1=YH[:, ti, :], op=ALU.add), reads=[rpm, rYH], writes=[rYH])
                P.op("dve", lambda e, pm=pm, Sd=Sd, d_=d_, ti=ti: e.tensor_scalar(out=Sd, in0=pm[0:64, 0:64], scalar1=PCS[:, d_, ti:ti + 1], scalar2=None, op0=ALU.mult), reads=[rpm, rPCS], writes=[rST[d_]])
        P.dma(LNG, self.ln_g[l:l + 1, h * 64:(h + 1) * 64].partition_broadcast(128), writes=[rLNG])
        P.dma(LNB, self.ln_b[l:l + 1, h * 64:(h + 1) * 64].partition_broadcast(128), writes=[rLNB])
        MEAN, VAR = GNS[:, 0:16], GNS[:, 16:32]
        P.op("dve", lambda e: e.tensor_reduce(out=MEAN, in_=YH, axis=AX.X, op=ALU.add), reads=[rYH], writes=[rGNS])
        P.op("dve", lambda e: e.tensor_scalar(out=MEAN, in0=MEAN, scalar1=1.0 / 64, scalar2=None, op0=ALU.mult), reads=[rGNS], writes=[rGNS])
        for t in range(NT):
            P.op("dve", lambda e, t=t: e.tensor_scalar(out=YH[:, t, :], in0=YH[:, t, :], scalar1=MEAN[:, t:t + 1], scalar2=None, op0=ALU.subtract), reads=[rYH, rGNS], writes=[rYH])
            P.op("act", lambda e, t=t: e.activation(out=YC, in_=YH[:, t, :], func=AF.Square, accum_out=VAR[:, t:t + 1]), reads=[rYH], writes=[rYC, rGNS])
        P.op("act", lambda e: e.activation(out=VAR, in_=VAR, func=AF.Ln, scale=1.0 / 64, bias=64e-5), reads=[rGNS], writes=[rGNS])
        P.op("act", lambda e: e.activation(out=VAR, in_=VAR, func=AF.Exp, scale=-0.5), reads=[rGNS], writes=[rGNS])
        for t in range(NT):
            tc_ = slice(t * 128, (t + 1) * 128)
            pb, rpb = self.PB[cnt[0] % 2], self.rPB[cnt[0] % 2]
            cnt[0] += 1

            def fe(e, pb=pb, tc_=tc_):
                e.matmul(pb[:, 0:64], lhsT=Vv[0:64, tc_], rhs=IDF[0:64, 0:64], start=True, stop=True)
                return e.matmul(pb[:, 64:128], lhsT=SGD[:, tc_], rhs=GUP[:, hc], start=True, stop=True)
            P.op("pe", fe, reads=[rV, rSGD, rGUP, self.rCST], writes=[rpb])
            P.op("dve", lambda e, t=t: e.scalar_tensor_tensor(out=YC, in0=YH[:, t, :], scalar=VAR[:, t:t + 1], in1=LNG, op0=ALU.mult, op1=ALU.mult), reads=[rYH, rGNS, rLNG], writes=[rYC])
            P.op("pool", lambda e: e.tensor_tensor(out=YC, in0=YC, in1=LNB, op=ALU.add), reads=[rYC, rLNB], writes=[rYC])
            P.op("dve", lambda e, t=t, pb=pb: e.scalar_tensor_tensor(out=YB, in0=pb[:, 0:64], scalar=BON[:, t, h:h + 1], in1=YC, op0=ALU.mult, op1=ALU.add), reads=[rpb, rBON, rYC], writes=[rYB])
            P.op("dve", lambda e, t=t, pb=pb: e.tensor_tensor(out=OBK[:, t, (h % 2) * 64:(h % 2) * 64 + 64], in0=YB, in1=pb[:, 64:128], op=ALU.mult), reads=[rYB, rpb], writes=[rOBK])
        if h % 2 == 1:
            j = h // 2
            for tq in range(4):
                pbt = self.PB[cnt[0] % 2]
                rpbt = self.rPB[cnt[0] % 2]
                cnt[0] += 1

                def ftr(e, pbt=pbt, tq=tq):
                    for tt in range(4):
                        ins = e.matmul(pbt[:, tt * 128:(tt + 1) * 128], lhsT=OBK[:, tq * 4 + tt, :], rhs=self.IDB[:], start=True, stop=True)
                    return ins
                P.op("pe", ftr, reads=[rOBK, self.rIDB], writes=[rpbt])
                P.op("act", lambda e, pbt=pbt, tq=tq, j=j: e.activation(out=OBT[:, j, tq * 512:(tq + 1) * 512], in_=pbt[:, 0:512], func=AF.Copy), reads=[rpbt], writes=[rOBT])
    for h_ in range(12):
        do_head(h_)
    if "ob" in self.dbg:
        self.dump("dbg_ob", OBT, rOBT, [128, 6, T], BF16)
    WBRB, rWBRB = self.view("ARENA", 0, [128, 6, D], BF16, "wbrb")
    P.dma(WBRB, self.w_br_b[l].rearrange("(j p) n -> p j n", p=128), writes=[rWBRB], eng="pool")
    self.release("MG")
    self.merge(l, 1, lambda j: (WBRB[:, j, :], OBT[:, j, :], [rWBRB, rOBT]), 6)


Builder.rwkv_phase = _rwkv_phase
```

```python
import math
import numpy as np
from contextlib import ExitStack
import concourse.bass as bass
import concourse.mybir as mybir
from concourse.bass_utils import run_bass_kernel_spmd

F32 = mybir.dt.float32
BF16 = mybir.dt.bfloat16
AF = mybir.ActivationFunctionType
ALU = mybir.AluOpType
AX = mybir.AxisListType

D = 1024
T = 2048
NT = 16
KC = 8
L = 2
E = 32
INC = 10368
SEM_CHUNK = 30000
A_OFF, B_OFF, C_OFF, G_OFF = 0, 2304, 4992, 7296
DECAY_C = math.exp(-0.5)


class Res:
    __slots__ = ("name", "w", "r", "dsem", "dcnt")

    def __init__(self, name):
        self.name = name
        self.w = None
        self.r = []
        self.dsem = None
        self.dcnt = 0


class Prog:
    ENGS = ("sp", "act", "dve", "pool", "pe")

    def __init__(self, nc):
        self.nc = nc
        self.q = {e: [] for e in self.ENGS}
        self.cnt = {e: 0 for e in self.ENGS}
        self.known = {e: {} for e in self.ENGS}
        self.ndsem = 0
        self.dma_final = {}

    def _collect(self, eng, reads, writes, is_dma):
        waits = {}

        def need(ev, kind):
            if ev is None:
                return
            key, val, src = ev
            if src == eng and not is_dma and key[0] == "e":
                if eng == "pe" or kind == "war":
                    return
            if self.known[eng].get(key, 0) >= val:
                return
            if waits.get(key, 0) < val:
                waits[key] = val

        for r in reads:
            need(r.w, "raw")
        for w in writes:
            need(w.w, "waw")
            for ev in w.r:
                need(ev, "war")
        for k, v in waits.items():
            self.known[eng][k] = v
        return waits

    def _commit(self, ev, reads, writes):
        for r in reads:
            r.r.append(ev)
            if len(r.r) > 24:
                best = {}
                for e2 in r.r:
                    if e2[0] not in best or best[e2[0]][1] < e2[1]:
                        best[e2[0]] = e2
                r.r = list(best.values())
        for w in writes:
            w.w = ev
            w.r = []

    def op(self, eng, fn, reads=(), writes=()):
        waits = self._collect(eng, reads, writes, False)
        n = self.cnt[eng] + 1
        self.cnt[eng] = n
        ev = (("e", eng, (n - 1) // SEM_CHUNK), (n - 1) % SEM_CHUNK + 1, eng)
        self.q[eng].append((waits, fn, [(ev[0], 1)]))
        self._commit(ev, reads, writes)
        return ev

    def dma(self, out_ap, in_ap, reads=(), writes=(), eng="sp", **kw):
        waits = self._collect(eng, reads, writes, True)
        w0 = writes[0]
        if w0.dsem is None:
            w0.dsem = self.ndsem
            self.ndsem += 1
        w0.dcnt += 16
        ev = (("d", w0.dsem), w0.dcnt, "dma")
        self.dma_final[ev[0]] = w0.dcnt

        def fn(e, out_ap=out_ap, in_ap=in_ap, kw=kw):
            return e.dma_start(out=out_ap, in_=in_ap, **kw)

        self.q[eng].append((waits, fn, [(ev[0], 16)]))
        self._commit(ev, reads, writes)
        return ev

    def final_wait(self, eng, res_list):
        waits = dict(self.dma_final)
        for r in res_list:
            if r.w is not None:
                key, val, _ = r.w
                waits[key] = max(waits.get(key, 0), val)
        self.q[eng].append((waits, None, []))

    def emit(self, stack):
        nc = self.nc
        sems = {}
        for e in self.ENGS:
            for waits, fn, incs in self.q[e]:
                for k in list(waits) + [k for k, _ in incs]:
                    if k not in sems:
                        sems[k] = stack.enter_context(nc.semaphore("s_" + "_".join(str(x) for x in k)))
        self.nsems = len(sems)
        block = stack.enter_context(nc.Block())

        def replay(engname):
            def body(e):
                for waits, fn, incs in self.q[engname]:
                    for k, v in waits.items():
                        e.wait_ge(sems[k], v)
                    if fn is None:
                        continue
                    ins = fn(e)
                    for k, v in incs:
                        ins = ins.then_inc(sems[k], v)
            return body

        block.sync(replay("sp"))
        block.scalar(replay("act"))
        block.vector(replay("dve"))
        block.gpsimd(replay("pool"))
        block.tensor(replay("pe"))


def _t5_bucket_np(rel):
    nb, max_exact = 16, 8
    rel = np.asarray(rel, np.int64)
    ret = np.where(rel > 0, nb, 0)
    n = np.abs(rel)
    nf = np.maximum(n, 1).astype(np.float32)
    large = max_exact + (np.log(nf / np.float32(max_exact)) / np.float32(math.log(128 / max_exact)) * np.float32(nb - max_exact)).astype(np.int32)
    large = np.minimum(large, nb - 1)
    return ret + np.where(n < max_exact, n, large)


def _host_consts():
    cst = np.zeros((128, 1792), np.float32)
    cst[:, 0:128] = np.eye(128, dtype=np.float32)
    r = np.arange(128)[:, None]
    c = np.arange(128)[None, :]
    su, iu = (r < c), (r <= c)
    sl, il = (r > c), (r >= c)
    cst[:, 128:768] = np.concatenate([su, iu, su, iu, sl], 1)
    cst[:, 768:1408] = np.concatenate([sl, il, sl, il, su], 1)
    cst[:, 1408:1536] = 1.0
    EA = np.zeros((33, 1536), np.float32)
    for g, d in enumerate((1, 4, 16)):
        j = np.arange(512) - 255
        b = _t5_bucket_np(j * d)
        b = np.where(np.abs(j) <= 64, b, 32)
        EA[b, g * 512 + np.arange(512)] = 1.0
    EC = np.zeros((33, 1536), np.float32)
    b = _t5_bucket_np(np.arange(1536) - 767)
    EC[b, np.arange(1536)] = 1.0
    return cst, EA, EC


class Builder:
    def __init__(self, layers=(0, 1), do_mixer=True, do_moe=True, dbg=()):
        self.layers = layers
        self.do_mixer = do_mixer
        self.do_moe = do_moe
        self.dbg = dbg
        self.nc = bass.Bass("TRN2", target_bir_lowering=False)
        self.st = ExitStack()
        self.P = Prog(self.nc)
        self.outs = []
        self._uid = 0

    def din(self, name, shape, dt=F32):
        return self.nc.dram_tensor(name, list(shape), dt, kind="ExternalInput").ap()

    def dout(self, name, shape, dt=F32):
        return self.nc.dram_tensor(name, list(shape), dt, kind="ExternalOutput").ap()

    def dscratch(self, name, shape, dt=F32):
        return self.nc.dram_tensor(name, list(shape), dt, kind="Internal").ap()

    def sb(self, name, shape, dt=F32):
        return self.st.enter_context(self.nc.sbuf_tensor(name, list(shape), dt))

    def ps(self, name, shape, dt=F32):
        return self.st.enter_context(self.nc.psum_tensor(name, list(shape), dt))

    def R(self, name):
        self._uid += 1
        return Res(f"{name}{self._uid}")

    def dump(self, name, ap_sb, res, shape, dt=F32):
        o = self.dout(name, shape, dt)
        r = self.R("dump")
        self.P.dma(o, ap_sb, reads=[res], writes=[r])
        self.outs.append(r)

    @staticmethod
    def carve(reg, off_bytes, shape, dt):
        n = 1
        for s in shape[1:]:
            n *= s
        esz = 4 if dt == F32 else 2
        assert off_bytes % 4 == 0
        a = off_bytes // 2
        b = a + n * esz // 2
        assert b <= reg.shape[1], (b, reg.shape)
        ap = reg[0:shape[0], a:b]
        if dt == F32:
            ap = ap.bitcast(F32)
        if len(shape) == 2:
            return ap
        names = " ".join(f"d{i}" for i in range(len(shape) - 1))
        kw = {f"d{i}": shape[i + 1] for i in range(len(shape) - 2)}
        return ap.rearrange(f"p ({names}) -> p {names}", **kw)

    def declare(self):
        d = self.din
        self.x_in = d("x", [T, D])
        self.cT_in = d("cT", [128, 8])
        self.w_mod = d("w_mod", [L, D, 6 * D])
        self.bmodT = d("bmodT", [L, 128, 48])
        self.bmod_row = d("bmod_row", [L, 1, 6 * D])
        self.ncol = d("ncol", [L, 128, 16])
        self.w_in = d("w_in", [L, D, INC])
        self.muT = d("muT", [L, 128, 36, 2])
        self.muL = d("muL", [L, 128, 3, 2])
        self.rwcol = d("rwcol", [L, 128, 12, 4])
        self.rkcol = d("rkcol", [L, 128, 12])
        self.w_up = d("w_up", [L, 128, 768])
        self.a_up = d("a_up", [L, 128, 768])
        self.g_up = d("g_up", [L, 128, 768])
        self.ln_g = d("ln_g", [L, 768])
        self.ln_b = d("ln_b", [L, 768])
        self.dlam = d("dlam", [L, 256])
        self.subln = d("subln", [L, 128, 1])
        self.rel_bias = d("rel_bias", [32, 18])
        self.w_br_a = d("w_br_a", [L, 256, D])
        self.w_br_b = d("w_br_b", [L, 768, D])
        self.w_br_c = d("w_br_c", [L, 768, D])
        self.w_out = d("w_out", [L, D, D])
        self.router_w = d("router_w", [L, D, E])
        self.router_b = d("router_b", [L, E])
        self.moe_w1 = d("moe_w1", [L, E, D, 2, 1024])
        self.b1T = d("b1T", [L, 128, E, 2, 8])
        self.moe_w2 = d("moe_w2", [L, E, 1024, D])
        self.moe_b2 = d("moe_b2", [L, E, D])
        self.fng = d("fng", [1, D])
        self.cst_in = d("cst", [128, 1792])
        self.EA_in = d("EA", [33, 1536])
        self.EC_in = d("EC", [33, 1536])
        self.y_out = self.dout("y", [T, D])
        self.x_spill = self.dscratch("x_spill", [T, D])
        self.ebA = self.dscratch("ebA", [12, 512])
        self.ebC = self.dscratch("ebC", [6, 1536])

    def alloc(self):
        sb, ps = self.sb, self.ps
        self.ARENA = sb("ARENA", [128, 32768], BF16)
        self.HT = sb("HT", [128, KC, T], BF16)
        self.MG = sb("MG", [128, 16384], BF16)
        self.EX = sb("EX", [128, 20 * 1024], BF16)
        self.CST = sb("CST", [128, 1792], F32)
        self.IDB = sb("IDB", [128, 128], BF16)
        self.WB = [sb(f"WB{i}", [128, KC, 512], BF16) for i in range(2)]
        self.MODT = sb("MODT", [128, 48], F32)
        self.GEFF = sb("GEFF", [128, 16], F32)
        self.NCOL = sb("NCOL", [128, 16], F32)
        self.SS = sb("SS", [128, 32], F32)
        self.GTB = sb("GTB", [128, 2, D], F32)
        self.CONDT = sb("CONDT", [128, 8], F32)
        self.JUNK = sb("JUNK", [128, D], BF16)
        self.X = self.ARENA[:, :].bitcast(F32).rearrange("p (t d) -> p t d", t=NT)
        self.IDF = self.CST[:, 0:128]
        self.ONES = self.CST[:, 1408:1536]
        self.PB = [ps(f"PB{i}", [128, 512], F32) for i in range(6)]
        self.PW = ps("PW", [128, 1024], F32)
        R = self.R
        self.rX = R("X")
        self.rHT = R("HT")
        self.rMG = R("MG")
        self.rCST = R("CST")
        self.rIDB = R("IDB")
        self.rWB = [R("WB0"), R("WB1")]
        self.rPB = [R(f"PB{i}") for i in range(6)]
        self.rPW = R("PW")
        self.rMODT, self.rGEFF, self.rNCOL, self.rSS = R("MODT"), R("GEFF"), R("NCOL"), R("SS")
        self.rGTB, self.rCONDT, self.rJUNK = R("GTB"), R("CONDT"), R("JUNK")
        self.rEX = R("EX")
        self.wbi = 0

    def prologue(self):
        P = self.P
        P.dma(self.CST[:], self.cst_in[:, :], writes=[self.rCST])
        P.op("dve", lambda e: e.tensor_copy(out=self.IDB[:], in_=self.IDF), reads=[self.rCST], writes=[self.rIDB])
        P.dma(self.CONDT[:], self.cT_in[:, :], writes=[self.rCONDT])
        P.op("act", lambda e: e.activation(out=self.CONDT[:], in_=self.CONDT[:], func=AF.Silu), reads=[self.rCONDT], writes=[self.rCONDT])
        xin = self.x_in.rearrange("(t p) d -> p t d", p=128)
        for q in range(4):
            P.dma(self.X[:, q * 4:(q + 1) * 4, :], xin[:, q * 4:(q + 1) * 4, :], writes=[self.rX])

    def mod_phase(self, l):
        P = self.P
        _w = [self.view("MG", i * 16384, [128, KC, 512], F32, f"wst{i}") for i in range(2)]
        WST = [a for a, _ in _w]
        rW = [r_ for _, r_ in _w]
        PC, rPC = self.PB[0], self.rPB[0]
        PR, rPR = self.PB[1], self.rPB[1]
        wsrc = self.w_mod[l].rearrange("(kc p) n -> p kc n", p=128)
        bt, rbt = self.view("EX", 0, [128, 48], F32, "bt")
        P.dma(bt, self.bmodT[l], writes=[rbt])
        brow, rbrow = self.view("EX", 256, [1, 2 * D], F32, "brow")
        self.GTROW, self.rGTROW = self.view("EX", 256 + 8192, [1, 2 * D], F32, "gtrow")
        P.dma(brow[0:1, 0:D], self.bmod_row[l][:, 2 * D:3 * D], writes=[rbrow])
        P.dma(brow[0:1, D:2 * D], self.bmod_row[l][:, 5 * D:6 * D], writes=[rbrow])
        P.dma(self.NCOL[:], self.ncol[l], writes=[self.rNCOL])
        for cb in range(12):
            s = cb % 2
            vec = cb // 2
            P.dma(WST[s][:], wsrc[:, :, cb * 512:(cb + 1) * 512], writes=[rW[s]])
            if vec in (2, 5):
                col0 = (0 if vec == 2 else D) + (cb % 2) * 512

                def f(e, s=s):
                    for kc in range(KC):
                        ins = e.matmul(PR[0:1, :], lhsT=self.CONDT[:, kc:kc + 1], rhs=WST[s][:, kc, :], start=(kc == 0), stop=(kc == KC - 1))
                    return ins
                P.op("pe", f, reads=[rW[s], self.rCONDT], writes=[rPR])
                P.op("dve", lambda e, col0=col0: e.tensor_tensor(out=self.GTROW[0:1, col0:col0 + 512], in0=PR[0:1, :], in1=brow[0:1, col0:col0 + 512], op=ALU.add),
                     reads=[rPR, rbrow], writes=[self.rGTROW])
            else:
                def f(e, s=s, cb=cb):
                    for j in range(4):
                        for kc in range(KC):
                            ins = e.matmul(PC[:, cb * 4 + j:cb * 4 + j + 1], lhsT=WST[s][:, kc, j * 128:(j + 1) * 128], rhs=self.CONDT[:, kc:kc + 1],
                                           start=(kc == 0), stop=(kc == KC - 1))
                    return ins
                P.op("pe", f, reads=[rW[s], self.rCONDT], writes=[rPC])
        for (a, b) in ((0, 16), (24, 40)):
            P.op("dve", lambda e, a=a, b=b: e.tensor_tensor(out=self.MODT[:, a:b], in0=PC[:, a:b], in1=bt[:, a:b], op=ALU.add),
                 reads=[rPC, rbt], writes=[self.rMODT])
        for i, c0 in ((0, 8), (1, 32)):
            P.op("dve", lambda e, i=i, c0=c0: e.scalar_tensor_tensor(out=self.GEFF[:, i * 8:(i + 1) * 8], in0=self.MODT[:, c0:c0 + 8], scalar=1.0,
                                                                   in1=self.NCOL[:, i * 8:(i + 1) * 8], op0=ALU.add, op1=ALU.mult),
                 reads=[self.rMODT, self.rNCOL], writes=[self.rGEFF])
        for i in range(2):
            for hf in range(2):
                pb, rpb = self.PB[2 + hf], self.rPB[2 + hf]
                P.op("pe", lambda e, i=i, hf=hf, pb=pb: e.matmul(pb[:, :], lhsT=self.ONES[0:1, :], rhs=self.GTROW[0:1, i * D + hf * 512:i * D + (hf + 1) * 512], start=True, stop=True),
                     reads=[self.rGTROW, self.rCST], writes=[rpb])
                P.op("act", lambda e, i=i, hf=hf, pb=pb: e.activation(out=self.GTB[:, i, hf * 512:(hf + 1) * 512], in_=pb[:, :], func=AF.Copy),
                     reads=[rpb], writes=[self.rGTB])
        self.release("MG")

    def norm_phase(self, which, router=None):
        P = self.P
        gcol = self.GEFF[:, which * 8:(which + 1) * 8]
        shc = self.MODT[:, (0 if which == 0 else 24):(8 if which == 0 else 32)]
        SS, RS = self.SS[:, 0:16], self.SS[:, 16:32]
        for t in range(NT):
            P.op("act", lambda e, t=t: e.activation(out=self.JUNK[:], in_=self.X[:, t, :], func=AF.Square, accum_out=SS[:, t:t + 1]),
                 reads=[self.rX], writes=[self.rJUNK, self.rSS])
        import os
        NCUT = int(os.environ.get("NCUT", "99"))
        if NCUT <= 1:
            return
        P.op("act", lambda e: e.activation(out=RS, in_=SS, func=AF.Ln, scale=1.0 / D, bias=1e-6), reads=[self.rSS], writes=[self.rSS])
        P.op("act", lambda e: e.activation(out=RS, in_=RS, func=AF.Exp, scale=-0.5), reads=[self.rSS], writes=[self.rSS])
        if NCUT <= 2:
            return
        _x = [self.view("EX", 1024 + i * 4096, [128, D], F32, f"xn{i}") for i in range(4)]
        XNs = [a for a, _ in _x]
        rXN = [r_ for _, r_ in _x]
        if router is not None:
            H2F, rH2F = self.view("EX", 1024 + 16384, [128, KC, 512], F32, "h2f")
        for tg in range(4):
            for tt in range(4):
                t = tg * 4 + tt
                P.op("act", lambda e, t=t, tt=tt: e.activation(out=XNs[tt], in_=self.X[:, t, :], func=AF.Copy, scale=RS[:, t:t + 1]),
                     reads=[self.rX, self.rSS], writes=[rXN[tt]])
            if NCUT <= 3:
                continue
            for kc in range(KC):
                pb, rpb = self.PB[kc % 2], self.rPB[kc % 2]

                def f(e, kc=kc, pb=pb):
                    for tt in range(4):
                        ins = e.matmul(pb[:, tt * 128:(tt + 1) * 128], lhsT=XNs[tt][:, kc * 128:(kc + 1) * 128], rhs=self.IDF, start=True, stop=True)
                    return ins
                P.op("pe", f, reads=rXN + [self.rCST], writes=[rpb])
                if NCUT <= 4:
                    continue
                P.op("dve", lambda e, kc=kc, tg=tg, pb=pb: e.tensor_scalar(out=self.HT[:, kc, tg * 512:(tg + 1) * 512], in0=pb[:, :], scalar1=gcol[:, kc:kc + 1],
                                                                          scalar2=shc[:, kc:kc + 1], op0=ALU.mult, op1=ALU.add),
                     reads=[rpb, self.rGEFF, self.rMODT], writes=[self.rHT])
                if NCUT <= 5:
                    continue
                if router is not None:
                    P.op("dve", lambda e, kc=kc, pb=pb: e.tensor_scalar(out=H2F[:, kc, :], in0=pb[:, :], scalar1=gcol[:, kc:kc + 1],
                                                                        scalar2=shc[:, kc:kc + 1], op0=ALU.mult, op1=ALU.add),
                         reads=[rpb, self.rGEFF, self.rMODT], writes=[rH2F])
            if router is not None and NCUT > 6:
                router(tg, H2F, rH2F)

    def view(self, regname, off, shape, dt, name="v"):
        reg = {"EX": self.EX, "MG": self.MG, "ARENA": self.ARENA}[regname]
        n = 1
        for s in shape[1:]:
            n *= s
        size = n * (4 if dt == F32 else 2)
        if not hasattr(self, "_live"):
            self._live = {"EX": [], "MG": [], "ARENA": []}
        res = self.R(name)
        keep = []
        evs = []
        for (o, sz, r_) in self._live[regname]:
            if o < off + size and off < o + sz:
                evs.extend(r_.r)
                if r_.w is not None:
                    evs.append(r_.w)
            else:
                keep.append((o, sz, r_))
        base = {"EX": self.rEX, "MG": self.rMG, "ARENA": self.rX}[regname]
        evs.extend(base.r)
        if base.w is not None:
            evs.append(base.w)
        best = {}
        for e2 in evs:
            if e2[0] not in best or best[e2[0]][1] < e2[1]:
                best[e2[0]] = e2
        res.r = list(best.values())
        keep.append((off, size, res))
        self._live[regname] = keep
        return self.carve(reg, off, shape, dt), res

    def release(self, regname):
        base = {"EX": self.rEX, "MG": self.rMG, "ARENA": self.rX}[regname]
        evs = list(base.r)
        for (o, sz, r_) in getattr(self, "_live", {}).get(regname, []):
            evs.extend(r_.r)
            if r_.w is not None:
                evs.append(r_.w)
        best = {}
        for e2 in evs:
            if e2[0] not in best or best[e2[0]][1] < e2[1]:
                best[e2[0]] = e2
        base.r = list(best.values())
        if hasattr(self, "_live"):
            self._live[regname] = []

    def moe_phase(self, l):
        P = self.P
        K1 = 1024
        GATES, rGATES = self.view("EX", 33 * K1, [128, NT, E], F32, "gates")
        RWf, rRWf = self.view("EX", 35 * K1, [128, KC, E], F32, "rwf")
        RB, rRB = self.view("EX", 36 * K1, [128, E], F32, "rb")
        SM, rSM = self.view("EX", 36 * K1 + 128, [128, 96], F32, "sm")
        M8, rM8 = self.view("EX", 36 * K1 + 512, [128, 16], F32, "m8")
        B1C, rB1C = self.view("EX", 37 * K1, [128, E, 2, 8], F32, "b1c")
        B1L7, rB1L7 = self.view("EX", 39 * K1, [128, E, 8], F32, "b1l7")
        GTt, rGTt = self.view("MG", 0, [32, T], F32, "gtt")
        B2, rB2 = self.view("MG", 8 * K1, [32, D], F32, "b2")
        P.dma(RWf, self.router_w[l].rearrange("(kc p) e -> p kc e", p=128), writes=[rRWf])
        rb_src = self.router_b[l:l + 1, :].partition_broadcast(128)
        P.dma(RB, rb_src, writes=[rRB])
        P.dma(B1C, self.b1T[l], writes=[rB1C])
        P.dma(B2, self.moe_b2[l], writes=[rB2])
        P.op("dve", lambda e: e.tensor_scalar(out=B1L7, in0=B1C[:, :, 1, :], scalar1=7.0, scalar2=None, op0=ALU.add), reads=[rB1C], writes=[rB1L7])

        def router(tg, H2F, rH2F):
            for tt in range(4):
                t = tg * 4 + tt
                pb, rpb = self.PB[2], self.rPB[2]

                def f(e, tt=tt):
                    for kc in range(KC):
                        ins = e.matmul(pb[:, 0:E], lhsT=H2F[:, kc, tt * 128:(tt + 1) * 128], rhs=RWf[:, kc, :], start=(kc == 0), stop=(kc == KC - 1))
                    return ins
                P.op("pe", f, reads=[rH2F, rRWf], writes=[rpb])
                LG, EXPV, MASK = SM[:, 0:32], SM[:, 32:64], SM[:, 64:96]
                P.op("dve", lambda e: e.tensor_tensor(out=LG, in0=pb[:, 0:E], in1=RB, op=ALU.add), reads=[rpb, rRB], writes=[rSM])
                P.op("dve", lambda e: e.max(out=M8[:, 0:8], in_=LG), reads=[rSM], writes=[rM8])
                P.op("dve", lambda e: e.tensor_scalar(out=M8[:, 8:9], in0=M8[:, 0:1], scalar1=-1.0, scalar2=None, op0=ALU.mult), reads=[rM8], writes=[rM8])
                P.op("act", lambda e: e.activation(out=EXPV, in_=LG, func=AF.Exp, bias=M8[:, 8:9]), reads=[rSM, rM8], writes=[rSM])
                P.op("dve", lambda e: e.tensor_scalar(out=MASK, in0=LG, scalar1=M8[:, 3:4], scalar2=None, op0=ALU.is_ge), reads=[rSM, rM8], writes=[rSM])
                P.op("dve", lambda e: e.tensor_tensor(out=EXPV, in0=EXPV, in1=MASK, op=ALU.mult), reads=[rSM], writes=[rSM])
                P.op("dve", lambda e: e.tensor_reduce(out=M8[:, 9:10], in_=EXPV, axis=AX.X, op=ALU.add), reads=[rSM], writes=[rM8])
                P.op("dve", lambda e: e.reciprocal(out=M8[:, 10:11], in_=M8[:, 9:10]), reads=[rM8], writes=[rM8])
                P.op("dve", lambda e, t=t: e.tensor_scalar(out=GATES[:, t, :], in0=EXPV, scalar1=M8[:, 10:11], scalar2=None, op0=ALU.mult), reads=[rSM, rM8], writes=[rGATES])
                pb3, rpb3 = self.PB[3], self.rPB[3]
                P.op("pe", lambda e, t=t: e.matmul(pb3[0:E, 0:128], lhsT=GATES[:, t, :], rhs=self.IDF, start=True, stop=True), reads=[rGATES, self.rCST], writes=[rpb3])
                P.op("act", lambda e, t=t: e.activation(out=GTt[:, t * 128:(t + 1) * 128], in_=pb3[0:E, 0:128], func=AF.Copy), reads=[rpb3], writes=[rGTt])

        self.norm_phase(1, router=router)
        if getattr(self, "moe_stop", 0) == 1:
            self.dump("dbg_gates", GATES, rGATES, [128, NT, E])
            return

        GT2 = self.GTB[:, 1, :]
        TMPB, rTMPB = self.view("EX", 1 * K1, [128, 512], F32, "tmpb")
        for t in range(NT):
            for nb in range(2):
                pb, rpb = self.PB[4 + nb], self.rPB[4 + nb]
                P.op("pe", lambda e, t=t, nb=nb, pb=pb: e.matmul(pb[:, :], lhsT=GTt[:, t * 128:(t + 1) * 128], rhs=B2[:, nb * 512:(nb + 1) * 512], start=True, stop=True),
                     reads=[rGTt, rB2], writes=[rpb])
                P.op("dve", lambda e, nb=nb, pb=pb: e.tensor_tensor(out=TMPB, in0=pb[:, :], in1=GT2[:, nb * 512:(nb + 1) * 512], op=ALU.mult),
                     reads=[rpb, self.rGTB], writes=[rTMPB])
                P.op("pool", lambda e, t=t, nb=nb: e.tensor_tensor(out=self.X[:, t, nb * 512:(nb + 1) * 512], in0=self.X[:, t, nb * 512:(nb + 1) * 512], in1=TMPB, op=ALU.add),
                     reads=[rTMPB, self.rX], writes=[self.rX])

        if getattr(self, "moe_stop", 0) == 2:
            return
        GT2b, rGT2b = self.view("EX", 3 * K1, [128, D], BF16, "gt2b")
        P.op("act", lambda e: e.activation(out=GT2b, in_=GT2, func=AF.Copy), reads=[self.rGTB], writes=[rGT2b])
        W1 = []
        for i in range(2):
            W1.append(self.view("MG", i * 16 * K1, [128, KC, 2, 512], BF16, f"w1_{i}"))
        W2 = [(self.WB[i][:].rearrange("p a b -> p (a b)").rearrange("p (f d) -> p f d", f=4), self.rWB[i]) for i in range(2)]
        ACTT = [self.view("EX", (5 + 4 * i) * K1, [128, 4, 512], BF16, f"actt{i}") for i in range(3)]
        TMP = [[self.view("EX", (17 + 8 * s + 2 * j) * K1, [128, 512], F32, f"tmp{s}{j}") for j in range(4)] for s in range(2)]
        w1src = self.moe_w1[l].rearrange("e (kc p) g f -> e p kc g f", p=128)
        w2src = self.moe_w2[l].rearrange("e (fc p) d -> e p fc d", p=128)
        blocks = [(e_, hf, tb) for e_ in range(E) for hf in range(2) for tb in range(4)]

        def load(e_, hf):
            import os
            if os.environ.get("NOLOAD") and (e_ > 0 or hf > 0):
                return
            s = (e_ * 2 + hf) % 2
            w1, rw1 = W1[s]
            w2, rw2 = W2[s]
            for kh in range(2):
                for gl in range(2):
                    P.dma(w1[:, kh * 4:(kh + 1) * 4, gl, :], w1src[e_, :, kh * 4:(kh + 1) * 4, gl, hf * 512:(hf + 1) * 512], writes=[rw1], eng="pool")
            P.dma(w2, w2src[e_, :, hf * 4:(hf + 1) * 4, :], writes=[rw2], eng="pool")
            for fc in range(4):
                P.op("dve", lambda e, fc=fc, w2=w2: e.tensor_tensor(out=w2[:, fc, :], in0=w2[:, fc, :], in1=GT2b, op=ALU.mult),
                     reads=[rw2, rGT2b], writes=[rw2])

        def phase1(bi):
            e_, hf, tb = blocks[bi]
            s = (e_ * 2 + hf) % 2
            w1, rw1 = W1[s]
            at, rat = ACTT[bi % 3]
            for fc in range(4):
                q = (bi * 4 + fc) % 2
                pg, rpg = self.PB[2 * q], self.rPB[2 * q]
                pl, rpl = self.PB[2 * q + 1], self.rPB[2 * q + 1]
                (glu, rglu), (sig, rsig), (t1, rt1), (v, rv) = TMP[q]
                fcg = hf * 4 + fc

                def f(e, fc=fc, pg=pg, pl=pl, w1=w1, tb=tb):
                    for gl, pp in ((0, pg), (1, pl)):
                        for kc in range(KC):
                            ins = e.matmul(pp[:, :], lhsT=w1[:, kc, gl, fc * 128:(fc + 1) * 128], rhs=self.HT[:, kc, tb * 512:(tb + 1) * 512], start=(kc == 0), stop=(kc == KC - 1))
                    return ins
                P.op("pe", f, reads=[rw1, self.rHT], writes=[rpg, rpl])
                P.op("dve", lambda e, pg=pg, glu=glu, e_=e_, fcg=fcg: e.tensor_scalar(out=glu, in0=pg[:, :], scalar1=B1C[:, e_, 0, fcg:fcg + 1], scalar2=7.0, op0=ALU.add, op1=ALU.min),
                     reads=[rpg, rB1C], writes=[rglu])
                P.op("act", lambda e, pl=pl, t1=t1, e_=e_, fcg=fcg: e.activation(out=t1, in_=pl[:, :], func=AF.Relu, bias=B1L7[:, e_, fcg:fcg + 1]),
                     reads=[rpl, rB1L7], writes=[rt1])
                P.op("act", lambda e, glu=glu, sig=sig: e.activation(out=sig, in_=glu, func=AF.Silu, scale=1.702), reads=[rglu], writes=[rsig])
                P.op("dve", lambda e, t1=t1: e.tensor_scalar(out=t1, in0=t1, scalar1=14.0, scalar2=-6.0, op0=ALU.min, op1=ALU.add), reads=[rt1], writes=[rt1])
                P.op("dve", lambda e, sig=sig, t1=t1, at=at, fc=fc: e.scalar_tensor_tensor(out=at[:, fc, :], in0=sig, scalar=1.0 / 1.702, in1=t1, op0=ALU.mult, op1=ALU.mult),
                     reads=[rsig, rt1], writes=[rat])

        def phase2(bi):
            e_, hf, tb = blocks[bi]
            s = (e_ * 2 + hf) % 2
            w2, rw2 = W2[s]
            at, rat = ACTT[bi % 3]
            for tt in range(4):
                t = tb * 4 + tt
                for nb in range(2):
                    po, rpo = self.PB[4 + nb], self.rPB[4 + nb]

                    def f(e, tt=tt, nb=nb, po=po, at=at, w2=w2):
                        for fc in range(4):
                            ins = e.matmul(po[:, :], lhsT=at[:, fc, tt * 128:(tt + 1) * 128], rhs=w2[:, fc, nb * 512:(nb + 1) * 512], start=(fc == 0), stop=(fc == 3))
                        return ins
                    P.op("pe", f, reads=[rat, rw2], writes=[rpo])
                    P.op("dve", lambda e, t=t, nb=nb, po=po, e_=e_: e.scalar_tensor_tensor(out=self.X[:, t, nb * 512:(nb + 1) * 512], in0=po[:, :], scalar=GATES[:, t, e_:e_ + 1],
                                                                                       in1=self.X[:, t, nb * 512:(nb + 1) * 512], op0=ALU.mult, op1=ALU.add),
                         reads=[rpo, rGATES, self.rX], writes=[self.rX])

        nb_ = len(blocks)
        load(0, 0)
        for bi in range(nb_ + 1):
            if bi < nb_:
                phase1(bi)
            if bi >= 1:
                phase2(bi - 1)
            if bi < nb_:
                e_, hf, tb = blocks[bi]
                if tb == 0:
                    nxt = e_ * 2 + hf + 1
                    if nxt < 2 * E:
                        load(nxt // 2, nxt % 2)
        self.release("EX")
        self.release("MG")

    def final_phase(self):
        P = self.P
        GF, rGF = self.view("EX", 0, [128, D], F32, "gf")
        P.dma(GF, self.fng[0:1, :].partition_broadcast(128), writes=[rGF])
        SS, RS = self.SS[:, 0:16], self.SS[:, 16:32]
        for t in range(NT):
            P.op("act", lambda e, t=t: e.activation(out=self.JUNK[:], in_=self.X[:, t, :], func=AF.Square, accum_out=SS[:, t:t + 1]),
                 reads=[self.rX], writes=[self.rJUNK, self.rSS])
        P.op("act", lambda e: e.activation(out=RS, in_=SS, func=AF.Ln, scale=1.0 / D, bias=1e-6), reads=[self.rSS], writes=[self.rSS])
        P.op("act", lambda e: e.activation(out=RS, in_=RS, func=AF.Exp, scale=-0.5), reads=[self.rSS], writes=[self.rSS])
        OB = [self.view("EX", (4 + 4 * i) * 1024, [128, D], F32, f"ob{i}") for i in range(2)]
        yv = self.y_out.rearrange("(t p) d -> p t d", p=128)
        rY = self.R("y")
        for t in range(NT):
            ob, rob = OB[t % 2]
            P.op("dve", lambda e, t=t, ob=ob: e.scalar_tensor_tensor(out=ob, in0=self.X[:, t, :], scalar=RS[:, t:t + 1], in1=GF, op0=ALU.mult, op1=ALU.mult),
                 reads=[self.rX, self.rSS, rGF], writes=[rob])
            P.dma(yv[:, t, :], ob, reads=[rob], writes=[rY])
        self.outs.append(rY)

    def build(self):
        self.declare()
        self.alloc()
        self.prologue()
        if self.do_mixer:
            self.eb_build()
        for l in self.layers:
            self.mod_phase(l)
            if self.do_mixer:
                self.mixer_phase(l)
            if self.do_moe:
                self.moe_phase(l)
        self.final_phase()
        self.P.final_wait("sp", self.outs)
        self.P.emit(self.st)
        self.st.close()
        return self.nc


def prep_inputs(inp, b):
    f = lambda a: np.ascontiguousarray(a, dtype=np.float32)
    cst, EA, EC = _host_consts()
    m = {}
    m["x"] = f(inp["x"][b])
    m["cT"] = f(inp["c"][b].reshape(8, 128).T)
    m["w_mod"] = f(inp["w_mod"])
    m["bmodT"] = f(inp["b_mod"].reshape(L, 48, 128).transpose(0, 2, 1))
    m["bmod_row"] = f(inp["b_mod"].reshape(L, 1, 6 * D))
    m["ncol"] = f(np.concatenate([inp["norm1_g"].reshape(L, 8, 128).transpose(0, 2, 1), inp["norm2_g"].reshape(L, 8, 128).transpose(0, 2, 1)], axis=2))
    m["w_in"] = f(inp["w_in"])
    mu = inp["rwkv_mu"]
    mu_rkv = mu[:, :, :2304].reshape(L, 2, 36, 64)
    muT = mu_rkv.transpose(0, 3, 2, 1)
    m["muT"] = f(np.concatenate([muT, muT], axis=1))
    m["muL"] = f(mu[:, :, 2304:].reshape(L, 2, 3, 128).transpose(0, 3, 2, 1))
    rw = np.stack([inp["rwkv_w0"], inp["rwkv_a0"], inp["rwkv_k_k"], inp["rwkv_k_a"]], axis=-1)
    m["rwcol"] = f(rw.reshape(L, 2, 12, 64, 4).transpose(0, 1, 3, 2, 4).reshape(L, 128, 12, 4))
    rk = inp["rwkv_r_k"].transpose(0, 2, 1)
    m["rkcol"] = f(np.concatenate([rk, rk], axis=1))
    m["w_up"] = f(inp["rwkv_w_up"].reshape(L, 128, 768))
    m["a_up"] = f(inp["rwkv_a_up"].reshape(L, 128, 768))
    m["g_up"] = f(inp["rwkv_g_up"])
    m["ln_g"] = f(inp["rwkv_ln_g"])
    m["ln_b"] = f(inp["rwkv_ln_b"])
    m["dlam"] = f(inp["diff_lambda"].reshape(L, 256))
    m["subln"] = f(inp["diff_subln_g"].reshape(L, 128, 1))
    m["rel_bias"] = f(inp["rel_bias"])
    m["w_br_a"] = f(inp["w_branch_a"])
    m["w_br_b"] = f(inp["w_branch_b"])
    m["w_br_c"] = f(inp["w_branch_c"])
    m["w_out"] = f(inp["w_out"])
    m["router_w"] = f(inp["router_w"])
    m["router_b"] = f(inp["router_b"])
    w1 = inp["moe_w1"]
    m["moe_w1"] = f(np.stack([w1[..., 0::2], w1[..., 1::2]], axis=3))
    b1 = inp["moe_b1"].reshape(L, E, 8, 128, 2)
    m["b1T"] = f(b1.transpose(0, 3, 1, 4, 2))
    m["moe_w2"] = f(inp["moe_w2"])
    m["moe_b2"] = f(inp["moe_b2"])
    m["fng"] = f(inp["final_norm_g"].reshape(1, D))
    m["cst"], m["EA"], m["EC"] = cst, EA, EC
    return m


_SHARED = ("w_mod", "bmodT", "bmod_row", "ncol", "w_in", "muT", "muL", "rwcol", "rkcol", "w_up", "a_up", "g_up", "ln_g", "ln_b", "dlam",
           "subln", "rel_bias", "w_br_a", "w_br_b", "w_br_c", "w_out", "router_w", "router_b", "moe_w1", "b1T", "moe_w2", "moe_b2",
           "fng", "cst", "EA", "EC")


def kernel(**inp):
    nb = inp["x"].shape[0]
    nc = Builder().build()
    m0 = prep_inputs(inp, 0)
    in_maps = [m0]
    for b in range(1, nb):
        mb = dict(m0)
        mb["x"] = np.ascontiguousarray(inp["x"][b], dtype=np.float32)
        mb["cT"] = np.ascontiguousarray(inp["c"][b].reshape(8, 128).T, dtype=np.float32)
        in_maps.append(mb)
    res = run_bass_kernel_spmd(nc, in_maps, core_ids=list(range(nb)))
    return np.stack([r["y"] for r in res.results], axis=0).astype(np.float32)


def _rev(ap2, start, n):
    if start == 0:
        return ap2[:, n - 1::-1]
    return ap2[:, start + n - 1:start - 1:-1]


def _mixer_phase(self, l):
    P = self.P
    self.norm_phase(0)
    xs = self.x_spill.rearrange("(t p) d -> p t d", p=128)
    rSP = self.R("xspill")
    for q in range(4):
        P.dma(xs[:, q * 4:(q + 1) * 4, :], self.X[:, q * 4:(q + 1) * 4, :], reads=[self.rX], writes=[rSP])
    self.mg_first = True
    stages = getattr(self, "mix_stages", "bac")
    if "b" in stages:
        self.rwkv_phase(l)
    if "a" in stages:
        self.mixa_phase(l)
    if "c" in stages:
        self.mixc_phase(l)
    self.release("ARENA")
    for q in range(4):
        P.dma(self.X[:, q * 4:(q + 1) * 4, :], xs[:, q * 4:(q + 1) * 4, :], reads=[rSP], writes=[self.rX])
    self.wout_phase(l)
    self.release("EX")


def _load_w(self, src_ap, ncols, col_off=0, slot=None):
    if slot is None:
        slot = self.wbi
        self.wbi = (self.wbi + 1) % 2
    wb, rwb = self.WB[slot], self.rWB[slot]
    self.P.dma(wb[:, :, col_off:col_off + ncols], src_ap.rearrange("(kc p) n -> p kc n", p=128), writes=[rwb], eng="pool")
    return wb, rwb, slot


def _merge(self, l, branch, oT_fn, nk):
    P = self.P
    MGv = self.carve(self.MG, 0, [128, KC, T], BF16)
    SIG = [self.view("EX", (32 + i) * 1024, [128, 512], BF16, f"sig{i}") for i in range(2)]
    TMPM = [self.view("EX", (34 + i) * 1024, [128, 512], BF16, f"tmpm{i}") for i in range(2)]
    first = self.mg_first
    self.mg_first = False
    it = 0
    for dcb in range(2):
        gcol = G_OFF + branch * D + dcb * 512
        wg, rwg, _ = self.load_w(self.w_in[l][:, gcol:gcol + 512], 512)
        for dci in range(4):
            dc = dcb * 4 + dci
            for tb in range(4):
                pg, rpg = self.PB[(it % 2) * 2], self.rPB[(it % 2) * 2]
                pbr, rpbr = self.PB[(it % 2) * 2 + 1], self.rPB[(it % 2) * 2 + 1]
                sig, rsig = SIG[it % 2]
                tmp, rtmp = TMPM[it % 2]
                it += 1

                def fg(e, dci=dci, tb=tb, pg=pg, wg=wg):
                    for kc in range(KC):
                        ins = e.matmul(pg[:, :], lhsT=wg[:, kc, dci * 128:(dci + 1) * 128], rhs=self.HT[:, kc, tb * 512:(tb + 1) * 512], start=(kc == 0), stop=(kc == KC - 1))
                    return ins
                P.op("pe", fg, reads=[rwg, self.rHT], writes=[rpg])
                ress = []
                parts = [oT_fn(j) for j in range(nk)]
                for (_, _, rr) in parts:
                    ress.extend(rr)

                def fb(e, dc=dc, tb=tb, pbr=pbr, parts=parts):
                    for j, (wrow, oT, _) in enumerate(parts):
                        ins = e.matmul(pbr[:, :], lhsT=wrow[:, dc * 128:(dc + 1) * 128], rhs=oT[:, tb * 512:(tb + 1) * 512], start=(j == 0), stop=(j == len(parts) - 1))
                    return ins
                P.op("pe", fb, reads=ress, writes=[rpbr])
                P.op("act", lambda e, pg=pg, sig=sig: e.activation(out=sig, in_=pg[:, :], func=AF.Sigmoid), reads=[rpg], writes=[rsig])
                if first:
                    P.op("dve", lambda e, dc=dc, tb=tb, pbr=pbr, sig=sig: e.tensor_tensor(out=MGv[:, dc, tb * 512:(tb + 1) * 512], in0=pbr[:, :], in1=sig, op=ALU.mult),
                         reads=[rpbr, rsig], writes=[self.rMG])
                else:
                    P.op("dve", lambda e, pbr=pbr, sig=sig, tmp=tmp: e.tensor_tensor(out=tmp, in0=pbr[:, :], in1=sig, op=ALU.mult), reads=[rpbr, rsig], writes=[rtmp])
                    P.op("pool", lambda e, dc=dc, tb=tb, tmp=tmp: e.tensor_tensor(out=MGv[:, dc, tb * 512:(tb + 1) * 512], in0=MGv[:, dc, tb * 512:(tb + 1) * 512], in1=tmp, op=ALU.add),
                         reads=[rtmp, self.rMG], writes=[self.rMG])


def _wout_phase(self, l):
    P = self.P
    MGv = self.carve(self.MG, 0, [128, KC, T], BF16)
    GT1b, rGT1b = self.view("EX", 0, [128, D], BF16, "gt1b")
    P.op("act", lambda e: e.activation(out=GT1b, in_=self.GTB[:, 0, :], func=AF.Copy), reads=[self.rGTB], writes=[rGT1b])
    ws = []
    for nb in range(2):
        wb, rwb, s = self.load_w(self.w_out[l][:, nb * 512:(nb + 1) * 512], 512, slot=nb)
        for kc in range(KC):
            P.op("dve" if kc % 2 else "pool", lambda e, wb=wb, kc=kc, nb=nb: e.tensor_tensor(out=wb[:, kc, :], in0=wb[:, kc, :], in1=GT1b[:, nb * 512:(nb + 1) * 512], op=ALU.mult),
                 reads=[rwb, rGT1b], writes=[rwb])
        ws.append((wb, rwb))
    for t in range(NT):
        for nb in range(2):
            wb, rwb = ws[nb]
            po, rpo = self.PB[(t * 2 + nb) % 4], self.rPB[(t * 2 + nb) % 4]

            def f(e, t=t, wb=wb, po=po):
                for kc in range(KC):
                    ins = e.matmul(po[:, :], lhsT=MGv[:, kc, t * 128:(t + 1) * 128], rhs=wb[:, kc, :], start=(kc == 0), stop=(kc == KC - 1))
                return ins
            P.op("pe", f, reads=[self.rMG, rwb], writes=[rpo])
            P.op("dve", lambda e, t=t, nb=nb, po=po: e.tensor_tensor(out=self.X[:, t, nb * 512:(nb + 1) * 512], in0=po[:, :], in1=self.X[:, t, nb * 512:(nb + 1) * 512], op=ALU.add),
                 reads=[rpo, self.rX], writes=[self.rX])


def _eb_build(self):
    P = self.P
    RBA, rRBA = self.view("EX", 0, [33, 18], F32, "rba")
    EAs, rEAs = self.view("EX", 1024, [33, 1536], F32, "eas")
    ECs, rECs = self.view("EX", 1024 + 6144, [33, 1536], F32, "ecs")
    ROW, rROW = self.view("EX", 1024 + 12288, [12, 1536], F32, "row")
    P.op("dve", lambda e: e.memset(RBA[32:33, :], -200.0), writes=[rRBA])
    P.dma(RBA[0:32, :], self.rel_bias[:, :], writes=[rRBA])
    P.dma(EAs, self.EA_in[:, :], writes=[rEAs])
    P.dma(ECs, self.EC_in[:, :], writes=[rECs])
    rA, rC = self.R("ebA"), self.R("ebC")
    for g in range(3):
        pb, rpb = self.PB[g % 2], self.rPB[g % 2]
        P.op("pe", lambda e, g=g, pb=pb: e.matmul(pb[0:4, :], lhsT=RBA[:, g * 4:(g + 1) * 4], rhs=EAs[:, g * 512:(g + 1) * 512], start=True, stop=True), reads=[rRBA, rEAs], writes=[rpb])
        P.op("act", lambda e, g=g, pb=pb: e.activation(out=ROW[0:4, g * 512:(g + 1) * 512], in_=pb[0:4, :], func=AF.Exp), reads=[rpb], writes=[rROW])
        P.dma(self.ebA[g * 4:(g + 1) * 4, :], ROW[0:4, g * 512:(g + 1) * 512], reads=[rROW], writes=[rA])
    ROWC, rROWC = self.view("EX", 1024 + 12288 + 6144, [6, 1536], F32, "rowc")
    for j in range(3):
        pb, rpb = self.PB[2 + j % 2], self.rPB[2 + j % 2]
        P.op("pe", lambda e, j=j, pb=pb: e.matmul(pb[0:6, :], lhsT=RBA[:, 12:18], rhs=ECs[:, j * 512:(j + 1) * 512], start=True, stop=True), reads=[rRBA, rECs], writes=[rpb])
        P.op("act", lambda e, j=j, pb=pb: e.activation(out=ROWC[0:6, j * 512:(j + 1) * 512], in_=pb[0:6, :], func=AF.Exp), reads=[rpb], writes=[rROWC])
    P.dma(self.ebC[:, :], ROWC, reads=[rROWC], writes=[rC])
    self.rEBA, self.rEBC = rA, rC
    self.release("EX")


Builder.mixer_phase = _mixer_phase
Builder.load_w = _load_w
Builder.merge = _merge
Builder.wout_phase = _wout_phase
Builder.eb_build = _eb_build


def _mixa_phase(self, l):
    P = self.P
    K1 = 1024
    GRP = ((0, 1), (1, 4), (2, 16))
    QK, rQK = self.view("ARENA", 0, [128, 2, 3, T], BF16, "qk")
    VA, rVA = self.view("ARENA", 24 * K1, [128, 3, 16, 2, 65], BF16, "va")
    OACC, rOACC = self.view("ARENA", 37 * K1, [65, 2, T], F32, "oacc")
    EBA, rEBAs = self.view("ARENA", 53 * K1, [128, 12, 384], BF16, "eba")
    HST, rHST = self.view("ARENA", 62 * K1, [128, 384], F32, "hst")
    OAT, rOAT = self.view("EX", 0, [64, 4, T], BF16, "oat")
    RDEN, rRDEN = self.view("EX", 16 * K1, [65, T], F32, "rden")
    WBRA, rWBRA = self.view("EX", 24 * K1, [64, 4, D], BF16, "wbra")
    PT = [self.view("EX", 36 * K1 + i * 768, [128, 384], BF16, f"pt{i}") for i in range(3)]
    for idx in range(12):
        P.dma(HST, bass.AP(self.ebA.tensor, idx * 512, [[1, 128], [1, 384]]), reads=[self.rEBA], writes=[rHST])
        for c in range(3):
            P.op("dve", lambda e, idx=idx, c=c: e.tensor_copy(out=EBA[:, idx, c * 128:(c + 1) * 128], in_=_rev(HST, c * 128, 128)), reads=[rHST], writes=[rEBAs])
    P.dma(WBRA, self.w_br_a[l].rearrange("(h p) n -> p h n", p=64), writes=[rWBRA], eng="pool")
    inst = 0
    for hp in range(2):
        for qk in range(2):
            slot = None
            for g in range(3):
                col = A_OFF + qk * 768 + g * 256 + hp * 128
                wb, rwb, slot = self.load_w(self.w_in[l][:, col:col + 128], 128, col_off=g * 128, slot=slot)
            for g in range(3):
                for tb in range(4):
                    pb, rpb = self.PB[inst % 2], self.rPB[inst % 2]
                    inst += 1

                    def f(e, g=g, tb=tb, pb=pb, wb=wb):
                        for kc in range(KC):
                            ins = e.matmul(pb[:, :], lhsT=wb[:, kc, g * 128:(g + 1) * 128], rhs=self.HT[:, kc, tb * 512:(tb + 1) * 512], start=(kc == 0), stop=(kc == KC - 1))
                        return ins
                    P.op("pe", f, reads=[rwb, self.rHT], writes=[rpb])
                    if inst % 2:
                        P.op("act", lambda e, qk=qk, g=g, tb=tb, pb=pb: e.activation(out=QK[:, qk, g, tb * 512:(tb + 1) * 512], in_=pb[:, :], func=AF.Copy), reads=[rpb], writes=[rQK])
                    else:
                        P.op("dve", lambda e, qk=qk, g=g, tb=tb, pb=pb: e.tensor_copy(out=QK[:, qk, g, tb * 512:(tb + 1) * 512], in_=pb[:, :]), reads=[rpb], writes=[rQK])
        slot = None
        for g in range(3):
            col = A_OFF + 1536 + g * 256 + hp * 128
            wbv, rwbv, slot = self.load_w(self.w_in[l][:, col:col + 128], 128, col_off=g * 128, slot=slot)
        import os
        for g in range(3):
            if os.environ.get("NOMEMSET1") and hp == 1:
                continue
            P.op("pool", lambda e, g=g: e.memset(VA[:, g, :, :, 64:65], 1.0), writes=[rVA])
        for g, d in GRP:
            Lg = T // d
            nkt = Lg // 128
            for tq in range(4):
                pb, rpb = self.PB[inst % 2], self.rPB[inst % 2]
                inst += 1

                def f(e, g=g, d=d, tq=tq, pb=pb, nkt=nkt, wbv=wbv):
                    for ti in range(4):
                        tile_i = tq * 4 + ti
                        r_, kt = tile_i // nkt, tile_i % nkt
                        s0 = r_ + d * 128 * kt
                        for kc in range(KC):
                            ins = e.matmul(pb[:, ti * 128:(ti + 1) * 128], lhsT=self.HT[:, kc, s0:s0 + d * 127 + 1:d], rhs=wbv[:, kc, g * 128:(g + 1) * 128],
                                           start=(kc == 0), stop=(kc == KC - 1))
                    return ins
                P.op("pe", f, reads=[rwbv, self.rHT], writes=[rpb])
                P.op("dve", lambda e, g=g, tq=tq, pb=pb: e.tensor_copy(out=VA[:, g, tq * 4:(tq + 1) * 4, :, 0:64], in_=pb[:, :].rearrange("p (a b c) -> p a b c", a=4, b=2)),
                     reads=[rpb], writes=[rVA])
        if "tr" in self.dbg and hp == 1:
            self.dump("dbg_t1", OAT[:, 0:2, :], rOAT, [64, 2, T], BF16)
        for g, d in GRP:
            Lg = T // d
            nkt = Lg // 128
            for r_ in range(d):
                for qt in range(nkt):
                    q0 = r_ + d * 128 * qt
                    kts = [kt for kt in (qt - 1, qt, qt + 1) if 0 <= kt < nkt]
                    c0, c1 = kts[0] - qt + 1, kts[-1] - qt + 2
                    for hh in range(2):
                        hg = 2 * hp + hh
                        ps, rps = self.PB[2 + inst % 2], self.rPB[2 + inst % 2]
                        po, rpo = self.PB[4 + inst % 2], self.rPB[4 + inst % 2]
                        pt, rpt = PT[inst % 3]
                        inst += 1
                        rows = slice(hh * 64, (hh + 1) * 64)

                        def fs(e, g=g, d=d, r_=r_, qt=qt, kts=kts, ps=ps, rows=rows, q0=q0):
                            for kt in kts:
                                c = kt - qt + 1
                                k0 = r_ + d * 128 * kt
                                ins = e.matmul(ps[:, c * 128:(c + 1) * 128], lhsT=QK[rows, 1, g, k0:k0 + d * 127 + 1:d], rhs=QK[rows, 0, g, q0:q0 + d * 127 + 1:d], start=True, stop=True)
                            return ins
                        P.op("pe", fs, reads=[rQK], writes=[rps])
                        P.op("act", lambda e, ps=ps, pt=pt, c0=c0, c1=c1: e.activation(out=pt[:, c0 * 128:c1 * 128], in_=ps[:, c0 * 128:c1 * 128], func=AF.Exp, scale=0.125), reads=[rps], writes=[rpt])
                        P.op("pool", lambda e, pt=pt, c0=c0, c1=c1, g=g, hg=hg: e.tensor_tensor(out=pt[:, c0 * 128:c1 * 128], in0=pt[:, c0 * 128:c1 * 128], in1=EBA[:, g * 4 + hg, c0 * 128:c1 * 128], op=ALU.mult),
                             reads=[rpt, rEBAs], writes=[rpt])

                        def fo(e, g=g, r_=r_, qt=qt, kts=kts, po=po, pt=pt, hh=hh, nkt=nkt):
                            for i, kt in enumerate(kts):
                                c = kt - qt + 1
                                ins = e.matmul(po[0:65, 0:128], lhsT=VA[:, g, r_ * nkt + kt, hh, :], rhs=pt[:, c * 128:(c + 1) * 128], start=(i == 0), stop=(i == len(kts) - 1))
                            return ins
                        P.op("pe", fo, reads=[rVA, rpt], writes=[rpo])
                        dst = OACC[:, hh, q0:q0 + d * 127 + 1:d]
                        if g == 0:
                            P.op("dve", lambda e, dst=dst, po=po: e.tensor_copy(out=dst, in_=po[0:65, 0:128]), reads=[rpo], writes=[rOACC])
                        else:
                            P.op("dve", lambda e, dst=dst, po=po: e.tensor_tensor(out=dst, in0=dst, in1=po[0:65, 0:128], op=ALU.add), reads=[rpo, rOACC], writes=[rOACC])
        if "tr" in self.dbg and hp == 1:
            self.dump("dbg_t2", OAT[:, 0:2, :], rOAT, [64, 2, T], BF16)
        import os
        for hh in range(2):
            if os.environ.get("SKIPN1") and hp == 1:
                continue
            hg = 2 * hp + hh
            P.op("dve", lambda e, hh=hh: e.reciprocal(out=RDEN[64:65, :], in_=OACC[64:65, hh, :]), reads=[rOACC], writes=[rRDEN])
            for tb in range(4):
                pb, rpb = self.PB[inst % 2], self.rPB[inst % 2]
                inst += 1
                P.op("pe", lambda e, tb=tb, pb=pb: e.matmul(pb[0:64, :], lhsT=self.ONES[64:65, 0:64], rhs=RDEN[64:65, tb * 512:(tb + 1) * 512], start=True, stop=True),
                     reads=[rRDEN, self.rCST], writes=[rpb])
                P.op("dve", lambda e, tb=tb, pb=pb, hh=hh, hg=hg: e.tensor_tensor(out=OAT[:, hg, tb * 512:(tb + 1) * 512], in0=OACC[0:64, hh, tb * 512:(tb + 1) * 512], in1=pb[0:64, :], op=ALU.mult),
                     reads=[rpb, rOACC], writes=[rOAT])
        if "tr" in self.dbg and hp == 0:
            self.dump("dbg_t0", OAT[:, 0:2, :], rOAT, [64, 2, T], BF16)
            self.dump("dbg_oacc_t0", OACC, rOACC, [65, 2, T], F32)
            self.dump("dbg_va_t0", VA, rVA, [128, 3, 16, 2, 65], BF16)
            self.dump("dbg_rden_t0", RDEN[64:65, :], rRDEN, [1, T], F32)
        if "oa0" in self.dbg and hp == 0:
            self.dump("dbg_oacc0", OACC, rOACC, [65, 2, T], F32)
            self.dump("dbg_qk0", QK, rQK, [128, 2, 3, T], BF16)
            self.dump("dbg_va0", VA, rVA, [128, 3, 16, 2, 65], BF16)
            self.dump("dbg_oa", OAT, rOAT, [64, 4, T], BF16)
            break
    if "oa" in self.dbg:
        self.dump("dbg_oa", OAT, rOAT, [64, 4, T], BF16)
        self.dump("dbg_qk", QK, rQK, [128, 2, 3, T], BF16)
        self.dump("dbg_va", VA, rVA, [128, 3, 16, 2, 65], BF16)
        self.dump("dbg_oacc", OACC, rOACC, [65, 2, T], F32)
        self.dump("dbg_eba", EBA, rEBAs, [128, 12, 384], BF16)
    self.merge(l, 0, lambda j: (WBRA[:, j, :], OAT[:, j, :], [rWBRA, rOAT]), 4)


Builder.mixa_phase = _mixa_phase


def _mixc_phase(self, l):
    P = self.P
    K1 = 1024
    lambda_init = 0.8 - 0.6 * math.exp(-0.3 * l)
    OCT, rOCT = self.view("EX", 0, [128, 6, T], BF16, "oct")
    SMC, rSMC = self.view("EX", 24 * K1, [128, 512], F32, "smc")
    SC, rSC = self.view("EX", 26 * K1, [128, 64], F32, "sc")
    OF = [self.view("EX", 27 * K1 + i * 512, [128, 128], F32, f"of{i}") for i in range(2)]
    ON = [self.view("EX", 28 * K1 + i * 512, [128, 128], F32, f"on{i}") for i in range(2)]
    PT = [self.view("EX", (29 + i) * K1, [128, 512], BF16, f"ptc{i}") for i in range(3)]
    P.dma(SMC[:, 0:256], self.dlam[l:l + 1, :].partition_broadcast(128), writes=[rSMC])
    P.dma(SC[:, 0:1], self.subln[l], writes=[rSC])
    P.op("dve", lambda e: e.tensor_tensor(out=SMC[:, 256:320], in0=SMC[:, 0:64], in1=SMC[:, 64:128], op=ALU.mult), reads=[rSMC], writes=[rSMC])
    P.op("dve", lambda e: e.tensor_tensor(out=SMC[:, 320:384], in0=SMC[:, 128:192], in1=SMC[:, 192:256], op=ALU.mult), reads=[rSMC], writes=[rSMC])
    P.op("dve", lambda e: e.tensor_reduce(out=SC[:, 1:2], in_=SMC[:, 256:320], axis=AX.X, op=ALU.add), reads=[rSMC], writes=[rSC])
    P.op("dve", lambda e: e.tensor_reduce(out=SC[:, 2:3], in_=SMC[:, 320:384], axis=AX.X, op=ALU.add), reads=[rSMC], writes=[rSC])
    P.op("act", lambda e: e.activation(out=SC[:, 3:5], in_=SC[:, 1:3], func=AF.Exp), reads=[rSC], writes=[rSC])
    P.op("dve", lambda e: e.tensor_tensor(out=SC[:, 5:6], in0=SC[:, 4:5], in1=SC[:, 3:4], op=ALU.subtract), reads=[rSC], writes=[rSC])
    P.op("dve", lambda e: e.tensor_scalar(out=SC[:, 5:6], in0=SC[:, 5:6], scalar1=-lambda_init, scalar2=None, op0=ALU.add), reads=[rSC], writes=[rSC])
    P.op("dve", lambda e: e.tensor_scalar(out=SC[:, 6:7], in0=SC[:, 0:1], scalar1=1.0 - lambda_init, scalar2=None, op0=ALU.mult), reads=[rSC], writes=[rSC])
    NEGLAM, SUBC = SC[:, 5:6], SC[:, 6:7]
    inst = 0
    for ps_ in range(2):
        QC, rQC = self.view("ARENA", 0, [128, 3, T], BF16, "qc")
        KCc, rKC = self.view("ARENA", 12 * K1, [128, 3, T], BF16, "kc")
        VC, rVC = self.view("ARENA", 24 * K1, [128, 16, 3, 129], BF16, "vc")
        GREV, rGREV = self.view("ARENA", 37 * K1, [128, 3, 1408], BF16, "grev")
        HSTC, rHSTC = self.view("ARENA", 46 * K1, [128, 1408], F32, "hstc")
        for hi in range(3):
            h = ps_ * 3 + hi
            P.dma(HSTC, bass.AP(self.ebC.tensor, h * 1536, [[1, 128], [1, 1408]]), reads=[self.rEBC], writes=[rHSTC])
            P.op("dve", lambda e, hi=hi: e.tensor_copy(out=GREV[:, hi, :], in_=_rev(HSTC, 0, 1408)), reads=[rHSTC], writes=[rGREV])
        for qk, (dst, rdst) in enumerate(((QC, rQC), (KCc, rKC))):
            col = C_OFF + qk * 768 + ps_ * 384
            wb, rwb, _ = self.load_w(self.w_in[l][:, col:col + 384], 384)
            for hi in range(3):
                for tb in range(4):
                    pb, rpb = self.PB[inst % 2], self.rPB[inst % 2]
                    inst += 1

                    def f(e, hi=hi, tb=tb, pb=pb, wb=wb):
                        for kc in range(KC):
                            ins = e.matmul(pb[:, :], lhsT=wb[:, kc, hi * 128:(hi + 1) * 128], rhs=self.HT[:, kc, tb * 512:(tb + 1) * 512], start=(kc == 0), stop=(kc == KC - 1))
                        return ins
                    P.op("pe", f, reads=[rwb, self.rHT], writes=[rpb])
                    if inst % 2:
                        P.op("act", lambda e, dst=dst, hi=hi, tb=tb, pb=pb: e.activation(out=dst[:, hi, tb * 512:(tb + 1) * 512], in_=pb[:, :], func=AF.Copy), reads=[rpb], writes=[rdst])
                    else:
                        P.op("dve", lambda e, dst=dst, hi=hi, tb=tb, pb=pb: e.tensor_copy(out=dst[:, hi, tb * 512:(tb + 1) * 512], in_=pb[:, :]), reads=[rpb], writes=[rdst])
        col = C_OFF + 1536 + ps_ * 384
        wbv, rwbv, _ = self.load_w(self.w_in[l][:, col:col + 384], 384)
        P.op("pool", lambda e, VC=VC: e.memset(VC[:, :, :, 128:129], 1.0), writes=[rVC])
        for kt in range(16):
            pb, rpb = self.PB[inst % 2], self.rPB[inst % 2]
            inst += 1

            def f(e, kt=kt, pb=pb, wbv=wbv):
                for kc in range(KC):
                    ins = e.matmul(pb[:, 0:384], lhsT=self.HT[:, kc, kt * 128:(kt + 1) * 128], rhs=wbv[:, kc, 0:384], start=(kc == 0), stop=(kc == KC - 1))
                return ins
            P.op("pe", f, reads=[rwbv, self.rHT], writes=[rpb])
            P.op("dve", lambda e, kt=kt, pb=pb, VC=VC: e.tensor_copy(out=VC[:, kt, :, 0:128], in_=pb[:, 0:384].rearrange("p (a b) -> p a b", a=3)), reads=[rpb], writes=[rVC])
        ACC = [[self.PW[:, j * 256:j * 256 + 129] for j in range(4)],
               [self.PB[4 + j // 2][:, (j % 2) * 256:(j % 2) * 256 + 129] for j in range(4)]]
        rACC = [self.rPW, self.rPB[4], self.rPB[5]]
        for hi in range(3):
            h = ps_ * 3 + hi
            for qb in range(4):
                for kt in range(16):
                    delta = 128 * kt - 512 * qb
                    de = min(max(delta, -256), 640)
                    s0 = 640 - de
                    for c in range(2):
                        ps, rps = self.PB[2 + inst % 2], self.rPB[2 + inst % 2]
                        pt, rpt = PT[inst % 3]
                        inst += 1
                        rows = slice(c * 64, (c + 1) * 64)
                        P.op("pe", lambda e, ps=ps, rows=rows, hi=hi, kt=kt, qb=qb, KCc=KCc, QC=QC: e.matmul(ps[:, :], lhsT=KCc[rows, hi, kt * 128:(kt + 1) * 128], rhs=QC[rows, hi, qb * 512:(qb + 1) * 512], start=True, stop=True),
                             reads=[rQC, rKC], writes=[rps])
                        P.op("act", lambda e, ps=ps, pt=pt: e.activation(out=pt, in_=ps[:, :], func=AF.Exp, scale=0.125), reads=[rps], writes=[rpt])
                        P.op("pool" if inst % 2 else "dve", lambda e, pt=pt, hi=hi, s0=s0, GREV=GREV: e.tensor_tensor(out=pt, in0=pt, in1=GREV[:, hi, s0:s0 + 512], op=ALU.mult), reads=[rpt, rGREV], writes=[rpt])

                        def fo(e, c=c, kt=kt, hi=hi, pt=pt, VC=VC):
                            for j in range(4):
                                ins = e.matmul(ACC[c][j], lhsT=pt[:, j * 128:(j + 1) * 128], rhs=VC[:, kt, hi, :], start=(kt == 0 and j % 2 == 0), stop=(kt == 15 and j % 2 == 1))
                            return ins
                        P.op("pe", fo, reads=[rpt, rVC], writes=[rACC[0]] if c == 0 else [rACC[1], rACC[2]])
                R0, R1 = SC[:, 8:12], SC[:, 12:16]
                P.op("dve", lambda e: e.reciprocal(out=R0, in_=self.PW[:, 128:1024:256]), reads=[rACC[0]], writes=[rSC])
                for j in range(4):
                    P.op("dve", lambda e, j=j: e.reciprocal(out=SC[:, 12 + j:13 + j], in_=ACC[1][j][:, 128:129]), reads=[rACC[1], rACC[2]], writes=[rSC])
                P.op("dve", lambda e: e.tensor_scalar(out=R1, in0=R1, scalar1=NEGLAM, scalar2=None, op0=ALU.mult), reads=[rSC], writes=[rSC])
                for j in range(4):
                    t = qb * 4 + j
                    of, rof = OF[j % 2]
                    on, ron = ON[j % 2]
                    P.op("dve", lambda e, j=j, of=of: e.tensor_scalar(out=of, in0=ACC[0][j][:, 0:128], scalar1=SC[:, 8 + j:9 + j], scalar2=None, op0=ALU.mult), reads=[rACC[0], rSC], writes=[rof])
                    P.op("dve", lambda e, j=j, of=of: e.scalar_tensor_tensor(out=of, in0=ACC[1][j][:, 0:128], scalar=SC[:, 12 + j:13 + j], in1=of, op0=ALU.mult, op1=ALU.add),
                         reads=[rACC[1], rACC[2], rSC, rof], writes=[rof])
                    P.op("act", lambda e, j=j, of=of, on=on: e.activation(out=on, in_=of, func=AF.Square, accum_out=SC[:, 16 + j:17 + j]), reads=[rof], writes=[ron, rSC])
                    P.op("act", lambda e, j=j: e.activation(out=SC[:, 20 + j:21 + j], in_=SC[:, 16 + j:17 + j], func=AF.Ln, scale=1.0 / 128, bias=1e-5), reads=[rSC], writes=[rSC])
                    P.op("act", lambda e, j=j: e.activation(out=SC[:, 20 + j:21 + j], in_=SC[:, 20 + j:21 + j], func=AF.Exp, scale=-0.5), reads=[rSC], writes=[rSC])
                    P.op("act", lambda e, j=j, of=of, on=on: e.activation(out=on, in_=of, func=AF.Copy, scale=SC[:, 20 + j:21 + j]), reads=[rof, rSC], writes=[ron])
                    pbt, rpbt = self.PB[inst % 2], self.rPB[inst % 2]
                    inst += 1
                    P.op("pe", lambda e, on=on, pbt=pbt: e.matmul(pbt[:, 0:128], lhsT=on, rhs=self.IDF, start=True, stop=True), reads=[ron, self.rCST], writes=[rpbt])
                    P.op("dve", lambda e, h=h, t=t, pbt=pbt: e.tensor_scalar(out=OCT[:, h, t * 128:(t + 1) * 128], in0=pbt[:, 0:128], scalar1=SUBC, scalar2=None, op0=ALU.mult),
                         reads=[rpbt, rSC], writes=[rOCT])
    if "oc" in self.dbg:
        self.dump("dbg_oc", OCT, rOCT, [128, 6, T], BF16)
    WBRC, rWBRC = self.view("ARENA", 0, [128, 6, D], BF16, "wbrc")
    P.dma(WBRC, self.w_br_c[l].rearrange("(j p) n -> p j n", p=128), writes=[rWBRC], eng="pool")
    self.merge(l, 2, lambda j: (WBRC[:, j, :], OCT[:, j, :], [rWBRC, rOCT]), 6)


Builder.mixc_phase = _mixc_phase


def _rwkv_phase(self, l):
    P = self.P
    K1 = 1024
    CDEC = DECAY_C
    SL = 8 * K1
    def A(i, shape=(128, T), dt=F32, name="a"):
        return self.view("ARENA", i * SL, list(shape), dt, f"{name}{i}")
    OBT, rOBT = self.view("EX", 0, [128, 6, T], BF16, "obt")
    YH, rYH = self.view("EX", 24 * K1, [128, 16, 64], F32, "yh")
    OBK, rOBK = self.view("EX", 28 * K1, [128, 16, 128], BF16, "obk")
    BON, rBON = self.view("EX", 32 * K1, [128, 16, 12], F32, "bon")
    GUP, rGUP = self.view("EX", 33 * K1, [128, 768], BF16, "gup")
    BDW, rBDW = self.view("EX", 34 * K1 + 512, [128, 128], BF16, "bdw")
    BDA, rBDA = self.view("EX", 34 * K1 + 768, [128, 128], BF16, "bda")
    BONES, rBONES = self.view("EX", 35 * K1, [128, 128], F32, "bones")
    o = 35 * K1 + 512
    RWC, rRWC = self.view("EX", o, [128, 12, 4], F32, "rwc"); o += 192
    RKC, rRKC = self.view("EX", o, [128, 12], F32, "rkc"); o += 48
    OMKA, rOMKA = self.view("EX", o, [128, 12], F32, "omka"); o += 48
    MUT, rMUT = self.view("EX", o, [128, 36, 2], F32, "mut"); o += 288
    C0T, rC0T = self.view("EX", o, [128, 36], F32, "c0t"); o += 144
    MUL, rMUL = self.view("EX", o, [128, 3, 2], F32, "mul"); o += 24
    C0L, rC0L = self.view("EX", o, [128, 4], F32, "c0l"); o += 16
    PCS, rPCS = self.view("EX", o, [64, 2, 16], F32, "pcs"); o += 128
    ST, rST0 = self.view("EX", o, [64, 2, 64], F32, "st"); o += 512
    GNS, rGNS = self.view("EX", o, [128, 64], F32, "gns"); o += 256
    YC, rYC = self.view("EX", o, [128, 64], F32, "yc"); o += 256
    YB, rYB = self.view("EX", o, [128, 64], F32, "yb"); o += 256
    LNG, rLNG = self.view("EX", o, [128, 64], F32, "lng"); o += 256
    LNB, rLNB = self.view("EX", o, [128, 64], F32, "lnb"); o += 256
    assert o <= 40 * K1
    rST = [rST0, self.R("st1")]
    RAW, rRAW = self.view("MG", 0, [128, T + 2], F32, "raw")
    BT, rBT = self.view("MG", 8 * K1 + 512, [128, T], F32, "bt")
    RKD, rRKD = self.view("MG", 16 * K1 + 512, [128, T], F32, "rkd")
    SGD, rSGD = self.view("MG", 24 * K1 + 512, [128, T], BF16, "sgd")
    WUP, rWUP = self.view("MG", 28 * K1 + 512, [128, 768], BF16, "wup")
    AUP, rAUP = self.view("MG", 30 * K1, [128, 768], BF16, "aup")
    TW = self.WB[1][:].rearrange("p a b -> p (a b)")[:, 0:T]
    AD = self.WB[1][:].rearrange("p a b -> p (a b)")[:, T:2 * T]
    rTW = self.rWB[1]
    WB0, rWB0 = self.WB[0], self.rWB[0]
    IDF, ONES = self.IDF, self.ONES

    P.dma(RWC, self.rwcol[l], writes=[rRWC])
    P.dma(RKC, self.rkcol[l], writes=[rRKC])
    P.dma(MUT, self.muT[l], writes=[rMUT])
    P.dma(MUL, self.muL[l], writes=[rMUL])
    P.dma(WUP, self.w_up[l], writes=[rWUP], eng="pool")
    P.dma(AUP, self.a_up[l], writes=[rAUP], eng="pool")
    P.dma(GUP, self.g_up[l], writes=[rGUP], eng="pool")
    P.op("dve", lambda e: e.tensor_scalar(out=OMKA, in0=RWC[:, :, 3], scalar1=-1.0, scalar2=1.0, op0=ALU.mult, op1=ALU.add), reads=[rRWC], writes=[rOMKA])
    P.op("dve", lambda e: e.tensor_tensor(out=C0T, in0=MUT[:, :, 0], in1=MUT[:, :, 1], op=ALU.add), reads=[rMUT], writes=[rC0T])
    P.op("dve", lambda e: e.tensor_scalar(out=C0T, in0=C0T, scalar1=-1.0, scalar2=1.0, op0=ALU.mult, op1=ALU.add), reads=[rC0T], writes=[rC0T])
    P.op("dve", lambda e: e.tensor_tensor(out=C0L[:, 0:3], in0=MUL[:, :, 0], in1=MUL[:, :, 1], op=ALU.add), reads=[rMUL], writes=[rC0L])
    P.op("dve", lambda e: e.tensor_scalar(out=C0L[:, 0:3], in0=C0L[:, 0:3], scalar1=-1.0, scalar2=1.0, op0=ALU.mult, op1=ALU.add), reads=[rC0L], writes=[rC0L])
    P.op("dve", lambda e: e.memset(BONES, 0.0), writes=[rBONES])
    P.op("dve", lambda e: e.memset(BONES[0:64, 0:64], 1.0), writes=[rBONES])
    P.op("dve", lambda e: e.memset(BONES[64:128, 64:128], 1.0), writes=[rBONES])
    P.op("dve", lambda e: e.memset(BDW, 0.0), writes=[rBDW])
    P.op("dve", lambda e: e.memset(BDA, 0.0), writes=[rBDA])
    P.op("dve", lambda e: e.memset(RAW[:, 0:1], 0.0), writes=[rRAW])
    P.op("dve", lambda e: e.memset(RAW[:, T + 1:T + 2], 0.0), writes=[rRAW])
    P.op("dve", lambda e: e.memset(BON, 0.0), writes=[rBON])
    cnt = [0]

    def proj_shift(wb, rwb, wcols, mu0, mu1, c0, rmu, dst, rdst, post):
        for tb in range(4):
            pb, rpb = self.PB[cnt[0] % 2], self.rPB[cnt[0] % 2]
            cnt[0] += 1

            def f(e, tb=tb, pb=pb, wb=wb, wcols=wcols):
                for kc in range(KC):
                    ins = e.matmul(pb[:, :], lhsT=wb[:, kc, wcols], rhs=self.HT[:, kc, tb * 512:(tb + 1) * 512], start=(kc == 0), stop=(kc == KC - 1))
                return ins
            P.op("pe", f, reads=[rwb, self.rHT], writes=[rpb])
            P.op("act", lambda e, tb=tb, pb=pb: e.activation(out=RAW[:, 1 + tb * 512:1 + (tb + 1) * 512], in_=pb[:, :], func=AF.Copy), reads=[rpb], writes=[rRAW])
        post(mu0, mu1, c0, rmu, dst, rdst)

    def shift_to(mu0, mu1, c0, rmu, dst, rdst):
        P.op("dve", lambda e: e.tensor_scalar(out=dst, in0=RAW[:, 1:T + 1], scalar1=c0, scalar2=None, op0=ALU.mult), reads=[rRAW] + rmu, writes=[rdst])
        P.op("dve", lambda e: e.scalar_tensor_tensor(out=dst, in0=RAW[:, 0:T], scalar=mu0, in1=dst, op0=ALU.mult, op1=ALU.add), reads=[rRAW, rdst] + rmu, writes=[rdst])
        P.op("dve", lambda e: e.scalar_tensor_tensor(out=dst, in0=RAW[:, 2:T + 2], scalar=mu1, in1=dst, op0=ALU.mult, op1=ALU.add), reads=[rRAW, rdst] + rmu, writes=[rdst])

    TMPL, rTMPL = A(7, name="tmpl")
    wsrc = self.w_in[l]
    self.P.dma(WB0[:, :, 0:384], wsrc[:, B_OFF + 2304:B_OFF + 2688].rearrange("(kc p) n -> p kc n", p=128), writes=[rWB0], eng="pool")
    for q, (dstv, rdstv, fn_) in enumerate(((TW, rTW, AF.Tanh), (AD, rTW, AF.Copy), (SGD, rSGD, AF.Sigmoid))):
        def post(mu0, mu1, c0, rmu, dst, rdst, dstv=dstv, rdstv=rdstv, fn_=fn_):
            shift_to(mu0, mu1, c0, rmu, dst, rdst)
            P.op("act", lambda e: e.activation(out=dstv, in_=dst, func=fn_), reads=[rdst], writes=[rdstv])
        proj_shift(WB0, rWB0, slice(q * 128, (q + 1) * 128), MUL[:, q, 0:1], MUL[:, q, 1:2], C0L[:, q:q + 1], [rMUL, rC0L], TMPL, rTMPL, post)

    MASK = [self.CST[:, 128:768], self.CST[:, 768:1408]]
    def do_head(h):
        Rr, rR = A(0, name="r")
        Kk, rK = A(1, name="k")
        Vv, rV = A(2, name="v")
        SG, rSG = A(3, name="sg")
        CS, rCS = A(4, name="cs")
        Pp, rPp = A(5, name="p")
        AAa, rAA = A(6, name="aa")
        KK, rKK = A(7, name="kk")
        for w_ in range(3):
            col = B_OFF + w_ * 768 + h * 64
            for dup in range(2):
                P.dma(WB0[:, :, w_ * 128 + dup * 64:w_ * 128 + (dup + 1) * 64], wsrc[:, col:col + 64].rearrange("(kc p) n -> p kc n", p=128), writes=[rWB0], eng="pool")
        for w_, (dst, rdst) in enumerate(((Rr, rR), (Kk, rK), (Vv, rV))):
            ci = w_ * 12 + h
            proj_shift(WB0, rWB0, slice(w_ * 128, (w_ + 1) * 128), MUT[:, ci, 0:1], MUT[:, ci, 1:2], C0T[:, ci:ci + 1], [rMUT, rC0T], dst, rdst, shift_to)
        hc = slice(h * 64, (h + 1) * 64)
        for (BD, rBD, UP, rUP) in ((BDW, rBDW, WUP, rWUP), (BDA, rBDA, AUP, rAUP)):
            P.op("pool", lambda e, BD=BD, UP=UP: e.tensor_copy(out=BD[0:64, 0:64], in_=UP[0:64, hc]), reads=[rUP], writes=[rBD])
            P.op("pool", lambda e, BD=BD, UP=UP: e.tensor_copy(out=BD[64:128, 64:128], in_=UP[64:128, hc]), reads=[rUP], writes=[rBD])
        for (BD, rBD, src, dst, rdst, bcol) in ((BDW, rBDW, TW, SG, rSG, 0), (BDA, rBDA, AD, AAa, rAA, 1)):
            for tb in range(4):
                pb, rpb = self.PB[cnt[0] % 2], self.rPB[cnt[0] % 2]
                cnt[0] += 1
                P.op("pe", lambda e, pb=pb, BD=BD, src=src, tb=tb: e.matmul(pb[:, :], lhsT=BD, rhs=src[:, tb * 512:(tb + 1) * 512], start=True, stop=True), reads=[rBD, rTW], writes=[rpb])
                P.op("act", lambda e, pb=pb, dst=dst, tb=tb, bcol=bcol: e.activation(out=dst[:, tb * 512:(tb + 1) * 512], in_=pb[:, :], func=AF.Sigmoid, bias=RWC[:, h, bcol:bcol + 1]),
                     reads=[rpb, rRWC], writes=[rdst])
        for t in range(NT):
            c0_, c1_ = t * 128, (t + 1) * 128
            P.op("dve", lambda e, c0_=c0_, c1_=c1_: e.tensor_tensor_scan(out=CS[0:64, c0_:c1_], data0=ONES[0:64, :], data1=SG[0:64, c0_:c1_], initial=0.0, op0=ALU.mult, op1=ALU.add),
                 reads=[rSG, self.rCST], writes=[rCS])
            P.op("dve", lambda e, c0_=c0_: e.tensor_tensor_scan(out=_rev(CS[64:128, :], c0_, 128), data0=ONES[64:128, :], data1=_rev(SG[64:128, :], c0_, 128), initial=0.0, op0=ALU.mult, op1=ALU.add),
                 reads=[rSG, self.rCST], writes=[rCS])
        P.op("dve", lambda e: e.tensor_tensor(out=SG, in0=CS, in1=SG, op=ALU.subtract), reads=[rCS, rSG], writes=[rSG])
        P.op("act", lambda e: e.activation(out=SG, in_=SG, func=AF.Exp, scale=-CDEC), reads=[rSG], writes=[rSG])
        P.op("act", lambda e: e.activation(out=Pp, in_=CS, func=AF.Exp, scale=-CDEC), reads=[rCS], writes=[rPp])
        P.op("act", lambda e: e.activation(out=CS, in_=CS, func=AF.Exp, scale=CDEC), reads=[rCS], writes=[rCS])
        PPv, PI = SG, CS
        P.op("dve", lambda e: e.tensor_copy(out=PCS[:, 0, :], in_=Pp[0:64, 127:T:128]), reads=[rPp], writes=[rPCS])
        pb, rpb = self.PB[cnt[0] % 2], self.rPB[cnt[0] % 2]
        cnt[0] += 1
        P.op("pe", lambda e, pb=pb: e.matmul(pb[0:64, 0:16], lhsT=IDF[64:128, 64:128], rhs=Pp[64:128, 0:T:128], start=True, stop=True), reads=[rPp, self.rCST], writes=[rpb])
        P.op("dve", lambda e, pb=pb: e.tensor_copy(out=PCS[:, 1, :], in_=pb[0:64, 0:16]), reads=[rpb], writes=[rPCS])
        P.op("dve", lambda e: e.tensor_scalar(out=KK, in0=Kk, scalar1=RWC[:, h, 2:3], scalar2=None, op0=ALU.mult), reads=[rK, rRWC], writes=[rKK])
        P.op("pool", lambda e: e.tensor_tensor(out=RKD, in0=KK, in1=KK, op=ALU.mult), reads=[rKK], writes=[rRKD])
        for tb in range(4):
            pb, rpb = self.PB[cnt[0] % 2], self.rPB[cnt[0] % 2]
            cnt[0] += 1
            P.op("pe", lambda e, pb=pb, tb=tb: e.matmul(pb[:, :], lhsT=BONES, rhs=RKD[:, tb * 512:(tb + 1) * 512], start=True, stop=True), reads=[rBONES, rRKD], writes=[rpb])
            P.op("dve", lambda e, pb=pb, tb=tb: e.tensor_scalar(out=BT[:, tb * 512:(tb + 1) * 512], in0=pb[:, :], scalar1=1e-24, scalar2=None, op0=ALU.max), reads=[rpb], writes=[rBT])
        P.op("act", lambda e: e.activation(out=BT, in_=BT, func=AF.Ln), reads=[rBT], writes=[rBT])
        P.op("act", lambda e: e.activation(out=BT, in_=BT, func=AF.Exp, scale=-0.5), reads=[rBT], writes=[rBT])
        P.op("dve", lambda e: e.tensor_tensor(out=KK, in0=KK, in1=BT, op=ALU.mult), reads=[rKK, rBT], writes=[rKK])
        AT = PPv
        P.op("dve", lambda e: e.scalar_tensor_tensor(out=AT, in0=KK, scalar=-1.0, in1=PPv, op0=ALU.mult, op1=ALU.mult), reads=[rKK, rSG], writes=[rSG])
        P.op("pool", lambda e: e.tensor_tensor(out=BT, in0=KK, in1=AAa, op=ALU.mult), reads=[rKK, rAA], writes=[rBT])
        P.op("dve", lambda e: e.tensor_tensor(out=BT, in0=BT, in1=PI, op=ALU.mult), reads=[rBT, rCS], writes=[rBT])
        P.op("dve", lambda e: e.tensor_scalar(out=AAa, in0=AAa, scalar1=RWC[:, h, 3:4], scalar2=OMKA[:, h:h + 1], op0=ALU.mult, op1=ALU.add), reads=[rAA, rRWC, rOMKA], writes=[rAA])
        P.op("pool", lambda e: e.tensor_tensor(out=AAa, in0=AAa, in1=Kk, op=ALU.mult), reads=[rAA, rK], writes=[rAA])
        P.op("dve", lambda e: e.tensor_tensor(out=RKD, in0=Rr, in1=AAa, op=ALU.mult), reads=[rR, rAA], writes=[rRKD])
        P.op("pool", lambda e: e.tensor_tensor(out=AAa, in0=AAa, in1=PI, op=ALU.mult), reads=[rAA, rCS], writes=[rAA])
        P.op("dve", lambda e: e.tensor_tensor(out=Rr, in0=Rr, in1=Pp, op=ALU.mult), reads=[rR, rPp], writes=[rR])
        KT, RT = AAa, Rr
        rAT, rKT, rRT = rSG, rAA, rR
        pbb, rpbb = self.PB[cnt[0] % 2], self.rPB[cnt[0] % 2]
        cnt[0] += 1

        def fbon(e, pbb=pbb):
            for t in range(NT):
                ins = e.matmul(pbb[:, t:t + 1], lhsT=RKD[:, t * 128:(t + 1) * 128], rhs=RKC[:, h:h + 1], start=True, stop=True)
            return ins
        P.op("pe", fbon, reads=[rRKD, rRKC], writes=[rpbb])
        P.op("dve", lambda e, pbb=pbb: e.tensor_copy(out=BON[:, :, h], in_=pbb[:, 0:16]), reads=[rpbb], writes=[rBON])
        IB = []
        for d_ in range(2):
            base = (4 + d_) * SL
            AM = self.view("ARENA", base, [128, 640], F32, f"am{d_}")
            LV = self.view("ARENA", base + 2560, [128, 384], F32, f"lv{d_}")
            TK = self.view("ARENA", base + 4096, [128, 256], F32, f"tk{d_}")
            AV = self.view("ARENA", base + 5120, [128, 64], F32, f"av{d_}")
            XU = self.view("ARENA", base + 5376, [128, 128], F32, f"xu{d_}")
            RN = self.view("ARENA", base + 5888, [64, 192], F32, f"rn{d_}")
            IB.append((AM, LV, TK, AV, XU, RN))
        P.op("dve", lambda e: e.memset(ST, 0.0), writes=[rST[0], rST[1]])
        P.op("pool", lambda e: e.memset(YH, 0.0), writes=[rYH])
        for step in range(NT):
            tiles = (step, NT - 1 - step)
            for d_ in range(2):
                (AM, rAM), (LV, rLV), (TK, rTK), (AV, rAV), (XU, rXU), (RN, rRN) = IB[d_]
                ti = tiles[d_]
                tc_ = slice(ti * 128, (ti + 1) * 128)
                rows = slice(d_ * 64, (d_ + 1) * 64)
                px, rpx = self.PB[2 + d_], self.rPB[2 + d_]
                pd, rpd = self.PB[4 + d_], self.rPB[4 + d_]
                pm = self.PW[:, d_ * 512:(d_ + 1) * 512]
                rpm = self.rPW if d_ == 0 else self.rPB[0]
                if d_ == 1:
                    pm = self.PB[0]
                idb = IDF[rows, rows]

                def fa(e, px=px, pd=pd, rows=rows, tc_=tc_):
                    e.matmul(px[:, 0:128], lhsT=BT[rows, tc_], rhs=AT[rows, tc_], start=True, stop=True)
                    e.matmul(px[:, 128:256], lhsT=BT[rows, tc_], rhs=RT[rows, tc_], start=True, stop=True)
                    e.matmul(px[:, 256:384], lhsT=KT[rows, tc_], rhs=AT[rows, tc_], start=True, stop=True)
                    e.matmul(px[:, 384:512], lhsT=KT[rows, tc_], rhs=RT[rows, tc_], start=True, stop=True)
                    return e.matmul(pd[:, 0:128], lhsT=AT[rows, tc_], rhs=BT[rows, tc_], start=True, stop=True)
                P.op("pe", fa, reads=[rBT, rAT, rKT, rRT], writes=[rpx, rpd])
                P.op("dve", lambda e, AM=AM, px=px, d_=d_: e.tensor_tensor(out=AM[:, 0:512], in0=px[:, :], in1=MASK[d_][:, 0:512], op=ALU.mult), reads=[rpx, self.rCST], writes=[rAM])
                P.op("dve", lambda e, LV=LV, pd=pd, d_=d_: e.tensor_tensor(out=LV[:, 0:128], in0=pd[:, 0:128], in1=MASK[d_][:, 512:640], op=ALU.mult), reads=[rpd, self.rCST], writes=[rLV])
                P.op("act", lambda e, LV=LV, AM=AM: e.activation(out=LV[:, 128:256], in_=AM[:, 0:128], func=AF.Copy), reads=[rAM], writes=[rLV])
                P.op("pool", lambda e, LV=LV, AM=AM: e.tensor_tensor(out=LV[:, 256:384], in0=AM[:, 0:128], in1=IDF, op=ALU.add), reads=[rAM, self.rCST], writes=[rLV])
                def ft(e, pm=pm, rows=rows, tc_=tc_, idb=idb):
                    e.matmul(pm[:, 0:64], lhsT=AT[rows, tc_], rhs=idb, start=True, stop=True)
                    e.matmul(pm[:, 64:128], lhsT=BT[rows, tc_], rhs=idb, start=True, stop=True)
                    e.matmul(pm[:, 128:192], lhsT=KT[rows, tc_], rhs=idb, start=True, stop=True)
                    return e.matmul(pm[:, 192:256], lhsT=Vv[rows, tc_], rhs=idb, start=True, stop=True)
                P.op("pe", ft, reads=[rBT, rAT, rKT, rV, self.rCST], writes=[rpm])
                P.op("act", lambda e, TK=TK, pm=pm: e.activation(out=TK, in_=pm[:, 0:256], func=AF.Copy), reads=[rpm], writes=[rTK])
                P.op("pe", lambda e, pm=pm, AM=AM, TK=TK: e.matmul(pm[:, 256:320], lhsT=AM[:, 256:384], rhs=TK[:, 192:256], start=True, stop=True), reads=[rAM, rTK], writes=[rpm])
                P.op("act", lambda e, AV=AV, pm=pm: e.activation(out=AV, in_=pm[:, 256:320], func=AF.Copy), reads=[rpm], writes=[rAV])
            for k in range(7):
                for d_ in range(2):
                    (AM, rAM), (LV, rLV), (TK, rTK), (AV, rAV), (XU, rXU), (RN, rRN) = IB[d_]
                    pd, rpd = self.PB[4 + d_], self.rPB[4 + d_]
                    if k == 0:
                        def f0(e, pd=pd, LV=LV):
                            e.matmul(pd[:, 0:128], lhsT=LV[:, 128:256], rhs=LV[:, 0:128], start=True, stop=True)
                            return e.matmul(pd[:, 128:256], lhsT=LV[:, 0:128], rhs=LV[:, 128:256], start=True, stop=True)
                        P.op("pe", f0, reads=[rLV], writes=[rpd])
                        P.op("act" if d_ else "dve", (lambda e, pd=pd, LV=LV: e.activation(out=LV[:, 0:256], in_=pd[:, 0:256], func=AF.Copy)) if d_ else
                             (lambda e, pd=pd, LV=LV: e.tensor_copy(out=LV[:, 0:256], in_=pd[:, 0:256])), reads=[rpd], writes=[rLV])
                    elif k < 6:
                        def fk(e, pd=pd, LV=LV):
                            e.matmul(pd[:, 0:128], lhsT=LV[:, 128:256], rhs=LV[:, 0:128], start=True, stop=True)
                            return e.matmul(pd[:, 128:384], lhsT=LV[:, 0:128], rhs=LV[:, 128:384], start=True, stop=True)
                        P.op("pe", fk, reads=[rLV], writes=[rpd])
                        P.op("dve", lambda e, pd=pd, LV=LV: e.tensor_tensor(out=LV[:, 256:384], in0=pd[:, 256:384], in1=LV[:, 256:384], op=ALU.add), reads=[rpd, rLV], writes=[rLV])
                        P.op("act", lambda e, pd=pd, LV=LV: e.activation(out=LV[:, 0:256], in_=pd[:, 0:256], func=AF.Copy), reads=[rpd], writes=[rLV])
                    else:
                        P.op("pe", lambda e, pd=pd, LV=LV: e.matmul(pd[:, 256:384], lhsT=LV[:, 0:128], rhs=LV[:, 256:384], start=True, stop=True), reads=[rLV], writes=[rpd])
                        P.op("dve", lambda e, pd=pd, LV=LV: e.tensor_tensor(out=LV[:, 256:384], in0=pd[:, 256:384], in1=LV[:, 256:384], op=ALU.add), reads=[rpd, rLV], writes=[rLV])
            for d_ in range(2):
                (AM, rAM), (LV, rLV), (TK, rTK), (AV, rAV), (XU, rXU), (RN, rRN) = IB[d_]
                ti = tiles[d_]
                tc_ = slice(ti * 128, (ti + 1) * 128)
                rows = slice(d_ * 64, (d_ + 1) * 64)
                pm = self.PW[:, d_ * 512:(d_ + 1) * 512] if d_ == 0 else self.PB[0]
                rpm = self.rPW if d_ == 0 else self.rPB[0]
                px, rpx = self.PB[2 + d_], self.rPB[2 + d_]
                idb = IDF[rows, rows]
                ZT = LV[:, 256:384]
                def fx(e, pm=pm, ZT=ZT, TK=TK, AV=AV):
                    e.matmul(pm[:, 320:384], lhsT=ZT, rhs=TK[:, 0:64], start=True, stop=True)
                    return e.matmul(pm[:, 384:448], lhsT=ZT, rhs=AV, start=True, stop=True)
                P.op("pe", fx, reads=[rLV, rTK, rAV], writes=[rpm])
                P.op("dve", lambda e, XU=XU, pm=pm: e.tensor_copy(out=XU, in_=pm[:, 320:448]), reads=[rpm], writes=[rXU])
                def fr(e, px=px, XU=XU, AM=AM, TK=TK, rows=rows, tc_=tc_, idb=idb):
                    e.matmul(px[0:64, 0:128], lhsT=XU[:, 0:64], rhs=AM[:, 128:256], start=True, stop=False)
                    e.matmul(px[0:64, 0:128], lhsT=idb, rhs=RT[rows, tc_], start=False, stop=True)
                    e.matmul(px[0:64, 128:192], lhsT=XU[:, 0:64], rhs=TK[:, 64:128], start=True, stop=False)
                    return e.matmul(px[0:64, 128:192], lhsT=IDF[0:64, 0:64], rhs=IDF[0:64, 0:64], start=False, stop=True)
                P.op("pe", fr, reads=[rXU, rAM, rTK, rRT, self.rCST], writes=[rpx])
                P.op("act", lambda e, RN=RN, px=px: e.activation(out=RN, in_=px[0:64, 0:192], func=AF.Copy), reads=[rpx], writes=[rRN])
                Sd = ST[:, d_, :]
                def fy(e, pm=pm, AM=AM, XU=XU, TK=TK, RN=RN, Sd=Sd):
                    e.matmul(pm[:, 448:512], lhsT=AM[:, 128:256], rhs=XU[:, 64:128], start=True, stop=False)
                    e.matmul(pm[:, 448:512], lhsT=AM[:, 384:512], rhs=TK[:, 192:256], start=False, stop=False)
                    e.matmul(pm[:, 448:512], lhsT=RN[:, 0:128], rhs=Sd, start=False, stop=True)
                    e.matmul(pm[0:64, 0:64], lhsT=TK[:, 64:128], rhs=XU[:, 64:128], start=True, stop=False)
                    e.matmul(pm[0:64, 0:64], lhsT=TK[:, 128:192], rhs=TK[:, 192:256], start=False, stop=False)
                    return e.matmul(pm[0:64, 0:64], lhsT=RN[:, 128:192], rhs=Sd, start=False, stop=True)
                P.op("pe", fy, reads=[rAM, rXU, rTK, rRN, rST[d_]], writes=[rpm])
                P.op("dve", lambda e, pm=pm, ti=ti: e.tensor_tensor(out=YH[:, ti, :], in0=pm[:, 448:512], in1=YH[:, ti, :], op=ALU.add), reads=[rpm, rYH], writes=[rYH])
                P.op("dve", lambda e, pm=pm, Sd=Sd, d_=d_, ti=ti: e.tensor_scalar(out=Sd, in0=pm[0:64, 0:64], scalar1=PCS[:, d_, ti:ti + 1], scalar2=None, op0=ALU.mult), reads=[rpm, rPCS], writes=[rST[d_]])
        P.dma(LNG, self.ln_g[l:l + 1, h * 64:(h + 1) * 64].partition_broadcast(128), writes=[rLNG])
        P.dma(LNB, self.ln_b[l:l + 1, h * 64:(h + 1) * 64].partition_broadcast(128), writes=[rLNB])
        MEAN, VAR = GNS[:, 0:16], GNS[:, 16:32]
        P.op("dve", lambda e: e.tensor_reduce(out=MEAN, in_=YH, axis=AX.X, op=ALU.add), reads=[rYH], writes=[rGNS])
        P.op("dve", lambda e: e.tensor_scalar(out=MEAN, in0=MEAN, scalar1=1.0 / 64, scalar2=None, op0=ALU.mult), reads=[rGNS], writes=[rGNS])
        for t in range(NT):
            P.op("dve", lambda e, t=t: e.tensor_scalar(out=YH[:, t, :], in0=YH[:, t, :], scalar1=MEAN[:, t:t + 1], scalar2=None, op0=ALU.subtract), reads=[rYH, rGNS], writes=[rYH])
            P.op("act", lambda e, t=t: e.activation(out=YC, in_=YH[:, t, :], func=AF.Square, accum_out=VAR[:, t:t + 1]), reads=[rYH], writes=[rYC, rGNS])
        P.op("act", lambda e: e.activation(out=VAR, in_=VAR, func=AF.Ln, scale=1.0 / 64, bias=64e-5), reads=[rGNS], writes=[rGNS])
        P.op("act", lambda e: e.activation(out=VAR, in_=VAR, func=AF.Exp, scale=-0.5), reads=[rGNS], writes=[rGNS])
        for t in range(NT):
            tc_ = slice(t * 128, (t + 1) * 128)
            pb, rpb = self.PB[cnt[0] % 2], self.rPB[cnt[0] % 2]
            cnt[0] += 1

            def fe(e, pb=pb, tc_=tc_):
                e.matmul(pb[:, 0:64], lhsT=Vv[0:64, tc_], rhs=IDF[0:64, 0:64], start=True, stop=True)
                return e.matmul(pb[:, 64:128], lhsT=SGD[:, tc_], rhs=GUP[:, hc], start=True, stop=True)
            P.op("pe", fe, reads=[rV, rSGD, rGUP, self.rCST], writes=[rpb])
            P.op("dve", lambda e, t=t: e.scalar_tensor_tensor(out=YC, in0=YH[:, t, :], scalar=VAR[:, t:t + 1], in1=LNG, op0=ALU.mult, op1=ALU.mult), reads=[rYH, rGNS, rLNG], writes=[rYC])
            P.op("pool", lambda e: e.tensor_tensor(out=YC, in0=YC, in1=LNB, op=ALU.add), reads=[rYC, rLNB], writes=[rYC])
            P.op("dve", lambda e, t=t, pb=pb: e.scalar_tensor_tensor(out=YB, in0=pb[:, 0:64], scalar=BON[:, t, h:h + 1], in1=YC, op0=ALU.mult, op1=ALU.add), reads=[rpb, rBON, rYC], writes=[rYB])
            P.op("dve", lambda e, t=t, pb=pb: e.tensor_tensor(out=OBK[:, t, (h % 2) * 64:(h % 2) * 64 + 64], in0=YB, in1=pb[:, 64:128], op=ALU.mult), reads=[rYB, rpb], writes=[rOBK])
        if h % 2 == 1:
            j = h // 2
            for tq in range(4):
                pbt = self.PB[cnt[0] % 2]
                rpbt = self.rPB[cnt[0] % 2]
                cnt[0] += 1

                def ftr(e, pbt=pbt, tq=tq):
                    for tt in range(4):
                        ins = e.matmul(pbt[:, tt * 128:(tt + 1) * 128], lhsT=OBK[:, tq * 4 + tt, :], rhs=self.IDB[:], start=True, stop=True)
                    return ins
                P.op("pe", ftr, reads=[rOBK, self.rIDB], writes=[rpbt])
                P.op("act", lambda e, pbt=pbt, tq=tq, j=j: e.activation(out=OBT[:, j, tq * 512:(tq + 1) * 512], in_=pbt[:, 0:512], func=AF.Copy), reads=[rpbt], writes=[rOBT])
    for h_ in range(12):
        do_head(h_)
    if "ob" in self.dbg:
        self.dump("dbg_ob", OBT, rOBT, [128, 6, T], BF16)
    WBRB, rWBRB = self.view("ARENA", 0, [128, 6, D], BF16, "wbrb")
    P.dma(WBRB, self.w_br_b[l].rearrange("(j p) n -> p j n", p=128), writes=[rWBRB], eng="pool")
    self.release("MG")
    self.merge(l, 1, lambda j: (WBRB[:, j, :], OBT[:, j, :], [rWBRB, rOBT]), 6)


Builder.rwkv_phase = _rwkv_phase
```

```python
import math
import numpy as np
from contextlib import ExitStack
import concourse.bass as bass
import concourse.mybir as mybir
from concourse.bass_utils import run_bass_kernel_spmd

F32 = mybir.dt.float32
BF16 = mybir.dt.bfloat16
F32R = mybir.dt.float32r
AF = mybir.ActivationFunctionType
ALU = mybir.AluOpType
AX = mybir.AxisListType

D = 1024
T = 2048
NT = 16
KC = 8
L = 2
E = 32
INC = 10368
SEM_CHUNK = 30000
A_OFF, B_OFF, C_OFF, G_OFF = 0, 2304, 4992, 7296
DECAY_C = math.exp(-0.5)


class Res:
    __slots__ = ("name", "w", "r", "dsem", "dcnt")

    def __init__(self, name):
        self.name = name
        self.w = None
        self.r = []
        self.dsem = None
        self.dcnt = 0


class Prog:
    ENGS = ("sp", "act", "dve", "pool", "pe")

    def __init__(self, nc):
        self.nc = nc
        self.q = {e: [] for e in self.ENGS}
        self.cnt = {e: 0 for e in self.ENGS}
        self.known = {e: {} for e in self.ENGS}
        self.ndsem = 0
        self.dma_final = {}

    def _collect(self, eng, reads, writes, is_dma):
        waits = {}

        def need(ev, kind):
            if ev is None:
                return
            key, val, src = ev
            if src == eng and not is_dma and key[0] == "e":
                if eng == "pe" or kind == "war":
                    return
            if self.known[eng].get(key, 0) >= val:
                return
            if waits.get(key, 0) < val:
                waits[key] = val

        for r in reads:
            need(r.w, "raw")
        for w in writes:
            need(w.w, "waw")
            for ev in w.r:
                need(ev, "war")
        for k, v in waits.items():
            self.known[eng][k] = v
        return waits

    def _commit(self, ev, reads, writes):
        for r in reads:
            r.r.append(ev)
            if len(r.r) > 24:
                best = {}
                for e2 in r.r:
                    if e2[0] not in best or best[e2[0]][1] < e2[1]:
                        best[e2[0]] = e2
                r.r = list(best.values())
        for w in writes:
            w.w = ev
            w.r = []

    def op(self, eng, fn, reads=(), writes=()):
        waits = self._collect(eng, reads, writes, False)
        n = self.cnt[eng] + 1
        self.cnt[eng] = n
        ev = (("e", eng, (n - 1) // SEM_CHUNK), (n - 1) % SEM_CHUNK + 1, eng)
        self.q[eng].append((waits, fn, [(ev[0], 1)]))
        self._commit(ev, reads, writes)
        return ev

    def dma(self, out_ap, in_ap, reads=(), writes=(), eng="sp", **kw):
        waits = self._collect(eng, reads, writes, True)
        w0 = writes[0]
        if w0.dsem is None:
            w0.dsem = self.ndsem
            self.ndsem += 1
        w0.dcnt += 16
        ev = (("d", w0.dsem), w0.dcnt, "dma")
        self.dma_final[ev[0]] = w0.dcnt

        def fn(e, out_ap=out_ap, in_ap=in_ap, kw=kw):
            return e.dma_start(out=out_ap, in_=in_ap, **kw)

        self.q[eng].append((waits, fn, [(ev[0], 16)]))
        self._commit(ev, reads, writes)
        return ev

    def final_wait(self, eng, res_list):
        waits = dict(self.dma_final)
        for r in res_list:
            if r.w is not None:
                key, val, _ = r.w
                waits[key] = max(waits.get(key, 0), val)
        self.q[eng].append((waits, None, []))

    def emit(self, stack):
        nc = self.nc
        sems = {}
        for e in self.ENGS:
            for waits, fn, incs in self.q[e]:
                for k in list(waits) + [k for k, _ in incs]:
                    if k not in sems:
                        sems[k] = stack.enter_context(nc.semaphore("s_" + "_".join(str(x) for x in k)))
        self.nsems = len(sems)
        block = stack.enter_context(nc.Block())

        def replay(engname):
            def body(e):
                for waits, fn, incs in self.q[engname]:
                    for k, v in waits.items():
                        e.wait_ge(sems[k], v)
                    if fn is None:
                        continue
                    ins = fn(e)
                    for k, v in incs:
                        ins = ins.then_inc(sems[k], v)
            return body

        block.sync(replay("sp"))
        block.scalar(replay("act"))
        block.vector(replay("dve"))
        block.gpsimd(replay("pool"))
        block.tensor(replay("pe"))


def _t5_bucket_np(rel):
    nb, max_exact = 16, 8
    rel = np.asarray(rel, np.int64)
    ret = np.where(rel > 0, nb, 0)
    n = np.abs(rel)
    nf = np.maximum(n, 1).astype(np.float32)
    large = max_exact + (np.log(nf / np.float32(max_exact)) / np.float32(math.log(128 / max_exact)) * np.float32(nb - max_exact)).astype(np.int32)
    large = np.minimum(large, nb - 1)
    return ret + np.where(n < max_exact, n, large)


def _host_consts():
    cst = np.zeros((128, 1792), np.float32)
    cst[:, 0:128] = np.eye(128, dtype=np.float32)
    r = np.arange(128)[:, None]
    c = np.arange(128)[None, :]
    su, iu = (r < c), (r <= c)
    sl, il = (r > c), (r >= c)
    cst[:, 128:768] = np.concatenate([su, iu, su, iu, sl], 1)
    cst[:, 768:1408] = np.concatenate([sl, il, sl, il, su], 1)
    cst[:, 1408:1536] = 1.0
    EA = np.zeros((33, 1536), np.float32)
    for g, d in enumerate((1, 4, 16)):
        j = np.arange(512) - 255
        b = _t5_bucket_np(j * d)
        b = np.where(np.abs(j) <= 64, b, 32)
        EA[b, g * 512 + np.arange(512)] = 1.0
    EC = np.zeros((33, 1536), np.float32)
    b = _t5_bucket_np(np.arange(1536) - 767)
    EC[b, np.arange(1536)] = 1.0
    return cst, EA, EC


class Builder:
    def __init__(self, layers=(0, 1), do_mixer=True, do_moe=True, dbg=()):
        self.layers = layers
        self.do_mixer = do_mixer
        self.do_moe = do_moe
        self.dbg = dbg
        self.nc = bass.Bass("TRN2", target_bir_lowering=False)
        self.st = ExitStack()
        self.P = Prog(self.nc)
        self.outs = []
        self._uid = 0

    def din(self, name, shape, dt=F32):
        return self.nc.dram_tensor(name, list(shape), dt, kind="ExternalInput").ap()

    def dout(self, name, shape, dt=F32):
        return self.nc.dram_tensor(name, list(shape), dt, kind="ExternalOutput").ap()

    def dscratch(self, name, shape, dt=F32):
        return self.nc.dram_tensor(name, list(shape), dt, kind="Internal").ap()

    def sb(self, name, shape, dt=F32):
        return self.st.enter_context(self.nc.sbuf_tensor(name, list(shape), dt))

    def ps(self, name, shape, dt=F32):
        return self.st.enter_context(self.nc.psum_tensor(name, list(shape), dt))

    def R(self, name):
        self._uid += 1
        return Res(f"{name}{self._uid}")

    def dump(self, name, ap_sb, res, shape, dt=F32):
        o = self.dout(name, shape, dt)
        r = self.R("dump")
        self.P.dma(o, ap_sb, reads=[res], writes=[r])
        self.outs.append(r)

    @staticmethod
    def carve(reg, off_bytes, shape, dt):
        n = 1
        for s in shape[1:]:
            n *= s
        esz = 4 if dt == F32 else 2
        assert off_bytes % 4 == 0
        a = off_bytes // 2
        b = a + n * esz // 2
        assert b <= reg.shape[1], (b, reg.shape)
        ap = reg[0:shape[0], a:b]
        if dt == F32:
            ap = ap.bitcast(F32)
        if len(shape) == 2:
            return ap
        names = " ".join(f"d{i}" for i in range(len(shape) - 1))
        kw = {f"d{i}": shape[i + 1] for i in range(len(shape) - 2)}
        return ap.rearrange(f"p ({names}) -> p {names}", **kw)

    def declare(self):
        d = self.din
        self.x_in = d("x", [T, D])
        self.cT_in = d("cT", [128, 8])
        self.w_mod = d("w_mod", [L, D, 6 * D])
        self.bmodT = d("bmodT", [L, 128, 48])
        self.bmod_row = d("bmod_row", [L, 1, 6 * D])
        self.ncol = d("ncol", [L, 128, 16])
        self.w_in = d("w_in", [L, D, INC])
        self.muT = d("muT", [L, 128, 36, 2])
        self.muL = d("muL", [L, 128, 3, 2])
        self.rwcol = d("rwcol", [L, 128, 12, 4])
        self.rkcol = d("rkcol", [L, 128, 12])
        self.w_up = d("w_up", [L, 128, 768])
        self.a_up = d("a_up", [L, 128, 768])
        self.g_up = d("g_up", [L, 128, 768])
        self.ln_g = d("ln_g", [L, 768])
        self.ln_b = d("ln_b", [L, 768])
        self.dlam = d("dlam", [L, 256])
        self.subln = d("subln", [L, 128, 1])
        self.rel_bias = d("rel_bias", [32, 18])
        self.w_br_a = d("w_br_a", [L, 256, D])
        self.w_br_b = d("w_br_b", [L, 768, D])
        self.w_br_c = d("w_br_c", [L, 768, D])
        self.w_out = d("w_out", [L, D, D])
        self.router_w = d("router_w", [L, D, E])
        self.router_b = d("router_b", [L, E])
        self.moe_w1 = d("moe_w1", [L, E, D, 2, 1024])
        self.b1T = d("b1T", [L, 128, E, 2, 8])
        self.moe_w2 = d("moe_w2", [L, E, 1024, D])
        self.moe_b2 = d("moe_b2", [L, E, D])
        self.fng = d("fng", [1, D])
        self.cst_in = d("cst", [128, 1792])
        self.EA_in = d("EA", [33, 1536])
        self.EC_in = d("EC", [33, 1536])
        self.y_out = self.dout("y", [T, D])
        self.x_spill = self.dscratch("x_spill", [T, D])
        self.ebA = self.dscratch("ebA", [12, 512])
        self.ebC = self.dscratch("ebC", [6, 1536])

    def alloc(self):
        sb, ps = self.sb, self.ps
        self.ARENA = sb("ARENA", [128, 32768], BF16)
        self.HT = sb("HT", [128, KC, T], BF16)
        self.MG = sb("MG", [128, 16384], BF16)
        self.EX = sb("EX", [128, 20 * 1024], BF16)
        self.CST = sb("CST", [128, 1792], F32)
        self.IDB = sb("IDB", [128, 128], BF16)
        self.WB = [sb(f"WB{i}", [128, KC, 512], BF16) for i in range(2)]
        self.MODT = sb("MODT", [128, 48], F32)
        self.GEFF = sb("GEFF", [128, 16], F32)
        self.NCOL = sb("NCOL", [128, 16], F32)
        self.SS = sb("SS", [128, 32], F32)
        self.GTB = sb("GTB", [128, 2, D], F32)
        self.CONDT = sb("CONDT", [128, 8], F32)
        self.JUNK = sb("JUNK", [128, D], BF16)
        self.LVR = [sb(f"LVR{i}", [128, 384], F32R) for i in range(2)]
        self.X = self.ARENA[:, :].bitcast(F32).rearrange("p (t d) -> p t d", t=NT)
        self.IDF = self.CST[:, 0:128]
        self.ONES = self.CST[:, 1408:1536]
        self.PB = [ps(f"PB{i}", [128, 512], F32) for i in range(6)]
        self.PW = ps("PW", [128, 1024], F32)
        R = self.R
        self.rX = R("X")
        self.rHT = R("HT")
        self.rMG = R("MG")
        self.rCST = R("CST")
        self.rIDB = R("IDB")
        self.rWB = [R("WB0"), R("WB1")]
        self.rPB = [R(f"PB{i}") for i in range(6)]
        self.rPW = R("PW")
        self.rMODT, self.rGEFF, self.rNCOL, self.rSS = R("MODT"), R("GEFF"), R("NCOL"), R("SS")
        self.rGTB, self.rCONDT, self.rJUNK = R("GTB"), R("CONDT"), R("JUNK")
        self.rEX = R("EX")
        self.wbi = 0

    def prologue(self):
        P = self.P
        P.dma(self.CST[:], self.cst_in[:, :], writes=[self.rCST])
        P.op("dve", lambda e: e.tensor_copy(out=self.IDB[:], in_=self.IDF), reads=[self.rCST], writes=[self.rIDB])
        P.dma(self.CONDT[:], self.cT_in[:, :], writes=[self.rCONDT])
        P.op("act", lambda e: e.activation(out=self.CONDT[:], in_=self.CONDT[:], func=AF.Silu), reads=[self.rCONDT], writes=[self.rCONDT])
        xin = self.x_in.rearrange("(t p) d -> p t d", p=128)
        for q in range(4):
            P.dma(self.X[:, q * 4:(q + 1) * 4, :], xin[:, q * 4:(q + 1) * 4, :], writes=[self.rX])

    def mod_phase(self, l):
        P = self.P
        _w = [self.view("MG", i * 16384, [128, KC, 512], F32, f"wst{i}") for i in range(2)]
        WST = [a for a, _ in _w]
        rW = [r_ for _, r_ in _w]
        PC, rPC = self.PB[0], self.rPB[0]
        PR, rPR = self.PB[1], self.rPB[1]
        wsrc = self.w_mod[l].rearrange("(kc p) n -> p kc n", p=128)
        bt, rbt = self.view("EX", 0, [128, 48], F32, "bt")
        P.dma(bt, self.bmodT[l], writes=[rbt])
        brow, rbrow = self.view("EX", 256, [1, 2 * D], F32, "brow")
        self.GTROW, self.rGTROW = self.view("EX", 256 + 8192, [1, 2 * D], F32, "gtrow")
        P.dma(brow[0:1, 0:D], self.bmod_row[l][:, 2 * D:3 * D], writes=[rbrow])
        P.dma(brow[0:1, D:2 * D], self.bmod_row[l][:, 5 * D:6 * D], writes=[rbrow])
        P.dma(self.NCOL[:], self.ncol[l], writes=[self.rNCOL])
        for cb in range(12):
            s = cb % 2
            vec = cb // 2
            P.dma(WST[s][:], wsrc[:, :, cb * 512:(cb + 1) * 512], writes=[rW[s]])
            if vec in (2, 5):
                col0 = (0 if vec == 2 else D) + (cb % 2) * 512

                def f(e, s=s):
                    for kc in range(KC):
                        ins = e.matmul(PR[0:1, :], lhsT=self.CONDT[:, kc:kc + 1], rhs=WST[s][:, kc, :], start=(kc == 0), stop=(kc == KC - 1))
                    return ins
                P.op("pe", f, reads=[rW[s], self.rCONDT], writes=[rPR])
                P.op("dve", lambda e, col0=col0: e.tensor_tensor(out=self.GTROW[0:1, col0:col0 + 512], in0=PR[0:1, :], in1=brow[0:1, col0:col0 + 512], op=ALU.add),
                     reads=[rPR, rbrow], writes=[self.rGTROW])
            else:
                def f(e, s=s, cb=cb):
                    for j in range(4):
                        for kc in range(KC):
                            ins = e.matmul(PC[:, cb * 4 + j:cb * 4 + j + 1], lhsT=WST[s][:, kc, j * 128:(j + 1) * 128], rhs=self.CONDT[:, kc:kc + 1],
                                           start=(kc == 0), stop=(kc == KC - 1))
                    return ins
                P.op("pe", f, reads=[rW[s], self.rCONDT], writes=[rPC])
        for (a, b) in ((0, 16), (24, 40)):
            P.op("dve", lambda e, a=a, b=b: e.tensor_tensor(out=self.MODT[:, a:b], in0=PC[:, a:b], in1=bt[:, a:b], op=ALU.add),
                 reads=[rPC, rbt], writes=[self.rMODT])
        for i, c0 in ((0, 8), (1, 32)):
            P.op("dve", lambda e, i=i, c0=c0: e.scalar_tensor_tensor(out=self.GEFF[:, i * 8:(i + 1) * 8], in0=self.MODT[:, c0:c0 + 8], scalar=1.0,
                                                                   in1=self.NCOL[:, i * 8:(i + 1) * 8], op0=ALU.add, op1=ALU.mult),
                 reads=[self.rMODT, self.rNCOL], writes=[self.rGEFF])
        for i in range(2):
            for hf in range(2):
                pb, rpb = self.PB[2 + hf], self.rPB[2 + hf]
                P.op("pe", lambda e, i=i, hf=hf, pb=pb: e.matmul(pb[:, :], lhsT=self.ONES[0:1, :], rhs=self.GTROW[0:1, i * D + hf * 512:i * D + (hf + 1) * 512], start=True, stop=True),
                     reads=[self.rGTROW, self.rCST], writes=[rpb])
                P.op("act", lambda e, i=i, hf=hf, pb=pb: e.activation(out=self.GTB[:, i, hf * 512:(hf + 1) * 512], in_=pb[:, :], func=AF.Copy),
                     reads=[rpb], writes=[self.rGTB])
        self.release("MG")

    def norm_phase(self, which, router=None):
        P = self.P
        gcol = self.GEFF[:, which * 8:(which + 1) * 8]
        shc = self.MODT[:, (0 if which == 0 else 24):(8 if which == 0 else 32)]
        SS, RS = self.SS[:, 0:16], self.SS[:, 16:32]
        for t in range(NT):
            P.op("act", lambda e, t=t: e.activation(out=self.JUNK[:], in_=self.X[:, t, :], func=AF.Square, accum_out=SS[:, t:t + 1]),
                 reads=[self.rX], writes=[self.rJUNK, self.rSS])
        import os
        NCUT = int(os.environ.get("NCUT", "99"))
        if NCUT <= 1:
            return
        P.op("act", lambda e: e.activation(out=RS, in_=SS, func=AF.Ln, scale=1.0 / D, bias=1e-6), reads=[self.rSS], writes=[self.rSS])
        P.op("act", lambda e: e.activation(out=RS, in_=RS, func=AF.Exp, scale=-0.5), reads=[self.rSS], writes=[self.rSS])
        if NCUT <= 2:
            return
        _x = [self.view("EX", 1024 + i * 4096, [128, D], F32, f"xn{i}") for i in range(4)]
        XNs = [a for a, _ in _x]
        rXN = [r_ for _, r_ in _x]
        if router is not None:
            H2F, rH2F = self.view("EX", 1024 + 16384, [128, KC, 512], F32, "h2f")
        for tg in range(4):
            for tt in range(4):
                t = tg * 4 + tt
                P.op("act", lambda e, t=t, tt=tt: e.activation(out=XNs[tt], in_=self.X[:, t, :], func=AF.Copy, scale=RS[:, t:t + 1]),
                     reads=[self.rX, self.rSS], writes=[rXN[tt]])
            if NCUT <= 3:
                continue
            for kc in range(KC):
                pb, rpb = self.PB[kc % 2], self.rPB[kc % 2]

                def f(e, kc=kc, pb=pb):
                    for tt in range(4):
                        ins = e.matmul(pb[:, tt * 128:(tt + 1) * 128], lhsT=XNs[tt][:, kc * 128:(kc + 1) * 128], rhs=self.IDF, start=True, stop=True)
                    return ins
                P.op("pe", f, reads=rXN + [self.rCST], writes=[rpb])
                if NCUT <= 4:
                    continue
                P.op("dve", lambda e, kc=kc, tg=tg, pb=pb: e.tensor_scalar(out=self.HT[:, kc, tg * 512:(tg + 1) * 512], in0=pb[:, :], scalar1=gcol[:, kc:kc + 1],
                                                                          scalar2=shc[:, kc:kc + 1], op0=ALU.mult, op1=ALU.add),
                     reads=[rpb, self.rGEFF, self.rMODT], writes=[self.rHT])
                if NCUT <= 5:
                    continue
                if router is not None:
                    P.op("dve", lambda e, kc=kc, pb=pb: e.tensor_scalar(out=H2F[:, kc, :], in0=pb[:, :], scalar1=gcol[:, kc:kc + 1],
                                                                        scalar2=shc[:, kc:kc + 1], op0=ALU.mult, op1=ALU.add),
                         reads=[rpb, self.rGEFF, self.rMODT], writes=[rH2F])
            if router is not None and NCUT > 6:
                router(tg, H2F, rH2F)

    def view(self, regname, off, shape, dt, name="v"):
        reg = {"EX": self.EX, "MG": self.MG, "ARENA": self.ARENA}[regname]
        n = 1
        for s in shape[1:]:
            n *= s
        size = n * (4 if dt == F32 else 2)
        if not hasattr(self, "_live"):
            self._live = {"EX": [], "MG": [], "ARENA": []}
        res = self.R(name)
        keep = []
        evs = []
        for (o, sz, r_) in self._live[regname]:
            if o < off + size and off < o + sz:
                evs.extend(r_.r)
                if r_.w is not None:
                    evs.append(r_.w)
            else:
                keep.append((o, sz, r_))
        base = {"EX": self.rEX, "MG": self.rMG, "ARENA": self.rX}[regname]
        evs.extend(base.r)
        if base.w is not None:
            evs.append(base.w)
        best = {}
        for e2 in evs:
            if e2[0] not in best or best[e2[0]][1] < e2[1]:
                best[e2[0]] = e2
        res.r = list(best.values())
        keep.append((off, size, res))
        self._live[regname] = keep
        return self.carve(reg, off, shape, dt), res

    def release(self, regname):
        base = {"EX": self.rEX, "MG": self.rMG, "ARENA": self.rX}[regname]
        evs = list(base.r)
        for (o, sz, r_) in getattr(self, "_live", {}).get(regname, []):
            evs.extend(r_.r)
            if r_.w is not None:
                evs.append(r_.w)
        best = {}
        for e2 in evs:
            if e2[0] not in best or best[e2[0]][1] < e2[1]:
                best[e2[0]] = e2
        base.r = list(best.values())
        if hasattr(self, "_live"):
            self._live[regname] = []

    def moe_phase(self, l):
        P = self.P
        K1 = 1024
        GATES, rGATES = self.view("EX", 33 * K1, [128, NT, E], F32, "gates")
        RWf, rRWf = self.view("EX", 35 * K1, [128, KC, E], F32, "rwf")
        RB, rRB = self.view("EX", 36 * K1, [128, E], F32, "rb")
        SM, rSM = self.view("EX", 36 * K1 + 128, [128, 96], F32, "sm")
        M8, rM8 = self.view("EX", 36 * K1 + 512, [128, 16], F32, "m8")
        B1C, rB1C = self.view("EX", 37 * K1, [128, E, 2, 8], F32, "b1c")
        B1L7, rB1L7 = self.view("EX", 39 * K1, [128, E, 8], F32, "b1l7")
        GTt, rGTt = self.view("MG", 0, [32, T], F32, "gtt")
        B2, rB2 = self.view("MG", 8 * K1, [32, D], F32, "b2")
        P.dma(RWf, self.router_w[l].rearrange("(kc p) e -> p kc e", p=128), writes=[rRWf])
        rb_src = self.router_b[l:l + 1, :].partition_broadcast(128)
        P.dma(RB, rb_src, writes=[rRB])
        P.dma(B1C, self.b1T[l], writes=[rB1C])
        P.dma(B2, self.moe_b2[l], writes=[rB2])
        P.op("dve", lambda e: e.tensor_scalar(out=B1L7, in0=B1C[:, :, 1, :], scalar1=7.0, scalar2=None, op0=ALU.add), reads=[rB1C], writes=[rB1L7])

        def router(tg, H2F, rH2F):
            for tt in range(4):
                t = tg * 4 + tt
                pb, rpb = self.PB[2], self.rPB[2]

                def f(e, tt=tt):
                    for kc in range(KC):
                        ins = e.matmul(pb[:, 0:E], lhsT=H2F[:, kc, tt * 128:(tt + 1) * 128], rhs=RWf[:, kc, :], start=(kc == 0), stop=(kc == KC - 1))
                    return ins
                P.op("pe", f, reads=[rH2F, rRWf], writes=[rpb])
                LG, EXPV, MASK = SM[:, 0:32], SM[:, 32:64], SM[:, 64:96]
                P.op("dve", lambda e: e.tensor_tensor(out=LG, in0=pb[:, 0:E], in1=RB, op=ALU.add), reads=[rpb, rRB], writes=[rSM])
                P.op("dve", lambda e: e.max(out=M8[:, 0:8], in_=LG), reads=[rSM], writes=[rM8])
                P.op("dve", lambda e: e.tensor_scalar(out=M8[:, 8:9], in0=M8[:, 0:1], scalar1=-1.0, scalar2=None, op0=ALU.mult), reads=[rM8], writes=[rM8])
                P.op("act", lambda e: e.activation(out=EXPV, in_=LG, func=AF.Exp, bias=M8[:, 8:9]), reads=[rSM, rM8], writes=[rSM])
                P.op("dve", lambda e: e.tensor_scalar(out=MASK, in0=LG, scalar1=M8[:, 3:4], scalar2=None, op0=ALU.is_ge), reads=[rSM, rM8], writes=[rSM])
                P.op("dve", lambda e: e.tensor_tensor(out=EXPV, in0=EXPV, in1=MASK, op=ALU.mult), reads=[rSM], writes=[rSM])
                P.op("dve", lambda e: e.tensor_reduce(out=M8[:, 9:10], in_=EXPV, axis=AX.X, op=ALU.add), reads=[rSM], writes=[rM8])
                P.op("dve", lambda e: e.reciprocal(out=M8[:, 10:11], in_=M8[:, 9:10]), reads=[rM8], writes=[rM8])
                P.op("dve", lambda e, t=t: e.tensor_scalar(out=GATES[:, t, :], in0=EXPV, scalar1=M8[:, 10:11], scalar2=None, op0=ALU.mult), reads=[rSM, rM8], writes=[rGATES])
                pb3, rpb3 = self.PB[3], self.rPB[3]
                P.op("pe", lambda e, t=t: e.matmul(pb3[0:E, 0:128], lhsT=GATES[:, t, :], rhs=self.IDF, start=True, stop=True), reads=[rGATES, self.rCST], writes=[rpb3])
                P.op("act", lambda e, t=t: e.activation(out=GTt[:, t * 128:(t + 1) * 128], in_=pb3[0:E, 0:128], func=AF.Copy), reads=[rpb3], writes=[rGTt])

        self.norm_phase(1, router=router)
        if getattr(self, "moe_stop", 0) == 1:
            self.dump("dbg_gates", GATES, rGATES, [128, NT, E])
            return

        GT2 = self.GTB[:, 1, :]
        TMPB, rTMPB = self.view("EX", 1 * K1, [128, 512], F32, "tmpb")
        for t in range(NT):
            for nb in range(2):
                pb, rpb = self.PB[4 + nb], self.rPB[4 + nb]
                P.op("pe", lambda e, t=t, nb=nb, pb=pb: e.matmul(pb[:, :], lhsT=GTt[:, t * 128:(t + 1) * 128], rhs=B2[:, nb * 512:(nb + 1) * 512], start=True, stop=True),
                     reads=[rGTt, rB2], writes=[rpb])
                P.op("dve", lambda e, nb=nb, pb=pb: e.tensor_tensor(out=TMPB, in0=pb[:, :], in1=GT2[:, nb * 512:(nb + 1) * 512], op=ALU.mult),
                     reads=[rpb, self.rGTB], writes=[rTMPB])
                P.op("pool", lambda e, t=t, nb=nb: e.tensor_tensor(out=self.X[:, t, nb * 512:(nb + 1) * 512], in0=self.X[:, t, nb * 512:(nb + 1) * 512], in1=TMPB, op=ALU.add),
                     reads=[rTMPB, self.rX], writes=[self.rX])

        if getattr(self, "moe_stop", 0) == 2:
            return
        GT2b, rGT2b = self.view("EX", 3 * K1, [128, D], BF16, "gt2b")
        P.op("act", lambda e: e.activation(out=GT2b, in_=GT2, func=AF.Copy), reads=[self.rGTB], writes=[rGT2b])
        W1 = []
        for i in range(2):
            W1.append(self.view("MG", i * 16 * K1, [128, KC, 2, 512], BF16, f"w1_{i}"))
        W2 = [(self.WB[i][:].rearrange("p a b -> p (a b)").rearrange("p (f d) -> p f d", f=4), self.rWB[i]) for i in range(2)]
        ACTT = [self.view("EX", (5 + 4 * i) * K1, [128, 4, 512], BF16, f"actt{i}") for i in range(3)]
        TMP = [[self.view("EX", (17 + 8 * s + 2 * j) * K1, [128, 512], F32, f"tmp{s}{j}") for j in range(4)] for s in range(2)]
        w1src = self.moe_w1[l].rearrange("e (kc p) g f -> e p kc g f", p=128)
        w2src = self.moe_w2[l].rearrange("e (fc p) d -> e p fc d", p=128)
        blocks = [(e_, hf, tb) for e_ in range(E) for hf in range(2) for tb in range(4)]

        def load(e_, hf):
            import os
            if os.environ.get("NOLOAD") and (e_ > 0 or hf > 0):
                return
            s = (e_ * 2 + hf) % 2
            w1, rw1 = W1[s]
            w2, rw2 = W2[s]
            for kh in range(2):
                for gl in range(2):
                    P.dma(w1[:, kh * 4:(kh + 1) * 4, gl, :], w1src[e_, :, kh * 4:(kh + 1) * 4, gl, hf * 512:(hf + 1) * 512], writes=[rw1], eng="pool")
            P.dma(w2, w2src[e_, :, hf * 4:(hf + 1) * 4, :], writes=[rw2], eng="pool")
            for fc in range(4):
                P.op("dve", lambda e, fc=fc, w2=w2: e.tensor_tensor(out=w2[:, fc, :], in0=w2[:, fc, :], in1=GT2b, op=ALU.mult),
                     reads=[rw2, rGT2b], writes=[rw2])

        def phase1(bi):
            e_, hf, tb = blocks[bi]
            s = (e_ * 2 + hf) % 2
            w1, rw1 = W1[s]
            at, rat = ACTT[bi % 3]
            for fc in range(4):
                q = (bi * 4 + fc) % 2
                pg, rpg = self.PB[2 * q], self.rPB[2 * q]
                pl, rpl = self.PB[2 * q + 1], self.rPB[2 * q + 1]
                (glu, rglu), (sig, rsig), (t1, rt1), (v, rv) = TMP[q]
                fcg = hf * 4 + fc

                def f(e, fc=fc, pg=pg, pl=pl, w1=w1, tb=tb):
                    for gl, pp in ((0, pg), (1, pl)):
                        for kc in range(KC):
                            ins = e.matmul(pp[:, :], lhsT=w1[:, kc, gl, fc * 128:(fc + 1) * 128], rhs=self.HT[:, kc, tb * 512:(tb + 1) * 512], start=(kc == 0), stop=(kc == KC - 1))
                    return ins
                P.op("pe", f, reads=[rw1, self.rHT], writes=[rpg, rpl])
                P.op("dve", lambda e, pg=pg, glu=glu, e_=e_, fcg=fcg: e.tensor_scalar(out=glu, in0=pg[:, :], scalar1=B1C[:, e_, 0, fcg:fcg + 1], scalar2=7.0, op0=ALU.add, op1=ALU.min),
                     reads=[rpg, rB1C], writes=[rglu])
                P.op("act", lambda e, pl=pl, t1=t1, e_=e_, fcg=fcg: e.activation(out=t1, in_=pl[:, :], func=AF.Relu, bias=B1L7[:, e_, fcg:fcg + 1]),
                     reads=[rpl, rB1L7], writes=[rt1])
                P.op("act", lambda e, glu=glu, sig=sig: e.activation(out=sig, in_=glu, func=AF.Silu, scale=1.702), reads=[rglu], writes=[rsig])
                P.op("dve", lambda e, t1=t1: e.tensor_scalar(out=t1, in0=t1, scalar1=14.0, scalar2=-6.0, op0=ALU.min, op1=ALU.add), reads=[rt1], writes=[rt1])
                P.op("dve", lambda e, sig=sig, t1=t1, at=at, fc=fc: e.scalar_tensor_tensor(out=at[:, fc, :], in0=sig, scalar=1.0 / 1.702, in1=t1, op0=ALU.mult, op1=ALU.mult),
                     reads=[rsig, rt1], writes=[rat])

        def phase2(bi):
            e_, hf, tb = blocks[bi]
            s = (e_ * 2 + hf) % 2
            w2, rw2 = W2[s]
            at, rat = ACTT[bi % 3]
            for tt in range(4):
                t = tb * 4 + tt
                for nb in range(2):
                    po, rpo = self.PB[4 + nb], self.rPB[4 + nb]

                    def f(e, tt=tt, nb=nb, po=po, at=at, w2=w2):
                        for fc in range(4):
                            ins = e.matmul(po[:, :], lhsT=at[:, fc, tt * 128:(tt + 1) * 128], rhs=w2[:, fc, nb * 512:(nb + 1) * 512], start=(fc == 0), stop=(fc == 3))
                        return ins
                    P.op("pe", f, reads=[rat, rw2], writes=[rpo])
                    P.op("dve", lambda e, t=t, nb=nb, po=po, e_=e_: e.scalar_tensor_tensor(out=self.X[:, t, nb * 512:(nb + 1) * 512], in0=po[:, :], scalar=GATES[:, t, e_:e_ + 1],
                                                                                       in1=self.X[:, t, nb * 512:(nb + 1) * 512], op0=ALU.mult, op1=ALU.add),
                         reads=[rpo, rGATES, self.rX], writes=[self.rX])

        nb_ = len(blocks)
        load(0, 0)
        for bi in range(nb_ + 1):
            if bi < nb_:
                phase1(bi)
            if bi >= 1:
                phase2(bi - 1)
            if bi < nb_:
                e_, hf, tb = blocks[bi]
                if tb == 0:
                    nxt = e_ * 2 + hf + 1
                    if nxt < 2 * E:
                        load(nxt // 2, nxt % 2)
        self.release("EX")
        self.release("MG")

    def final_phase(self):
        P = self.P
        GF, rGF = self.view("EX", 0, [128, D], F32, "gf")
        P.dma(GF, self.fng[0:1, :].partition_broadcast(128), writes=[rGF])
        SS, RS = self.SS[:, 0:16], self.SS[:, 16:32]
        for t in range(NT):
            P.op("act", lambda e, t=t: e.activation(out=self.JUNK[:], in_=self.X[:, t, :], func=AF.Square, accum_out=SS[:, t:t + 1]),
                 reads=[self.rX], writes=[self.rJUNK, self.rSS])
        P.op("act", lambda e: e.activation(out=RS, in_=SS, func=AF.Ln, scale=1.0 / D, bias=1e-6), reads=[self.rSS], writes=[self.rSS])
        P.op("act", lambda e: e.activation(out=RS, in_=RS, func=AF.Exp, scale=-0.5), reads=[self.rSS], writes=[self.rSS])
        OB = [self.view("EX", (4 + 4 * i) * 1024, [128, D], F32, f"ob{i}") for i in range(2)]
        yv = self.y_out.rearrange("(t p) d -> p t d", p=128)
        rY = self.R("y")
        for t in range(NT):
            ob, rob = OB[t % 2]
            P.op("dve", lambda e, t=t, ob=ob: e.scalar_tensor_tensor(out=ob, in0=self.X[:, t, :], scalar=RS[:, t:t + 1], in1=GF, op0=ALU.mult, op1=ALU.mult),
                 reads=[self.rX, self.rSS, rGF], writes=[rob])
            P.dma(yv[:, t, :], ob, reads=[rob], writes=[rY])
        self.outs.append(rY)

    def build(self):
        self.declare()
        self.alloc()
        self.prologue()
        if self.do_mixer:
            self.eb_build()
        for l in self.layers:
            self.mod_phase(l)
            if self.do_mixer:
                self.mixer_phase(l)
            if self.do_moe:
                self.moe_phase(l)
        self.final_phase()
        self.P.final_wait("sp", self.outs)
        self.P.emit(self.st)
        self.st.close()
        return self.nc


def prep_inputs(inp, b):
    f = lambda a: np.ascontiguousarray(a, dtype=np.float32)
    cst, EA, EC = _host_consts()
    m = {}
    m["x"] = f(inp["x"][b])
    m["cT"] = f(inp["c"][b].reshape(8, 128).T)
    m["w_mod"] = f(inp["w_mod"])
    m["bmodT"] = f(inp["b_mod"].reshape(L, 48, 128).transpose(0, 2, 1))
    m["bmod_row"] = f(inp["b_mod"].reshape(L, 1, 6 * D))
    m["ncol"] = f(np.concatenate([inp["norm1_g"].reshape(L, 8, 128).transpose(0, 2, 1), inp["norm2_g"].reshape(L, 8, 128).transpose(0, 2, 1)], axis=2))
    m["w_in"] = f(inp["w_in"])
    mu = inp["rwkv_mu"]
    mu_rkv = mu[:, :, :2304].reshape(L, 2, 36, 64)
    muT = mu_rkv.transpose(0, 3, 2, 1)
    m["muT"] = f(np.concatenate([muT, muT], axis=1))
    m["muL"] = f(mu[:, :, 2304:].reshape(L, 2, 3, 128).transpose(0, 3, 2, 1))
    rw = np.stack([inp["rwkv_w0"], inp["rwkv_a0"], inp["rwkv_k_k"], inp["rwkv_k_a"]], axis=-1)
    m["rwcol"] = f(rw.reshape(L, 2, 12, 64, 4).transpose(0, 1, 3, 2, 4).reshape(L, 128, 12, 4))
    rk = inp["rwkv_r_k"].transpose(0, 2, 1)
    m["rkcol"] = f(np.concatenate([rk, rk], axis=1))
    m["w_up"] = f(inp["rwkv_w_up"].reshape(L, 128, 768))
    m["a_up"] = f(inp["rwkv_a_up"].reshape(L, 128, 768))
    m["g_up"] = f(inp["rwkv_g_up"])
    m["ln_g"] = f(inp["rwkv_ln_g"])
    m["ln_b"] = f(inp["rwkv_ln_b"])
    m["dlam"] = f(inp["diff_lambda"].reshape(L, 256))
    m["subln"] = f(inp["diff_subln_g"].reshape(L, 128, 1))
    m["rel_bias"] = f(inp["rel_bias"])
    m["w_br_a"] = f(inp["w_branch_a"])
    m["w_br_b"] = f(inp["w_branch_b"])
    m["w_br_c"] = f(inp["w_branch_c"])
    m["w_out"] = f(inp["w_out"])
    m["router_w"] = f(inp["router_w"])
    m["router_b"] = f(inp["router_b"])
    w1 = inp["moe_w1"]
    m["moe_w1"] = f(np.stack([w1[..., 0::2], w1[..., 1::2]], axis=3))
    b1 = inp["moe_b1"].reshape(L, E, 8, 128, 2)
    m["b1T"] = f(b1.transpose(0, 3, 1, 4, 2))
    m["moe_w2"] = f(inp["moe_w2"])
    m["moe_b2"] = f(inp["moe_b2"])
    m["fng"] = f(inp["final_norm_g"].reshape(1, D))
    m["cst"], m["EA"], m["EC"] = cst, EA, EC
    return m


_SHARED = ("w_mod", "bmodT", "bmod_row", "ncol", "w_in", "muT", "muL", "rwcol", "rkcol", "w_up", "a_up", "g_up", "ln_g", "ln_b", "dlam",
           "subln", "rel_bias", "w_br_a", "w_br_b", "w_br_c", "w_out", "router_w", "router_b", "moe_w1", "b1T", "moe_w2", "moe_b2",
           "fng", "cst", "EA", "EC")


def kernel(**inp):
    nb = inp["x"].shape[0]
    nc = Builder().build()
    m0 = prep_inputs(inp, 0)
    in_maps = [m0]
    for b in range(1, nb):
        mb = dict(m0)
        mb["x"] = np.ascontiguousarray(inp["x"][b], dtype=np.float32)
        mb["cT"] = np.ascontiguousarray(inp["c"][b].reshape(8, 128).T, dtype=np.float32)
        in_maps.append(mb)
    res = run_bass_kernel_spmd(nc, in_maps, core_ids=list(range(nb)))
    return np.stack([r["y"] for r in res.results], axis=0).astype(np.float32)


def _rev(ap2, start, n):
    if start == 0:
        return ap2[:, n - 1::-1]
    return ap2[:, start + n - 1:start - 1:-1]


def _mixer_phase(self, l):
    P = self.P
    self.norm_phase(0)
    xs = self.x_spill.rearrange("(t p) d -> p t d", p=128)
    rSP = self.R("xspill")
    for q in range(4):
        P.dma(xs[:, q * 4:(q + 1) * 4, :], self.X[:, q * 4:(q + 1) * 4, :], reads=[self.rX], writes=[rSP])
    self.mg_first = True
    stages = getattr(self, "mix_stages", "bac")
    if "b" in stages:
        self.rwkv_phase(l)
    if "a" in stages:
        self.mixa_phase(l)
    if "c" in stages:
        self.mixc_phase(l)
    self.release("ARENA")
    for q in range(4):
        P.dma(self.X[:, q * 4:(q + 1) * 4, :], xs[:, q * 4:(q + 1) * 4, :], reads=[rSP], writes=[self.rX])
    self.wout_phase(l)
    self.release("EX")


def _load_w(self, src_ap, ncols, col_off=0, slot=None):
    if slot is None:
        slot = self.wbi
        self.wbi = (self.wbi + 1) % 2
    wb, rwb = self.WB[slot], self.rWB[slot]
    self.P.dma(wb[:, :, col_off:col_off + ncols], src_ap.rearrange("(kc p) n -> p kc n", p=128), writes=[rwb], eng="pool")
    return wb, rwb, slot


def _merge(self, l, branch, oT_fn, nk):
    P = self.P
    MGv = self.carve(self.MG, 0, [128, KC, T], BF16)
    SIG = [self.view("EX", (32 + i) * 1024, [128, 512], BF16, f"sig{i}") for i in range(2)]
    TMPM = [self.view("EX", (34 + i) * 1024, [128, 512], BF16, f"tmpm{i}") for i in range(2)]
    first = self.mg_first
    self.mg_first = False
    it = 0
    for dcb in range(2):
        gcol = G_OFF + branch * D + dcb * 512
        wg, rwg, _ = self.load_w(self.w_in[l][:, gcol:gcol + 512], 512)
        for dci in range(4):
            dc = dcb * 4 + dci
            for tb in range(4):
                pg, rpg = self.PB[(it % 2) * 2], self.rPB[(it % 2) * 2]
                pbr, rpbr = self.PB[(it % 2) * 2 + 1], self.rPB[(it % 2) * 2 + 1]
                sig, rsig = SIG[it % 2]
                tmp, rtmp = TMPM[it % 2]
                it += 1

                def fg(e, dci=dci, tb=tb, pg=pg, wg=wg):
                    for kc in range(KC):
                        ins = e.matmul(pg[:, :], lhsT=wg[:, kc, dci * 128:(dci + 1) * 128], rhs=self.HT[:, kc, tb * 512:(tb + 1) * 512], start=(kc == 0), stop=(kc == KC - 1))
                    return ins
                P.op("pe", fg, reads=[rwg, self.rHT], writes=[rpg])
                ress = []
                parts = [oT_fn(j) for j in range(nk)]
                for (_, _, rr) in parts:
                    ress.extend(rr)

                def fb(e, dc=dc, tb=tb, pbr=pbr, parts=parts):
                    for j, (wrow, oT, _) in enumerate(parts):
                        ins = e.matmul(pbr[:, :], lhsT=wrow[:, dc * 128:(dc + 1) * 128], rhs=oT[:, tb * 512:(tb + 1) * 512], start=(j == 0), stop=(j == len(parts) - 1))
                    return ins
                P.op("pe", fb, reads=ress, writes=[rpbr])
                P.op("act", lambda e, pg=pg, sig=sig: e.activation(out=sig, in_=pg[:, :], func=AF.Sigmoid), reads=[rpg], writes=[rsig])
                if first:
                    P.op("dve", lambda e, dc=dc, tb=tb, pbr=pbr, sig=sig: e.tensor_tensor(out=MGv[:, dc, tb * 512:(tb + 1) * 512], in0=pbr[:, :], in1=sig, op=ALU.mult),
                         reads=[rpbr, rsig], writes=[self.rMG])
                else:
                    P.op("dve", lambda e, pbr=pbr, sig=sig, tmp=tmp: e.tensor_tensor(out=tmp, in0=pbr[:, :], in1=sig, op=ALU.mult), reads=[rpbr, rsig], writes=[rtmp])
                    P.op("pool", lambda e, dc=dc, tb=tb, tmp=tmp: e.tensor_tensor(out=MGv[:, dc, tb * 512:(tb + 1) * 512], in0=MGv[:, dc, tb * 512:(tb + 1) * 512], in1=tmp, op=ALU.add),
                         reads=[rtmp, self.rMG], writes=[self.rMG])


def _wout_phase(self, l):
    P = self.P
    MGv = self.carve(self.MG, 0, [128, KC, T], BF16)
    GT1b, rGT1b = self.view("EX", 0, [128, D], BF16, "gt1b")
    P.op("act", lambda e: e.activation(out=GT1b, in_=self.GTB[:, 0, :], func=AF.Copy), reads=[self.rGTB], writes=[rGT1b])
    ws = []
    for nb in range(2):
        wb, rwb, s = self.load_w(self.w_out[l][:, nb * 512:(nb + 1) * 512], 512, slot=nb)
        for kc in range(KC):
            P.op("dve" if kc % 2 else "pool", lambda e, wb=wb, kc=kc, nb=nb: e.tensor_tensor(out=wb[:, kc, :], in0=wb[:, kc, :], in1=GT1b[:, nb * 512:(nb + 1) * 512], op=ALU.mult),
                 reads=[rwb, rGT1b], writes=[rwb])
        ws.append((wb, rwb))
    for t in range(NT):
        for nb in range(2):
            wb, rwb = ws[nb]
            po, rpo = self.PB[(t * 2 + nb) % 4], self.rPB[(t * 2 + nb) % 4]

            def f(e, t=t, wb=wb, po=po):
                for kc in range(KC):
                    ins = e.matmul(po[:, :], lhsT=MGv[:, kc, t * 128:(t + 1) * 128], rhs=wb[:, kc, :], start=(kc == 0), stop=(kc == KC - 1))
                return ins
            P.op("pe", f, reads=[self.rMG, rwb], writes=[rpo])
            P.op("dve", lambda e, t=t, nb=nb, po=po: e.tensor_tensor(out=self.X[:, t, nb * 512:(nb + 1) * 512], in0=po[:, :], in1=self.X[:, t, nb * 512:(nb + 1) * 512], op=ALU.add),
                 reads=[rpo, self.rX], writes=[self.rX])


def _eb_build(self):
    P = self.P
    RBA, rRBA = self.view("EX", 0, [33, 18], F32, "rba")
    EAs, rEAs = self.view("EX", 1024, [33, 1536], F32, "eas")
    ECs, rECs = self.view("EX", 1024 + 6144, [33, 1536], F32, "ecs")
    ROW, rROW = self.view("EX", 1024 + 12288, [12, 1536], F32, "row")
    P.op("dve", lambda e: e.memset(RBA[32:33, :], -200.0), writes=[rRBA])
    P.dma(RBA[0:32, :], self.rel_bias[:, :], writes=[rRBA])
    P.dma(EAs, self.EA_in[:, :], writes=[rEAs])
    P.dma(ECs, self.EC_in[:, :], writes=[rECs])
    rA, rC = self.R("ebA"), self.R("ebC")
    for g in range(3):
        pb, rpb = self.PB[g % 2], self.rPB[g % 2]
        P.op("pe", lambda e, g=g, pb=pb: e.matmul(pb[0:4, :], lhsT=RBA[:, g * 4:(g + 1) * 4], rhs=EAs[:, g * 512:(g + 1) * 512], start=True, stop=True), reads=[rRBA, rEAs], writes=[rpb])
        P.op("act", lambda e, g=g, pb=pb: e.activation(out=ROW[0:4, g * 512:(g + 1) * 512], in_=pb[0:4, :], func=AF.Exp), reads=[rpb], writes=[rROW])
        P.dma(self.ebA[g * 4:(g + 1) * 4, :], ROW[0:4, g * 512:(g + 1) * 512], reads=[rROW], writes=[rA])
    ROWC, rROWC = self.view("EX", 1024 + 12288 + 6144, [6, 1536], F32, "rowc")
    for j in range(3):
        pb, rpb = self.PB[2 + j % 2], self.rPB[2 + j % 2]
        P.op("pe", lambda e, j=j, pb=pb: e.matmul(pb[0:6, :], lhsT=RBA[:, 12:18], rhs=ECs[:, j * 512:(j + 1) * 512], start=True, stop=True), reads=[rRBA, rECs], writes=[rpb])
        P.op("act", lambda e, j=j, pb=pb: e.activation(out=ROWC[0:6, j * 512:(j + 1) * 512], in_=pb[0:6, :], func=AF.Exp), reads=[rpb], writes=[rROWC])
    P.dma(self.ebC[:, :], ROWC, reads=[rROWC], writes=[rC])
    self.rEBA, self.rEBC = rA, rC
    self.release("EX")


Builder.mixer_phase = _mixer_phase
Builder.load_w = _load_w
Builder.merge = _merge
Builder.wout_phase = _wout_phase
Builder.eb_build = _eb_build


def _mixa_phase(self, l):
    P = self.P
    K1 = 1024
    GRP = ((0, 1), (1, 4), (2, 16))
    QK, rQK = self.view("ARENA", 0, [128, 2, 3, T], BF16, "qk")
    VA, rVA = self.view("ARENA", 24 * K1, [128, 3, 16, 2, 65], BF16, "va")
    OACC, rOACC = self.view("ARENA", 37 * K1, [65, 2, T], F32, "oacc")
    EBA, rEBAs = self.view("ARENA", 53 * K1, [128, 12, 384], BF16, "eba")
    HST, rHST = self.view("ARENA", 62 * K1, [128, 384], F32, "hst")
    OAT, rOAT = self.view("EX", 0, [64, 4, T], BF16, "oat")
    RDEN, rRDEN = self.view("EX", 16 * K1, [65, T], F32, "rden")
    WBRA, rWBRA = self.view("EX", 24 * K1, [64, 4, D], BF16, "wbra")
    PT = [self.view("EX", 36 * K1 + i * 768, [128, 384], BF16, f"pt{i}") for i in range(3)]
    for idx in range(12):
        P.dma(HST, bass.AP(self.ebA.tensor, idx * 512, [[1, 128], [1, 384]]), reads=[self.rEBA], writes=[rHST])
        for c in range(3):
            P.op("dve", lambda e, idx=idx, c=c: e.tensor_copy(out=EBA[:, idx, c * 128:(c + 1) * 128], in_=_rev(HST, c * 128, 128)), reads=[rHST], writes=[rEBAs])
    P.dma(WBRA, self.w_br_a[l].rearrange("(h p) n -> p h n", p=64), writes=[rWBRA], eng="pool")
    inst = 0
    for hp in range(2):
        for qk in range(2):
            slot = None
            for g in range(3):
                col = A_OFF + qk * 768 + g * 256 + hp * 128
                wb, rwb, slot = self.load_w(self.w_in[l][:, col:col + 128], 128, col_off=g * 128, slot=slot)
            for g in range(3):
                for tb in range(4):
                    pb, rpb = self.PB[inst % 2], self.rPB[inst % 2]
                    inst += 1

                    def f(e, g=g, tb=tb, pb=pb, wb=wb):
                        for kc in range(KC):
                            ins = e.matmul(pb[:, :], lhsT=wb[:, kc, g * 128:(g + 1) * 128], rhs=self.HT[:, kc, tb * 512:(tb + 1) * 512], start=(kc == 0), stop=(kc == KC - 1))
                        return ins
                    P.op("pe", f, reads=[rwb, self.rHT], writes=[rpb])
                    if inst % 2:
                        P.op("act", lambda e, qk=qk, g=g, tb=tb, pb=pb: e.activation(out=QK[:, qk, g, tb * 512:(tb + 1) * 512], in_=pb[:, :], func=AF.Copy), reads=[rpb], writes=[rQK])
                    else:
                        P.op("dve", lambda e, qk=qk, g=g, tb=tb, pb=pb: e.tensor_copy(out=QK[:, qk, g, tb * 512:(tb + 1) * 512], in_=pb[:, :]), reads=[rpb], writes=[rQK])
        slot = None
        for g in range(3):
            col = A_OFF + 1536 + g * 256 + hp * 128
            wbv, rwbv, slot = self.load_w(self.w_in[l][:, col:col + 128], 128, col_off=g * 128, slot=slot)
        import os
        for g in range(3):
            if os.environ.get("NOMEMSET1") and hp == 1:
                continue
            P.op("pool", lambda e, g=g: e.memset(VA[:, g, :, :, 64:65], 1.0), writes=[rVA])
        for g, d in GRP:
            Lg = T // d
            nkt = Lg // 128
            for tq in range(4):
                pb, rpb = self.PB[inst % 2], self.rPB[inst % 2]
                inst += 1

                def f(e, g=g, d=d, tq=tq, pb=pb, nkt=nkt, wbv=wbv):
                    for ti in range(4):
                        tile_i = tq * 4 + ti
                        r_, kt = tile_i // nkt, tile_i % nkt
                        s0 = r_ + d * 128 * kt
                        for kc in range(KC):
                            ins = e.matmul(pb[:, ti * 128:(ti + 1) * 128], lhsT=self.HT[:, kc, s0:s0 + d * 127 + 1:d], rhs=wbv[:, kc, g * 128:(g + 1) * 128],
                                           start=(kc == 0), stop=(kc == KC - 1))
                    return ins
                P.op("pe", f, reads=[rwbv, self.rHT], writes=[rpb])
                P.op("dve", lambda e, g=g, tq=tq, pb=pb: e.tensor_copy(out=VA[:, g, tq * 4:(tq + 1) * 4, :, 0:64], in_=pb[:, :].rearrange("p (a b c) -> p a b c", a=4, b=2)),
                     reads=[rpb], writes=[rVA])
        if "tr" in self.dbg and hp == 1:
            self.dump("dbg_t1", OAT[:, 0:2, :], rOAT, [64, 2, T], BF16)
        for g, d in GRP:
            Lg = T // d
            nkt = Lg // 128
            for r_ in range(d):
                for qt in range(nkt):
                    q0 = r_ + d * 128 * qt
                    kts = [kt for kt in (qt - 1, qt, qt + 1) if 0 <= kt < nkt]
                    c0, c1 = kts[0] - qt + 1, kts[-1] - qt + 2
                    for hh in range(2):
                        hg = 2 * hp + hh
                        ps, rps = self.PB[2 + inst % 2], self.rPB[2 + inst % 2]
                        po, rpo = self.PB[4 + inst % 2], self.rPB[4 + inst % 2]
                        pt, rpt = PT[inst % 3]
                        inst += 1
                        rows = slice(hh * 64, (hh + 1) * 64)

                        def fs(e, g=g, d=d, r_=r_, qt=qt, kts=kts, ps=ps, rows=rows, q0=q0):
                            for kt in kts:
                                c = kt - qt + 1
                                k0 = r_ + d * 128 * kt
                                ins = e.matmul(ps[:, c * 128:(c + 1) * 128], lhsT=QK[rows, 1, g, k0:k0 + d * 127 + 1:d], rhs=QK[rows, 0, g, q0:q0 + d * 127 + 1:d], start=True, stop=True)
                            return ins
                        P.op("pe", fs, reads=[rQK], writes=[rps])
                        P.op("act", lambda e, ps=ps, pt=pt, c0=c0, c1=c1: e.activation(out=pt[:, c0 * 128:c1 * 128], in_=ps[:, c0 * 128:c1 * 128], func=AF.Exp, scale=0.125), reads=[rps], writes=[rpt])
                        P.op("pool", lambda e, pt=pt, c0=c0, c1=c1, g=g, hg=hg: e.tensor_tensor(out=pt[:, c0 * 128:c1 * 128], in0=pt[:, c0 * 128:c1 * 128], in1=EBA[:, g * 4 + hg, c0 * 128:c1 * 128], op=ALU.mult),
                             reads=[rpt, rEBAs], writes=[rpt])

                        def fo(e, g=g, r_=r_, qt=qt, kts=kts, po=po, pt=pt, hh=hh, nkt=nkt):
                            for i, kt in enumerate(kts):
                                c = kt - qt + 1
                                ins = e.matmul(po[0:65, 0:128], lhsT=VA[:, g, r_ * nkt + kt, hh, :], rhs=pt[:, c * 128:(c + 1) * 128], start=(i == 0), stop=(i == len(kts) - 1))
                            return ins
                        P.op("pe", fo, reads=[rVA, rpt], writes=[rpo])
                        dst = OACC[:, hh, q0:q0 + d * 127 + 1:d]
                        if g == 0:
                            P.op("dve", lambda e, dst=dst, po=po: e.tensor_copy(out=dst, in_=po[0:65, 0:128]), reads=[rpo], writes=[rOACC])
                        else:
                            P.op("dve", lambda e, dst=dst, po=po: e.tensor_tensor(out=dst, in0=dst, in1=po[0:65, 0:128], op=ALU.add), reads=[rpo, rOACC], writes=[rOACC])
        if "tr" in self.dbg and hp == 1:
            self.dump("dbg_t2", OAT[:, 0:2, :], rOAT, [64, 2, T], BF16)
        import os
        for hh in range(2):
            if os.environ.get("SKIPN1") and hp == 1:
                continue
            hg = 2 * hp + hh
            P.op("dve", lambda e, hh=hh: e.reciprocal(out=RDEN[64:65, :], in_=OACC[64:65, hh, :]), reads=[rOACC], writes=[rRDEN])
            for tb in range(4):
                pb, rpb = self.PB[inst % 2], self.rPB[inst % 2]
                inst += 1
                P.op("pe", lambda e, tb=tb, pb=pb: e.matmul(pb[0:64, :], lhsT=self.ONES[64:65, 0:64], rhs=RDEN[64:65, tb * 512:(tb + 1) * 512], start=True, stop=True),
                     reads=[rRDEN, self.rCST], writes=[rpb])
                P.op("dve", lambda e, tb=tb, pb=pb, hh=hh, hg=hg: e.tensor_tensor(out=OAT[:, hg, tb * 512:(tb + 1) * 512], in0=OACC[0:64, hh, tb * 512:(tb + 1) * 512], in1=pb[0:64, :], op=ALU.mult),
                     reads=[rpb, rOACC], writes=[rOAT])
        if "tr" in self.dbg and hp == 0:
            self.dump("dbg_t0", OAT[:, 0:2, :], rOAT, [64, 2, T], BF16)
            self.dump("dbg_oacc_t0", OACC, rOACC, [65, 2, T], F32)
            self.dump("dbg_va_t0", VA, rVA, [128, 3, 16, 2, 65], BF16)
            self.dump("dbg_rden_t0", RDEN[64:65, :], rRDEN, [1, T], F32)
        if "oa0" in self.dbg and hp == 0:
            self.dump("dbg_oacc0", OACC, rOACC, [65, 2, T], F32)
            self.dump("dbg_qk0", QK, rQK, [128, 2, 3, T], BF16)
            self.dump("dbg_va0", VA, rVA, [128, 3, 16, 2, 65], BF16)
            self.dump("dbg_oa", OAT, rOAT, [64, 4, T], BF16)
            break
    if "oa" in self.dbg:
        self.dump("dbg_oa", OAT, rOAT, [64, 4, T], BF16)
        self.dump("dbg_qk", QK, rQK, [128, 2, 3, T], BF16)
        self.dump("dbg_va", VA, rVA, [128, 3, 16, 2, 65], BF16)
        self.dump("dbg_oacc", OACC, rOACC, [65, 2, T], F32)
        self.dump("dbg_eba", EBA, rEBAs, [128, 12, 384], BF16)
    self.merge(l, 0, lambda j: (WBRA[:, j, :], OAT[:, j, :], [rWBRA, rOAT]), 4)


Builder.mixa_phase = _mixa_phase


def _mixc_phase(self, l):
    P = self.P
    K1 = 1024
    lambda_init = 0.8 - 0.6 * math.exp(-0.3 * l)
    OCT, rOCT = self.view("EX", 0, [128, 6, T], BF16, "oct")
    SMC, rSMC = self.view("EX", 24 * K1, [128, 512], F32, "smc")
    SC, rSC = self.view("EX", 26 * K1, [128, 64], F32, "sc")
    OF = [self.view("EX", 27 * K1 + i * 512, [128, 128], F32, f"of{i}") for i in range(2)]
    ON = [self.view("EX", 28 * K1 + i * 512, [128, 128], F32, f"on{i}") for i in range(2)]
    PT = [self.view("EX", (29 + i) * K1, [128, 512], BF16, f"ptc{i}") for i in range(3)]
    P.dma(SMC[:, 0:256], self.dlam[l:l + 1, :].partition_broadcast(128), writes=[rSMC])
    P.dma(SC[:, 0:1], self.subln[l], writes=[rSC])
    P.op("dve", lambda e: e.tensor_tensor(out=SMC[:, 256:320], in0=SMC[:, 0:64], in1=SMC[:, 64:128], op=ALU.mult), reads=[rSMC], writes=[rSMC])
    P.op("dve", lambda e: e.tensor_tensor(out=SMC[:, 320:384], in0=SMC[:, 128:192], in1=SMC[:, 192:256], op=ALU.mult), reads=[rSMC], writes=[rSMC])
    P.op("dve", lambda e: e.tensor_reduce(out=SC[:, 1:2], in_=SMC[:, 256:320], axis=AX.X, op=ALU.add), reads=[rSMC], writes=[rSC])
    P.op("dve", lambda e: e.tensor_reduce(out=SC[:, 2:3], in_=SMC[:, 320:384], axis=AX.X, op=ALU.add), reads=[rSMC], writes=[rSC])
    P.op("act", lambda e: e.activation(out=SC[:, 3:5], in_=SC[:, 1:3], func=AF.Exp), reads=[rSC], writes=[rSC])
    P.op("dve", lambda e: e.tensor_tensor(out=SC[:, 5:6], in0=SC[:, 4:5], in1=SC[:, 3:4], op=ALU.subtract), reads=[rSC], writes=[rSC])
    P.op("dve", lambda e: e.tensor_scalar(out=SC[:, 5:6], in0=SC[:, 5:6], scalar1=-lambda_init, scalar2=None, op0=ALU.add), reads=[rSC], writes=[rSC])
    P.op("dve", lambda e: e.tensor_scalar(out=SC[:, 6:7], in0=SC[:, 0:1], scalar1=1.0 - lambda_init, scalar2=None, op0=ALU.mult), reads=[rSC], writes=[rSC])
    NEGLAM, SUBC = SC[:, 5:6], SC[:, 6:7]
    inst = 0
    for ps_ in range(2):
        QC, rQC = self.view("ARENA", 0, [128, 3, T], BF16, "qc")
        KCc, rKC = self.view("ARENA", 12 * K1, [128, 3, T], BF16, "kc")
        VC, rVC = self.view("ARENA", 24 * K1, [128, 16, 3, 129], BF16, "vc")
        GREV, rGREV = self.view("ARENA", 37 * K1, [128, 3, 1408], BF16, "grev")
        HSTC, rHSTC = self.view("ARENA", 46 * K1, [128, 1408], F32, "hstc")
        for hi in range(3):
            h = ps_ * 3 + hi
            P.dma(HSTC, bass.AP(self.ebC.tensor, h * 1536, [[1, 128], [1, 1408]]), reads=[self.rEBC], writes=[rHSTC])
            P.op("dve", lambda e, hi=hi: e.tensor_copy(out=GREV[:, hi, :], in_=_rev(HSTC, 0, 1408)), reads=[rHSTC], writes=[rGREV])
        for qk, (dst, rdst) in enumerate(((QC, rQC), (KCc, rKC))):
            col = C_OFF + qk * 768 + ps_ * 384
            wb, rwb, _ = self.load_w(self.w_in[l][:, col:col + 384], 384)
            for hi in range(3):
                for tb in range(4):
                    pb, rpb = self.PB[inst % 2], self.rPB[inst % 2]
                    inst += 1

                    def f(e, hi=hi, tb=tb, pb=pb, wb=wb):
                        for kc in range(KC):
                            ins = e.matmul(pb[:, :], lhsT=wb[:, kc, hi * 128:(hi + 1) * 128], rhs=self.HT[:, kc, tb * 512:(tb + 1) * 512], start=(kc == 0), stop=(kc == KC - 1))
                        return ins
                    P.op("pe", f, reads=[rwb, self.rHT], writes=[rpb])
                    if inst % 2:
                        P.op("act", lambda e, dst=dst, hi=hi, tb=tb, pb=pb: e.activation(out=dst[:, hi, tb * 512:(tb + 1) * 512], in_=pb[:, :], func=AF.Copy), reads=[rpb], writes=[rdst])
                    else:
                        P.op("dve", lambda e, dst=dst, hi=hi, tb=tb, pb=pb: e.tensor_copy(out=dst[:, hi, tb * 512:(tb + 1) * 512], in_=pb[:, :]), reads=[rpb], writes=[rdst])
        col = C_OFF + 1536 + ps_ * 384
        wbv, rwbv, _ = self.load_w(self.w_in[l][:, col:col + 384], 384)
        P.op("pool", lambda e, VC=VC: e.memset(VC[:, :, :, 128:129], 1.0), writes=[rVC])
        for kt in range(16):
            pb, rpb = self.PB[inst % 2], self.rPB[inst % 2]
            inst += 1

            def f(e, kt=kt, pb=pb, wbv=wbv):
                for kc in range(KC):
                    ins = e.matmul(pb[:, 0:384], lhsT=self.HT[:, kc, kt * 128:(kt + 1) * 128], rhs=wbv[:, kc, 0:384], start=(kc == 0), stop=(kc == KC - 1))
                return ins
            P.op("pe", f, reads=[rwbv, self.rHT], writes=[rpb])
            P.op("dve", lambda e, kt=kt, pb=pb, VC=VC: e.tensor_copy(out=VC[:, kt, :, 0:128], in_=pb[:, 0:384].rearrange("p (a b) -> p a b", a=3)), reads=[rpb], writes=[rVC])
        ACC = [[self.PW[:, j * 256:j * 256 + 129] for j in range(4)],
               [self.PB[4 + j // 2][:, (j % 2) * 256:(j % 2) * 256 + 129] for j in range(4)]]
        rACC = [self.rPW, self.rPB[4], self.rPB[5]]
        for hi in range(3):
            h = ps_ * 3 + hi
            for qb in range(4):
                for kt in range(16):
                    delta = 128 * kt - 512 * qb
                    de = min(max(delta, -256), 640)
                    s0 = 640 - de
                    for c in range(2):
                        ps, rps = self.PB[2 + inst % 2], self.rPB[2 + inst % 2]
                        pt, rpt = PT[inst % 3]
                        inst += 1
                        rows = slice(c * 64, (c + 1) * 64)
                        P.op("pe", lambda e, ps=ps, rows=rows, hi=hi, kt=kt, qb=qb, KCc=KCc, QC=QC: e.matmul(ps[:, :], lhsT=KCc[rows, hi, kt * 128:(kt + 1) * 128], rhs=QC[rows, hi, qb * 512:(qb + 1) * 512], start=True, stop=True),
                             reads=[rQC, rKC], writes=[rps])
                        P.op("act", lambda e, ps=ps, pt=pt: e.activation(out=pt, in_=ps[:, :], func=AF.Exp, scale=0.125), reads=[rps], writes=[rpt])
                        P.op("pool" if inst % 2 else "dve", lambda e, pt=pt, hi=hi, s0=s0, GREV=GREV: e.tensor_tensor(out=pt, in0=pt, in1=GREV[:, hi, s0:s0 + 512], op=ALU.mult), reads=[rpt, rGREV], writes=[rpt])

                        def fo(e, c=c, kt=kt, hi=hi, pt=pt, VC=VC):
                            for j in range(4):
                                ins = e.matmul(ACC[c][j], lhsT=pt[:, j * 128:(j + 1) * 128], rhs=VC[:, kt, hi, :], start=(kt == 0 and j % 2 == 0), stop=(kt == 15 and j % 2 == 1))
                            return ins
                        P.op("pe", fo, reads=[rpt, rVC], writes=[rACC[0]] if c == 0 else [rACC[1], rACC[2]])
                R0, R1 = SC[:, 8:12], SC[:, 12:16]
                P.op("dve", lambda e: e.reciprocal(out=R0, in_=self.PW[:, 128:1024:256]), reads=[rACC[0]], writes=[rSC])
                for j in range(4):
                    P.op("dve", lambda e, j=j: e.reciprocal(out=SC[:, 12 + j:13 + j], in_=ACC[1][j][:, 128:129]), reads=[rACC[1], rACC[2]], writes=[rSC])
                P.op("dve", lambda e: e.tensor_scalar(out=R1, in0=R1, scalar1=NEGLAM, scalar2=None, op0=ALU.mult), reads=[rSC], writes=[rSC])
                for j in range(4):
                    t = qb * 4 + j
                    of, rof = OF[j % 2]
                    on, ron = ON[j % 2]
                    P.op("dve", lambda e, j=j, of=of: e.tensor_scalar(out=of, in0=ACC[0][j][:, 0:128], scalar1=SC[:, 8 + j:9 + j], scalar2=None, op0=ALU.mult), reads=[rACC[0], rSC], writes=[rof])
                    P.op("dve", lambda e, j=j, of=of: e.scalar_tensor_tensor(out=of, in0=ACC[1][j][:, 0:128], scalar=SC[:, 12 + j:13 + j], in1=of, op0=ALU.mult, op1=ALU.add),
                         reads=[rACC[1], rACC[2], rSC, rof], writes=[rof])
                    P.op("act", lambda e, j=j, of=of, on=on: e.activation(out=on, in_=of, func=AF.Square, accum_out=SC[:, 16 + j:17 + j]), reads=[rof], writes=[ron, rSC])
                    P.op("act", lambda e, j=j: e.activation(out=SC[:, 20 + j:21 + j], in_=SC[:, 16 + j:17 + j], func=AF.Ln, scale=1.0 / 128, bias=1e-5), reads=[rSC], writes=[rSC])
                    P.op("act", lambda e, j=j: e.activation(out=SC[:, 20 + j:21 + j], in_=SC[:, 20 + j:21 + j], func=AF.Exp, scale=-0.5), reads=[rSC], writes=[rSC])
                    P.op("act", lambda e, j=j, of=of, on=on: e.activation(out=on, in_=of, func=AF.Copy, scale=SC[:, 20 + j:21 + j]), reads=[rof, rSC], writes=[ron])
                    pbt, rpbt = self.PB[inst % 2], self.rPB[inst % 2]
                    inst += 1
                    P.op("pe", lambda e, on=on, pbt=pbt: e.matmul(pbt[:, 0:128], lhsT=on, rhs=self.IDF, start=True, stop=True), reads=[ron, self.rCST], writes=[rpbt])
                    P.op("dve", lambda e, h=h, t=t, pbt=pbt: e.tensor_scalar(out=OCT[:, h, t * 128:(t + 1) * 128], in0=pbt[:, 0:128], scalar1=SUBC, scalar2=None, op0=ALU.mult),
                         reads=[rpbt, rSC], writes=[rOCT])
    if "oc" in self.dbg:
        self.dump("dbg_oc", OCT, rOCT, [128, 6, T], BF16)
    WBRC, rWBRC = self.view("ARENA", 0, [128, 6, D], BF16, "wbrc")
    P.dma(WBRC, self.w_br_c[l].rearrange("(j p) n -> p j n", p=128), writes=[rWBRC], eng="pool")
    self.merge(l, 2, lambda j: (WBRC[:, j, :], OCT[:, j, :], [rWBRC, rOCT]), 6)


Builder.mixc_phase = _mixc_phase


def _rwkv_phase(self, l):
    P = self.P
    K1 = 1024
    CDEC = DECAY_C
    SL = 8 * K1
    def A(i, shape=(128, T), dt=F32, name="a"):
        return self.view("ARENA", i * SL, list(shape), dt, f"{name}{i}")
    OBT, rOBT = self.view("EX", 0, [128, 6, T], BF16, "obt")
    YH, rYH = self.view("EX", 24 * K1, [128, 16, 64], F32, "yh")
    OBK, rOBK = self.view("EX", 28 * K1, [128, 16, 128], BF16, "obk")
    BON, rBON = self.view("EX", 32 * K1, [128, 16, 12], F32, "bon")
    GUP, rGUP = self.view("EX", 33 * K1, [128, 768], BF16, "gup")
    BDW, rBDW = self.view("EX", 34 * K1 + 512, [128, 128], BF16, "bdw")
    BDA, rBDA = self.view("EX", 34 * K1 + 768, [128, 128], BF16, "bda")
    BONES, rBONES = self.view("EX", 35 * K1, [128, 128], F32, "bones")
    o = 35 * K1 + 512
    RWC, rRWC = self.view("EX", o, [128, 12, 4], F32, "rwc"); o += 192
    RKC, rRKC = self.view("EX", o, [128, 12], F32, "rkc"); o += 48
    OMKA, rOMKA = self.view("EX", o, [128, 12], F32, "omka"); o += 48
    MUT, rMUT = self.view("EX", o, [128, 36, 2], F32, "mut"); o += 288
    C0T, rC0T = self.view("EX", o, [128, 36], F32, "c0t"); o += 144
    MUL, rMUL = self.view("EX", o, [128, 3, 2], F32, "mul"); o += 24
    C0L, rC0L = self.view("EX", o, [128, 4], F32, "c0l"); o += 16
    PCS, rPCS = self.view("EX", o, [64, 2, 16], F32, "pcs"); o += 128
    ST, rST0 = self.view("EX", o, [64, 2, 64], F32, "st"); o += 512
    GNS, rGNS = self.view("EX", o, [128, 64], F32, "gns"); o += 256
    YC, rYC = self.view("EX", o, [128, 64], F32, "yc"); o += 256
    YB, rYB = self.view("EX", o, [128, 64], F32, "yb"); o += 256
    LNG, rLNG = self.view("EX", o, [128, 64], F32, "lng"); o += 256
    LNB, rLNB = self.view("EX", o, [128, 64], F32, "lnb"); o += 256
    assert o <= 40 * K1
    rST = [rST0, self.R("st1")]
    RAW, rRAW = self.view("MG", 0, [128, T + 2], F32, "raw")
    BT, rBT = self.view("MG", 8 * K1 + 512, [128, T], F32, "bt")
    RKD, rRKD = self.view("MG", 16 * K1 + 512, [128, T], F32, "rkd")
    SGD, rSGD = self.view("MG", 24 * K1 + 512, [128, T], BF16, "sgd")
    WUP, rWUP = self.view("MG", 28 * K1 + 512, [128, 768], BF16, "wup")
    AUP, rAUP = self.view("MG", 30 * K1, [128, 768], BF16, "aup")
    TW = self.WB[1][:].rearrange("p a b -> p (a b)")[:, 0:T]
    AD = self.WB[1][:].rearrange("p a b -> p (a b)")[:, T:2 * T]
    rTW = self.rWB[1]
    WB0, rWB0 = self.WB[0], self.rWB[0]
    IDF, ONES = self.IDF, self.ONES

    P.dma(RWC, self.rwcol[l], writes=[rRWC])
    P.dma(RKC, self.rkcol[l], writes=[rRKC])
    P.dma(MUT, self.muT[l], writes=[rMUT])
    P.dma(MUL, self.muL[l], writes=[rMUL])
    P.dma(WUP, self.w_up[l], writes=[rWUP], eng="pool")
    P.dma(AUP, self.a_up[l], writes=[rAUP], eng="pool")
    P.dma(GUP, self.g_up[l], writes=[rGUP], eng="pool")
    P.op("dve", lambda e: e.tensor_scalar(out=OMKA, in0=RWC[:, :, 3], scalar1=-1.0, scalar2=1.0, op0=ALU.mult, op1=ALU.add), reads=[rRWC], writes=[rOMKA])
    P.op("dve", lambda e: e.tensor_tensor(out=C0T, in0=MUT[:, :, 0], in1=MUT[:, :, 1], op=ALU.add), reads=[rMUT], writes=[rC0T])
    P.op("dve", lambda e: e.tensor_scalar(out=C0T, in0=C0T, scalar1=-1.0, scalar2=1.0, op0=ALU.mult, op1=ALU.add), reads=[rC0T], writes=[rC0T])
    P.op("dve", lambda e: e.tensor_tensor(out=C0L[:, 0:3], in0=MUL[:, :, 0], in1=MUL[:, :, 1], op=ALU.add), reads=[rMUL], writes=[rC0L])
    P.op("dve", lambda e: e.tensor_scalar(out=C0L[:, 0:3], in0=C0L[:, 0:3], scalar1=-1.0, scalar2=1.0, op0=ALU.mult, op1=ALU.add), reads=[rC0L], writes=[rC0L])
    P.op("dve", lambda e: e.memset(BONES, 0.0), writes=[rBONES])
    P.op("dve", lambda e: e.memset(BONES[0:64, 0:64], 1.0), writes=[rBONES])
    P.op("dve", lambda e: e.memset(BONES[64:128, 64:128], 1.0), writes=[rBONES])
    P.op("dve", lambda e: e.memset(BDW, 0.0), writes=[rBDW])
    P.op("dve", lambda e: e.memset(BDA, 0.0), writes=[rBDA])
    P.op("dve", lambda e: e.memset(RAW[:, 0:1], 0.0), writes=[rRAW])
    P.op("dve", lambda e: e.memset(RAW[:, T + 1:T + 2], 0.0), writes=[rRAW])
    P.op("dve", lambda e: e.memset(BON, 0.0), writes=[rBON])
    cnt = [0]

    def proj_shift(wb, rwb, wcols, mu0, mu1, c0, rmu, dst, rdst, post):
        for tb in range(4):
            pb, rpb = self.PB[cnt[0] % 2], self.rPB[cnt[0] % 2]
            cnt[0] += 1

            def f(e, tb=tb, pb=pb, wb=wb, wcols=wcols):
                for kc in range(KC):
                    ins = e.matmul(pb[:, :], lhsT=wb[:, kc, wcols], rhs=self.HT[:, kc, tb * 512:(tb + 1) * 512], start=(kc == 0), stop=(kc == KC - 1))
                return ins
            P.op("pe", f, reads=[rwb, self.rHT], writes=[rpb])
            P.op("act", lambda e, tb=tb, pb=pb: e.activation(out=RAW[:, 1 + tb * 512:1 + (tb + 1) * 512], in_=pb[:, :], func=AF.Copy), reads=[rpb], writes=[rRAW])
        post(mu0, mu1, c0, rmu, dst, rdst)

    def shift_to(mu0, mu1, c0, rmu, dst, rdst):
        P.op("dve", lambda e: e.tensor_scalar(out=dst, in0=RAW[:, 1:T + 1], scalar1=c0, scalar2=None, op0=ALU.mult), reads=[rRAW] + rmu, writes=[rdst])
        P.op("dve", lambda e: e.scalar_tensor_tensor(out=dst, in0=RAW[:, 0:T], scalar=mu0, in1=dst, op0=ALU.mult, op1=ALU.add), reads=[rRAW, rdst] + rmu, writes=[rdst])
        P.op("dve", lambda e: e.scalar_tensor_tensor(out=dst, in0=RAW[:, 2:T + 2], scalar=mu1, in1=dst, op0=ALU.mult, op1=ALU.add), reads=[rRAW, rdst] + rmu, writes=[rdst])

    TMPL, rTMPL = A(7, name="tmpl")
    wsrc = self.w_in[l]
    self.P.dma(WB0[:, :, 0:384], wsrc[:, B_OFF + 2304:B_OFF + 2688].rearrange("(kc p) n -> p kc n", p=128), writes=[rWB0], eng="pool")
    for q, (dstv, rdstv, fn_) in enumerate(((TW, rTW, AF.Tanh), (AD, rTW, AF.Copy), (SGD, rSGD, AF.Sigmoid))):
        def post(mu0, mu1, c0, rmu, dst, rdst, dstv=dstv, rdstv=rdstv, fn_=fn_):
            shift_to(mu0, mu1, c0, rmu, dst, rdst)
            P.op("act", lambda e: e.activation(out=dstv, in_=dst, func=fn_), reads=[rdst], writes=[rdstv])
        proj_shift(WB0, rWB0, slice(q * 128, (q + 1) * 128), MUL[:, q, 0:1], MUL[:, q, 1:2], C0L[:, q:q + 1], [rMUL, rC0L], TMPL, rTMPL, post)

    MASK = [self.CST[:, 128:768], self.CST[:, 768:1408]]
    def do_head(h):
        Rr, rR = A(0, name="r")
        Kk, rK = A(1, name="k")
        Vv, rV = A(2, name="v")
        SG, rSG = A(3, name="sg")
        CS, rCS = A(4, name="cs")
        Pp, rPp = A(5, name="p")
        AAa, rAA = A(6, name="aa")
        KK, rKK = A(7, name="kk")
        for w_ in range(3):
            col = B_OFF + w_ * 768 + h * 64
            for dup in range(2):
                P.dma(WB0[:, :, w_ * 128 + dup * 64:w_ * 128 + (dup + 1) * 64], wsrc[:, col:col + 64].rearrange("(kc p) n -> p kc n", p=128), writes=[rWB0], eng="pool")
        for w_, (dst, rdst) in enumerate(((Rr, rR), (Kk, rK), (Vv, rV))):
            ci = w_ * 12 + h
            proj_shift(WB0, rWB0, slice(w_ * 128, (w_ + 1) * 128), MUT[:, ci, 0:1], MUT[:, ci, 1:2], C0T[:, ci:ci + 1], [rMUT, rC0T], dst, rdst, shift_to)
        hc = slice(h * 64, (h + 1) * 64)
        for (BD, rBD, UP, rUP) in ((BDW, rBDW, WUP, rWUP), (BDA, rBDA, AUP, rAUP)):
            P.op("pool", lambda e, BD=BD, UP=UP: e.tensor_copy(out=BD[0:64, 0:64], in_=UP[0:64, hc]), reads=[rUP], writes=[rBD])
            P.op("pool", lambda e, BD=BD, UP=UP: e.tensor_copy(out=BD[64:128, 64:128], in_=UP[64:128, hc]), reads=[rUP], writes=[rBD])
        for (BD, rBD, src, dst, rdst, bcol) in ((BDW, rBDW, TW, SG, rSG, 0), (BDA, rBDA, AD, AAa, rAA, 1)):
            for tb in range(4):
                pb, rpb = self.PB[cnt[0] % 2], self.rPB[cnt[0] % 2]
                cnt[0] += 1
                P.op("pe", lambda e, pb=pb, BD=BD, src=src, tb=tb: e.matmul(pb[:, :], lhsT=BD, rhs=src[:, tb * 512:(tb + 1) * 512], start=True, stop=True), reads=[rBD, rTW], writes=[rpb])
                P.op("act", lambda e, pb=pb, dst=dst, tb=tb, bcol=bcol: e.activation(out=dst[:, tb * 512:(tb + 1) * 512], in_=pb[:, :], func=AF.Sigmoid, bias=RWC[:, h, bcol:bcol + 1]),
                     reads=[rpb, rRWC], writes=[rdst])
        for t in range(NT):
            c0_, c1_ = t * 128, (t + 1) * 128
            P.op("dve", lambda e, c0_=c0_, c1_=c1_: e.tensor_tensor_scan(out=CS[0:64, c0_:c1_], data0=ONES[0:64, :], data1=SG[0:64, c0_:c1_], initial=0.0, op0=ALU.mult, op1=ALU.add),
                 reads=[rSG, self.rCST], writes=[rCS])
            P.op("dve", lambda e, c0_=c0_: e.tensor_tensor_scan(out=_rev(CS[64:128, :], c0_, 128), data0=ONES[64:128, :], data1=_rev(SG[64:128, :], c0_, 128), initial=0.0, op0=ALU.mult, op1=ALU.add),
                 reads=[rSG, self.rCST], writes=[rCS])
        P.op("dve", lambda e: e.tensor_tensor(out=SG, in0=CS, in1=SG, op=ALU.subtract), reads=[rCS, rSG], writes=[rSG])
        P.op("act", lambda e: e.activation(out=SG, in_=SG, func=AF.Exp, scale=-CDEC), reads=[rSG], writes=[rSG])
        P.op("act", lambda e: e.activation(out=Pp, in_=CS, func=AF.Exp, scale=-CDEC), reads=[rCS], writes=[rPp])
        P.op("act", lambda e: e.activation(out=CS, in_=CS, func=AF.Exp, scale=CDEC), reads=[rCS], writes=[rCS])
        PPv, PI = SG, CS
        P.op("dve", lambda e: e.tensor_copy(out=PCS[:, 0, :], in_=Pp[0:64, 127:T:128]), reads=[rPp], writes=[rPCS])
        pb, rpb = self.PB[cnt[0] % 2], self.rPB[cnt[0] % 2]
        cnt[0] += 1
        P.op("pe", lambda e, pb=pb: e.matmul(pb[0:64, 0:16], lhsT=IDF[64:128, 64:128], rhs=Pp[64:128, 0:T:128], start=True, stop=True), reads=[rPp, self.rCST], writes=[rpb])
        P.op("dve", lambda e, pb=pb: e.tensor_copy(out=PCS[:, 1, :], in_=pb[0:64, 0:16]), reads=[rpb], writes=[rPCS])
        P.op("dve", lambda e: e.tensor_scalar(out=KK, in0=Kk, scalar1=RWC[:, h, 2:3], scalar2=None, op0=ALU.mult), reads=[rK, rRWC], writes=[rKK])
        P.op("pool", lambda e: e.tensor_tensor(out=RKD, in0=KK, in1=KK, op=ALU.mult), reads=[rKK], writes=[rRKD])
        for tb in range(4):
            pb, rpb = self.PB[cnt[0] % 2], self.rPB[cnt[0] % 2]
            cnt[0] += 1
            P.op("pe", lambda e, pb=pb, tb=tb: e.matmul(pb[:, :], lhsT=BONES, rhs=RKD[:, tb * 512:(tb + 1) * 512], start=True, stop=True), reads=[rBONES, rRKD], writes=[rpb])
            P.op("dve", lambda e, pb=pb, tb=tb: e.tensor_scalar(out=BT[:, tb * 512:(tb + 1) * 512], in0=pb[:, :], scalar1=1e-24, scalar2=None, op0=ALU.max), reads=[rpb], writes=[rBT])
        P.op("act", lambda e: e.activation(out=BT, in_=BT, func=AF.Ln), reads=[rBT], writes=[rBT])
        P.op("act", lambda e: e.activation(out=BT, in_=BT, func=AF.Exp, scale=-0.5), reads=[rBT], writes=[rBT])
        P.op("dve", lambda e: e.tensor_tensor(out=KK, in0=KK, in1=BT, op=ALU.mult), reads=[rKK, rBT], writes=[rKK])
        AT = PPv
        P.op("dve", lambda e: e.scalar_tensor_tensor(out=AT, in0=KK, scalar=-1.0, in1=PPv, op0=ALU.mult, op1=ALU.mult), reads=[rKK, rSG], writes=[rSG])
        P.op("pool", lambda e: e.tensor_tensor(out=BT, in0=KK, in1=AAa, op=ALU.mult), reads=[rKK, rAA], writes=[rBT])
        P.op("dve", lambda e: e.tensor_tensor(out=BT, in0=BT, in1=PI, op=ALU.mult), reads=[rBT, rCS], writes=[rBT])
        P.op("dve", lambda e: e.tensor_scalar(out=AAa, in0=AAa, scalar1=RWC[:, h, 3:4], scalar2=OMKA[:, h:h + 1], op0=ALU.mult, op1=ALU.add), reads=[rAA, rRWC, rOMKA], writes=[rAA])
        P.op("pool", lambda e: e.tensor_tensor(out=AAa, in0=AAa, in1=Kk, op=ALU.mult), reads=[rAA, rK], writes=[rAA])
        P.op("dve", lambda e: e.tensor_tensor(out=RKD, in0=Rr, in1=AAa, op=ALU.mult), reads=[rR, rAA], writes=[rRKD])
        P.op("pool", lambda e: e.tensor_tensor(out=AAa, in0=AAa, in1=PI, op=ALU.mult), reads=[rAA, rCS], writes=[rAA])
        P.op("dve", lambda e: e.tensor_tensor(out=Rr, in0=Rr, in1=Pp, op=ALU.mult), reads=[rR, rPp], writes=[rR])
        KT, RT = AAa, Rr
        rAT, rKT, rRT = rSG, rAA, rR
        pbb, rpbb = self.PB[cnt[0] % 2], self.rPB[cnt[0] % 2]
        cnt[0] += 1

        def fbon(e, pbb=pbb):
            for t in range(NT):
                ins = e.matmul(pbb[:, t:t + 1], lhsT=RKD[:, t * 128:(t + 1) * 128], rhs=RKC[:, h:h + 1], start=True, stop=True)
            return ins
        P.op("pe", fbon, reads=[rRKD, rRKC], writes=[rpbb])
        P.op("dve", lambda e, pbb=pbb: e.tensor_copy(out=BON[:, :, h], in_=pbb[:, 0:16]), reads=[rpbb], writes=[rBON])
        IB = []
        for d_ in range(2):
            base = (4 + d_) * SL
            AM = self.view("ARENA", base, [128, 640], F32, f"am{d_}")
            LV = self.view("ARENA", base + 2560, [128, 384], F32, f"lv{d_}")
            TK = self.view("ARENA", base + 4096, [128, 256], F32, f"tk{d_}")
            AV = self.view("ARENA", base + 5120, [128, 64], F32, f"av{d_}")
            XU = self.view("ARENA", base + 5376, [128, 128], F32, f"xu{d_}")
            RN = self.view("ARENA", base + 5888, [64, 192], F32, f"rn{d_}")
            IB.append((AM, LV, TK, AV, XU, RN))
        P.op("dve", lambda e: e.memset(ST, 0.0), writes=[rST[0], rST[1]])
        P.op("pool", lambda e: e.memset(YH, 0.0), writes=[rYH])
        for step in range(NT):
            tiles = (step, NT - 1 - step)
            for d_ in range(2):
                (AM, rAM), (LV, rLV), (TK, rTK), (AV, rAV), (XU, rXU), (RN, rRN) = IB[d_]
                ti = tiles[d_]
                tc_ = slice(ti * 128, (ti + 1) * 128)
                rows = slice(d_ * 64, (d_ + 1) * 64)
                px, rpx = self.PB[2 + d_], self.rPB[2 + d_]
                pd, rpd = self.PB[4 + d_], self.rPB[4 + d_]
                pm = self.PW[:, d_ * 512:(d_ + 1) * 512]
                rpm = self.rPW if d_ == 0 else self.rPB[0]
                if d_ == 1:
                    pm = self.PB[0]
                idb = IDF[rows, rows]

                def fa(e, px=px, pd=pd, rows=rows, tc_=tc_):
                    e.matmul(px[:, 0:128], lhsT=BT[rows, tc_], rhs=AT[rows, tc_], start=True, stop=True)
                    e.matmul(px[:, 128:256], lhsT=BT[rows, tc_], rhs=RT[rows, tc_], start=True, stop=True)
                    e.matmul(px[:, 256:384], lhsT=KT[rows, tc_], rhs=AT[rows, tc_], start=True, stop=True)
                    e.matmul(px[:, 384:512], lhsT=KT[rows, tc_], rhs=RT[rows, tc_], start=True, stop=True)
                    return e.matmul(pd[:, 0:128], lhsT=AT[rows, tc_], rhs=BT[rows, tc_], start=True, stop=True)
                P.op("pe", fa, reads=[rBT, rAT, rKT, rRT], writes=[rpx, rpd])
                P.op("dve", lambda e, AM=AM, px=px, d_=d_: e.tensor_tensor(out=AM[:, 0:512], in0=px[:, :], in1=MASK[d_][:, 0:512], op=ALU.mult), reads=[rpx, self.rCST], writes=[rAM])
                LVr = self.LVR[d_]
                P.op("dve", lambda e, LVr=LVr, pd=pd, d_=d_: e.tensor_tensor(out=LVr[:, 0:128], in0=pd[:, 0:128], in1=MASK[d_][:, 512:640], op=ALU.mult), reads=[rpd, self.rCST], writes=[rLV])
                P.op("act", lambda e, LVr=LVr, AM=AM: e.activation(out=LVr[:, 128:256], in_=AM[:, 0:128], func=AF.Copy), reads=[rAM], writes=[rLV])
                P.op("dve", lambda e, LVr=LVr, AM=AM: e.tensor_tensor(out=LVr[:, 256:384], in0=AM[:, 0:128], in1=IDF, op=ALU.add), reads=[rAM, self.rCST], writes=[rLV])
                def ft(e, pm=pm, rows=rows, tc_=tc_, idb=idb):
                    e.matmul(pm[:, 0:64], lhsT=AT[rows, tc_], rhs=idb, start=True, stop=True)
                    e.matmul(pm[:, 64:128], lhsT=BT[rows, tc_], rhs=idb, start=True, stop=True)
                    e.matmul(pm[:, 128:192], lhsT=KT[rows, tc_], rhs=idb, start=True, stop=True)
                    return e.matmul(pm[:, 192:256], lhsT=Vv[rows, tc_], rhs=idb, start=True, stop=True)
                P.op("pe", ft, reads=[rBT, rAT, rKT, rV, self.rCST], writes=[rpm])
                P.op("act", lambda e, TK=TK, pm=pm: e.activation(out=TK, in_=pm[:, 0:256], func=AF.Copy), reads=[rpm], writes=[rTK])
                P.op("pe", lambda e, pm=pm, AM=AM, TK=TK: e.matmul(pm[:, 256:320], lhsT=AM[:, 256:384], rhs=TK[:, 192:256], start=True, stop=True), reads=[rAM, rTK], writes=[rpm])
                P.op("act", lambda e, AV=AV, pm=pm: e.activation(out=AV, in_=pm[:, 256:320], func=AF.Copy), reads=[rpm], writes=[rAV])
            for k in range(7):
                for d_ in range(2):
                    (AM, rAM), (LV, rLV), (TK, rTK), (AV, rAV), (XU, rXU), (RN, rRN) = IB[d_]
                    LVr = self.LVR[d_]
                    pd, rpd = self.PB[4 + d_], self.rPB[4 + d_]
                    if k == 0:
                        def f0(e, pd=pd, LVr=LVr):
                            e.matmul(pd[:, 0:128], lhsT=LVr[:, 128:256], rhs=LVr[:, 0:128], start=True, stop=True)
                            return e.matmul(pd[:, 128:256], lhsT=LVr[:, 0:128], rhs=LVr[:, 128:256], start=True, stop=True)
                        P.op("pe", f0, reads=[rLV], writes=[rpd])
                        if d_:
                            P.op("act", lambda e, pd=pd, LVr=LVr: e.activation(out=LVr[:, 0:256], in_=pd[:, 0:256], func=AF.Copy), reads=[rpd], writes=[rLV])
                        else:
                            P.op("dve", lambda e, pd=pd, LVr=LVr: e.tensor_copy(out=LVr[:, 0:256], in_=pd[:, 0:256]), reads=[rpd], writes=[rLV])
                    elif k < 6:
                        def fk(e, pd=pd, LVr=LVr):
                            e.matmul(pd[:, 0:128], lhsT=LVr[:, 128:256], rhs=LVr[:, 0:128], start=True, stop=True)
                            return e.matmul(pd[:, 128:384], lhsT=LVr[:, 0:128], rhs=LVr[:, 128:384], start=True, stop=True)
                        P.op("pe", fk, reads=[rLV], writes=[rpd])
                        P.op("dve", lambda e, pd=pd, LVr=LVr: e.tensor_tensor(out=LVr[:, 256:384], in0=pd[:, 256:384], in1=LVr[:, 256:384], op=ALU.add), reads=[rpd, rLV], writes=[rLV])
                        P.op("act", lambda e, pd=pd, LVr=LVr: e.activation(out=LVr[:, 0:256], in_=pd[:, 0:256], func=AF.Copy), reads=[rpd], writes=[rLV])
                    else:
                        P.op("pe", lambda e, pd=pd, LVr=LVr: e.matmul(pd[:, 256:384], lhsT=LVr[:, 0:128], rhs=LVr[:, 256:384], start=True, stop=True), reads=[rLV], writes=[rpd])
                        P.op("dve", lambda e, pd=pd, LV=LV, LVr=LVr: e.tensor_tensor(out=LV[:, 256:384], in0=pd[:, 256:384], in1=LVr[:, 256:384], op=ALU.add), reads=[rpd, rLV], writes=[rLV])
            for d_ in range(2):
                (AM, rAM), (LV, rLV), (TK, rTK), (AV, rAV), (XU, rXU), (RN, rRN) = IB[d_]
                ti = tiles[d_]
                tc_ = slice(ti * 128, (ti + 1) * 128)
                rows = slice(d_ * 64, (d_ + 1) * 64)
                pm = self.PW[:, d_ * 512:(d_ + 1) * 512] if d_ == 0 else self.PB[0]
                rpm = self.rPW if d_ == 0 else self.rPB[0]
                px, rpx = self.PB[2 + d_], self.rPB[2 + d_]
                idb = IDF[rows, rows]
                ZT = LV[:, 256:384]
                def fx(e, pm=pm, ZT=ZT, TK=TK, AV=AV):
                    e.matmul(pm[:, 320:384], lhsT=ZT, rhs=TK[:, 0:64], start=True, stop=True)
                    return e.matmul(pm[:, 384:448], lhsT=ZT, rhs=AV, start=True, stop=True)
                P.op("pe", fx, reads=[rLV, rTK, rAV], writes=[rpm])
                P.op("dve", lambda e, XU=XU, pm=pm: e.tensor_copy(out=XU, in_=pm[:, 320:448]), reads=[rpm], writes=[rXU])
                def fr(e, px=px, XU=XU, AM=AM, TK=TK, rows=rows, tc_=tc_, idb=idb):
                    e.matmul(px[0:64, 0:128], lhsT=XU[:, 0:64], rhs=AM[:, 128:256], start=True, stop=False)
                    e.matmul(px[0:64, 0:128], lhsT=idb, rhs=RT[rows, tc_], start=False, stop=True)
                    e.matmul(px[0:64, 128:192], lhsT=XU[:, 0:64], rhs=TK[:, 64:128], start=True, stop=False)
                    return e.matmul(px[0:64, 128:192], lhsT=IDF[0:64, 0:64], rhs=IDF[0:64, 0:64], start=False, stop=True)
                P.op("pe", fr, reads=[rXU, rAM, rTK, rRT, self.rCST], writes=[rpx])
                P.op("act", lambda e, RN=RN, px=px: e.activation(out=RN, in_=px[0:64, 0:192], func=AF.Copy), reads=[rpx], writes=[rRN])
                Sd = ST[:, d_, :]
                def fy(e, pm=pm, AM=AM, XU=XU, TK=TK, RN=RN, Sd=Sd):
                    e.matmul(pm[:, 448:512], lhsT=AM[:, 128:256], rhs=XU[:, 64:128], start=True, stop=False)
                    e.matmul(pm[:, 448:512], lhsT=AM[:, 384:512], rhs=TK[:, 192:256], start=False, stop=False)
                    e.matmul(pm[:, 448:512], lhsT=RN[:, 0:128], rhs=Sd, start=False, stop=True)
                    e.matmul(pm[0:64, 0:64], lhsT=TK[:, 64:128], rhs=XU[:, 64:128], start=True, stop=False)
                    e.matmul(pm[0:64, 0:64], lhsT=TK[:, 128:192], rhs=TK[:, 192:256], start=False, stop=False)
                    return e.matmul(pm[0:64, 0:64], lhsT=RN[:, 128:192], rhs=Sd, start=False, stop=True)
                P.op("pe", fy, reads=[rAM, rXU, rTK, rRN, rST[d_]], writes=[rpm])
                P.op("dve", lambda e, pm=pm, ti=ti: e.tensor_tensor(out=YH[:, ti, :], in0=pm[:, 448:512], in1=YH[:, ti, :], op=ALU.add), reads=[rpm, rYH], writes=[rYH])
                P.op("dve", lambda e, pm=pm, Sd=Sd, d_=d_, ti=ti: e.tensor_scalar(out=Sd, in0=pm[0:64, 0:64], scalar1=PCS[:, d_, ti:ti + 1], scalar2=None, op0=ALU.mult), reads=[rpm, rPCS], writes=[rST[d_]])
        P.dma(LNG, self.ln_g[l:l + 1, h * 64:(h + 1) * 64].partition_broadcast(128), writes=[rLNG])
        P.dma(LNB, self.ln_b[l:l + 1, h * 64:(h + 1) * 64].partition_broadcast(128), writes=[rLNB])
        MEAN, VAR = GNS[:, 0:16], GNS[:, 16:32]
        P.op("dve", lambda e: e.tensor_reduce(out=MEAN, in_=YH, axis=AX.X, op=ALU.add), reads=[rYH], writes=[rGNS])
        P.op("dve", lambda e: e.tensor_scalar(out=MEAN, in0=MEAN, scalar1=1.0 / 64, scalar2=None, op0=ALU.mult), reads=[rGNS], writes=[rGNS])
        for t in range(NT):
            P.op("dve", lambda e, t=t: e.tensor_scalar(out=YH[:, t, :], in0=YH[:, t, :], scalar1=MEAN[:, t:t + 1], scalar2=None, op0=ALU.subtract), reads=[rYH, rGNS], writes=[rYH])
            P.op("act", lambda e, t=t: e.activation(out=YC, in_=YH[:, t, :], func=AF.Square, accum_out=VAR[:, t:t + 1]), reads=[rYH], writes=[rYC, rGNS])
        P.op("act", lambda e: e.activation(out=VAR, in_=VAR, func=AF.Ln, scale=1.0 / 64, bias=64e-5), reads=[rGNS], writes=[rGNS])
        P.op("act", lambda e: e.activation(out=VAR, in_=VAR, func=AF.Exp, scale=-0.5), reads=[rGNS], writes=[rGNS])
        for t in range(NT):
            tc_ = slice(t * 128, (t + 1) * 128)
            pb, rpb = self.PB[cnt[0] % 2], self.rPB[cnt[0] % 2]
            cnt[0] += 1

            def fe(e, pb=pb, tc_=tc_):
                e.matmul(pb[:, 0:64], lhsT=Vv[0:64, tc_], rhs=IDF[0:64, 0:64], start=True, stop=True)
                return e.matmul(pb[:, 64:128], lhsT=SGD[:, tc_], rhs=GUP[:, hc], start=True, stop=True)
            P.op("pe", fe, reads=[rV, rSGD, rGUP, self.rCST], writes=[rpb])
            P.op("dve", lambda e, t=t: e.scalar_tensor_tensor(out=YC, in0=YH[:, t, :], scalar=VAR[:, t:t + 1], in1=LNG, op0=ALU.mult, op1=ALU.mult), reads=[rYH, rGNS, rLNG], writes=[rYC])
            P.op("pool", lambda e: e.tensor_tensor(out=YC, in0=YC, in1=LNB, op=ALU.add), reads=[rYC, rLNB], writes=[rYC])
            P.op("dve", lambda e, t=t, pb=pb: e.scalar_tensor_tensor(out=YB, in0=pb[:, 0:64], scalar=BON[:, t, h:h + 1], in1=YC, op0=ALU.mult, op1=ALU.add), reads=[rpb, rBON, rYC], writes=[rYB])
            P.op("dve", lambda e, t=t, pb=pb: e.tensor_tensor(out=OBK[:, t, (h % 2) * 64:(h % 2) * 64 + 64], in0=YB, in1=pb[:, 64:128], op=ALU.mult), reads=[rYB, rpb], writes=[rOBK])
        if h % 2 == 1:
            j = h // 2
            for tq in range(4):
                pbt = self.PB[cnt[0] % 2]
                rpbt = self.rPB[cnt[0] % 2]
                cnt[0] += 1

                def ftr(e, pbt=pbt, tq=tq):
                    for tt in range(4):
                        ins = e.matmul(pbt[:, tt * 128:(tt + 1) * 128], lhsT=OBK[:, tq * 4 + tt, :], rhs=self.IDB[:], start=True, stop=True)
                    return ins
                P.op("pe", ftr, reads=[rOBK, self.rIDB], writes=[rpbt])
                P.op("act", lambda e, pbt=pbt, tq=tq, j=j: e.activation(out=OBT[:, j, tq * 512:(tq + 1) * 512], in_=pbt[:, 0:512], func=AF.Copy), reads=[rpbt], writes=[rOBT])
    for h_ in range(12):
        do_head(h_)
    if "ob" in self.dbg:
        self.dump("dbg_ob", OBT, rOBT, [128, 6, T], BF16)
    WBRB, rWBRB = self.view("ARENA", 0, [128, 6, D], BF16, "wbrb")
    P.dma(WBRB, self.w_br_b[l].rearrange("(j p) n -> p j n", p=128), writes=[rWBRB], eng="pool")
    self.release("MG")
    self.merge(l, 1, lambda j: (WBRB[:, j, :], OBT[:, j, :], [rWBRB, rOBT]), 6)


Builder.rwkv_phase = _rwkv_phase
```
